# Optimizing a Trainium2 kernel written in Bass

```python
import jax, jax.numpy as jnp
from jax import lax
import numpy as np

D_MODEL = 1024
BATCH = 4
SEQ = 8192
DEPTH = 1

D_RNN = 1024
RNN_BLOCKS = 8
RNN_BLOCK_W = D_RNN // RNN_BLOCKS
CONV_W = 4
LRU_C = 8.0
N_HEADS = 8
QK_NOPE = 128
QK_ROPE = 64
QK_DIM = QK_NOPE + QK_ROPE
V_DIM = 128
Q_LORA = 256
KV_LORA = 256
ROPE_THETA = 10000.0
Q_BLOCK = 128
N_GROUPS = 8
EXPERTS_PER_GROUP = 8
N_EXPERTS = N_GROUPS * EXPERTS_PER_GROUP
TOP_K = 2
D_EXPERT = 256
MOE_BLOCK = 128
EPS = 1e-6

IN_SPLITS = (D_RNN, D_RNN, Q_LORA, KV_LORA, QK_ROPE, D_MODEL, D_MODEL)
D_IN = D_RNN + D_RNN + Q_LORA + KV_LORA + QK_ROPE + D_MODEL + D_MODEL

kernel_name = "hybrid_rglru_mla_hiermoe_block"


def rmsnorm(x, g):
    xf = x.astype(jnp.float32)
    y = xf * lax.rsqrt(jnp.mean(xf * xf, axis=-1, keepdims=True) + EPS)
    return (y * g.astype(jnp.float32)).astype(x.dtype)


def causal_depthwise_conv(x, w, b):
    y = lax.conv_general_dilated(
        x, w[:, None, :].astype(x.dtype), window_strides=(1,),
        padding=[(CONV_W - 1, 0)], dimension_numbers=("NWC", "WIO", "NWC"),
        feature_group_count=x.shape[-1])
    return y + b.astype(x.dtype)


def rg_lru(x, wa, ba, wx, bx, lam):
    B, S, C = x.shape
    xb = x.reshape(B, S, RNN_BLOCKS, RNN_BLOCK_W)
    r = jax.nn.sigmoid(jnp.einsum("bsnk,nkj->bsnj", xb, wa).reshape(B, S, C) + ba)
    i = jax.nn.sigmoid(jnp.einsum("bsnk,nkj->bsnj", xb, wx).reshape(B, S, C) + bx)
    log_a = -LRU_C * r.astype(jnp.float32) * jax.nn.softplus(-lam.astype(jnp.float32))
    a = jnp.exp(log_a)
    mult = jnp.sqrt(-jnp.expm1(2.0 * log_a))
    b_in = x.astype(jnp.float32) * i.astype(jnp.float32) * mult

    def combine(left, right):
        a1, b1 = left
        a2, b2 = right
        return a1 * a2, a2 * b1 + b2

    _, h = lax.associative_scan(combine, (a, b_in), axis=1)
    return h.astype(x.dtype)


def apply_rope(x, cos, sin):
    half = x.shape[-1] // 2
    x1, x2 = x[..., :half], x[..., half:]
    return jnp.concatenate([x1 * cos - x2 * sin, x2 * cos + x1 * sin], axis=-1)


def mla_attention(c_q, c_kv, k_pe, positions, q_norm_g, w_uq, kv_norm_g, w_ukv,
                  qk_q_g, qk_k_g, w_mla_o):
    B, S, _ = c_q.shape
    q = (rmsnorm(c_q, q_norm_g) @ w_uq).reshape(B, S, N_HEADS, QK_DIM)
    kv = (rmsnorm(c_kv, kv_norm_g) @ w_ukv).reshape(B, S, N_HEADS, QK_NOPE + V_DIM)
    k_nope, v = kv[..., :QK_NOPE], kv[..., QK_NOPE:]
    k_pe_h = jnp.broadcast_to(k_pe[:, :, None, :], (B, S, N_HEADS, QK_ROPE))
    k = jnp.concatenate([k_nope, k_pe_h], axis=-1)
    q = rmsnorm(q, qk_q_g)
    k = rmsnorm(k, qk_k_g)
    inv_freq = ROPE_THETA ** (-jnp.arange(0, QK_ROPE, 2, dtype=jnp.float32) / QK_ROPE)
    ang = positions.astype(jnp.float32)[..., None] * inv_freq
    cos = jnp.cos(ang)[:, :, None, :].astype(q.dtype)
    sin = jnp.sin(ang)[:, :, None, :].astype(q.dtype)
    q = jnp.concatenate([q[..., :QK_NOPE], apply_rope(q[..., QK_NOPE:], cos, sin)], axis=-1)
    k = jnp.concatenate([k[..., :QK_NOPE], apply_rope(k[..., QK_NOPE:], cos, sin)], axis=-1)
    scale = QK_DIM ** -0.5
    outs = []
    for s0 in range(0, S, Q_BLOCK):
        e = s0 + Q_BLOCK
        s = jnp.einsum("bqhd,bkhd->bhqk", q[:, s0:e], k[:, :e]).astype(jnp.float32) * scale
        mask = jnp.arange(e)[None, :] <= (s0 + jnp.arange(Q_BLOCK))[:, None]
        p = jax.nn.softmax(jnp.where(mask, s, -jnp.inf), axis=-1).astype(v.dtype)
        outs.append(jnp.einsum("bhqk,bkhd->bqhd", p, v[:, :e]))
    o = jnp.concatenate(outs, axis=1).reshape(B, S, N_HEADS * V_DIM)
    return o @ w_mla_o


def hier_moe(u, wg, bg, we, be, w1, w3, w2):
    B, S, D = u.shape
    N = B * S
    xf = u.reshape(N, D)
    g_logits = (xf @ wg).astype(jnp.float32) + bg.astype(jnp.float32)
    g_prob = jax.nn.softmax(g_logits, axis=-1)
    g_sel = jnp.argmax(g_logits, axis=-1).astype(jnp.int32)
    e_logits = ((xf @ we).astype(jnp.float32) + be.astype(jnp.float32)).reshape(N, N_GROUPS, EXPERTS_PER_GROUP)
    e_in_group = jnp.take_along_axis(e_logits, g_sel[:, None, None], axis=1)[:, 0]
    e_prob = jax.nn.softmax(e_in_group, axis=-1)
    top_p, top_j = lax.top_k(e_prob, TOP_K)
    gate = jnp.take_along_axis(g_prob, g_sel[:, None], axis=1) * top_p / jnp.sum(top_p, axis=-1, keepdims=True)
    expert_id = g_sel[:, None] * EXPERTS_PER_GROUP + top_j.astype(jnp.int32)
    M = N * TOP_K
    flat_e = expert_id.reshape(M)
    flat_tok = jnp.repeat(jnp.arange(N, dtype=jnp.int32), TOP_K)
    flat_w = gate.reshape(M)
    order = jnp.argsort(flat_e)
    sorted_e = flat_e[order]
    counts = jnp.bincount(flat_e, length=N_EXPERTS)
    padded = ((counts + MOE_BLOCK - 1) // MOE_BLOCK) * MOE_BLOCK
    start = jnp.cumsum(counts) - counts
    pad_end = jnp.cumsum(padded)
    pad_start = pad_end - padded
    dest = pad_start[sorted_e] + (jnp.arange(M, dtype=jnp.int32) - start[sorted_e])
    m_pad = M + N_EXPERTS * MOE_BLOCK
    n_blocks = m_pad // MOE_BLOCK
    tok_buf = jnp.full((m_pad,), N, dtype=jnp.int32).at[dest].set(flat_tok[order])
    w_buf = jnp.zeros((m_pad,), dtype=jnp.float32).at[dest].set(flat_w[order])
    block_e = jnp.minimum(
        jnp.searchsorted(pad_end, jnp.arange(n_blocks, dtype=jnp.int32) * MOE_BLOCK, side="right"),
        N_EXPERTS - 1).astype(jnp.int32)
    x_pad = jnp.concatenate([xf, jnp.zeros((1, D), xf.dtype)], axis=0)[tok_buf]
    x_pad = x_pad.reshape(n_blocks, MOE_BLOCK, D)

    def expert_block(args):
        xb, e = args
        h = jax.nn.silu(xb @ w1[e]) * (xb @ w3[e])
        return h @ w2[e]

    y_pad = lax.map(expert_block, (x_pad, block_e)).reshape(m_pad, D)
    y = jax.ops.segment_sum(y_pad * w_buf[:, None].astype(y_pad.dtype), tok_buf, num_segments=N + 1)[:N]
    return y.reshape(B, S, D)


def setup_inputs(seed: int = 0) -> dict:
    key = jax.random.key(seed)
    ks = jax.random.split(key, 32)
    L, D, f32 = DEPTH, D_MODEL, jnp.float32

    def nrm(k, shape, fan_in):
        return jax.random.normal(k, shape, f32) * (fan_in ** -0.5)

    def gain(k, shape):
        return 1.0 + 0.1 * jax.random.normal(k, shape, f32)

    def bias(k, shape):
        return 0.01 * jax.random.normal(k, shape, f32)

    x = jax.random.normal(ks[0], (BATCH, SEQ, D), f32)
    offset = jax.random.randint(ks[1], (BATCH, 1), 0, 4096, dtype=jnp.int32)
    positions = offset + jnp.arange(SEQ, dtype=jnp.int32)[None, :]
    a0 = jax.random.uniform(ks[2], (L, D_RNN), f32, 0.9, 0.999)
    s0 = a0 ** (1.0 / LRU_C)
    lru_lambda = jnp.log(s0) - jnp.log1p(-s0)
    return {
        "x": x,
        "positions": positions,
        "norm1_g": gain(ks[3], (L, D)),
        "w_in": nrm(ks[4], (L, D, D_IN), D),
        "conv_w": nrm(ks[5], (L, CONV_W, D_RNN), CONV_W),
        "conv_b": bias(ks[6], (L, D_RNN)),
        "lru_wa": nrm(ks[7], (L, RNN_BLOCKS, RNN_BLOCK_W, RNN_BLOCK_W), RNN_BLOCK_W),
        "lru_ba": bias(ks[8], (L, D_RNN)),
        "lru_wx": nrm(ks[9], (L, RNN_BLOCKS, RNN_BLOCK_W, RNN_BLOCK_W), RNN_BLOCK_W),
        "lru_bx": bias(ks[10], (L, D_RNN)),
        "lru_lambda": lru_lambda,
        "w_rnn_o": nrm(ks[11], (L, D_RNN, D), D_RNN),
        "q_norm_g": gain(ks[12], (L, Q_LORA)),
        "w_uq": nrm(ks[13], (L, Q_LORA, N_HEADS * QK_DIM), Q_LORA),
        "kv_norm_g": gain(ks[14], (L, KV_LORA)),
        "w_ukv": nrm(ks[15], (L, KV_LORA, N_HEADS * (QK_NOPE + V_DIM)), KV_LORA),
        "qk_norm_q_g": gain(ks[16], (L, QK_DIM)),
        "qk_norm_k_g": gain(ks[17], (L, QK_DIM)),
        "w_mla_o": nrm(ks[18], (L, N_HEADS * V_DIM, D), N_HEADS * V_DIM),
        "w_out": nrm(ks[19], (L, D, D), D),
        "norm2_g": gain(ks[20], (L, D)),
        "router_wg": nrm(ks[21], (L, D, N_GROUPS), D),
        "router_bg": bias(ks[22], (L, N_GROUPS)),
        "router_we": nrm(ks[23], (L, D, N_EXPERTS), D),
        "router_be": bias(ks[24], (L, N_EXPERTS)),
        "exp_w1": nrm(ks[25], (L, N_EXPERTS, D, D_EXPERT), D),
        "exp_w3": nrm(ks[26], (L, N_EXPERTS, D, D_EXPERT), D),
        "exp_w2": nrm(ks[27], (L, N_EXPERTS, D_EXPERT, D), D_EXPERT),
    }


def reference(x, positions, norm1_g, w_in, conv_w, conv_b, lru_wa, lru_ba, lru_wx, lru_bx,
              lru_lambda, w_rnn_o, q_norm_g, w_uq, kv_norm_g, w_ukv, qk_norm_q_g, qk_norm_k_g,
              w_mla_o, w_out, norm2_g, router_wg, router_bg, router_we, router_be,
              exp_w1, exp_w3, exp_w2):
    split_points = [int(p) for p in np.cumsum(IN_SPLITS)[:-1]]
    h = x
    for l in range(DEPTH):
        u = rmsnorm(h, norm1_g[l])
        proj = u @ w_in[l]
        x_r, y_r, c_q, c_kv, k_pe, g_a, g_b = jnp.split(proj, split_points, axis=-1)
        x_r = causal_depthwise_conv(x_r, conv_w[l], conv_b[l])
        h_r = rg_lru(x_r, lru_wa[l], lru_ba[l], lru_wx[l], lru_bx[l], lru_lambda[l])
        branch_a = (h_r * jax.nn.gelu(y_r, approximate=True)) @ w_rnn_o[l]
        branch_b = mla_attention(c_q, c_kv, k_pe, positions, q_norm_g[l], w_uq[l], kv_norm_g[l],
                                 w_ukv[l], qk_norm_q_g[l], qk_norm_k_g[l], w_mla_o[l])
        merged = jax.nn.sigmoid(g_a) * branch_a + jax.nn.sigmoid(g_b) * branch_b
        h = h + merged @ w_out[l]
        h = h + hier_moe(rmsnorm(h, norm2_g[l]), router_wg[l], router_bg[l], router_we[l],
                         router_be[l], exp_w1[l], exp_w3[l], exp_w2[l])
    return h
```

```python
import math
import numpy as np
from contextlib import ExitStack
import ml_dtypes
import concourse.bass as bass
import concourse.mybir as mybir
from concourse.bass_utils import run_bass_kernel_spmd

F32 = mybir.dt.float32
BF16 = mybir.dt.bfloat16
I32 = mybir.dt.int32
AF = mybir.ActivationFunctionType
ALU = mybir.AluOpType

D = 1024
S = 8192
NU = 16
UT = 512
NOWN = 8
H = 8
EPS = 1e-6
OWN = {0: [0, 3, 4, 7, 8, 11, 12, 15], 1: [1, 2, 5, 6, 9, 10, 13, 14]}
EXT = [4 * (i // 2) + (2 if i % 2 == 0 else 4) for i in range(NOWN)]
MPAD = 16384
NBLK = MPAD // 128
TWO_PI = 6.283185
QSCALE = 192.0 ** -0.5


class T:
    __slots__ = ("t", "name", "writes", "reads")

    def __init__(self, t, name=""):
        self.t = t
        self.name = name
        self.writes = {}
        self.reads = {}

    def __getitem__(self, idx):
        return self.t[idx]


class Ctx:
    ENG = ("pe", "act", "dve", "pool", "sp")

    def __init__(self, nc, stack, block, n_dma_sems=10):
        self.nc = nc
        self.stack = stack
        self.block = block
        self.cnt = {}
        self.semobj = {}
        for n in self.ENG:
            self.semobj["e_" + n] = stack.enter_context(nc.semaphore("s_" + n))
            self.cnt["e_" + n] = 0
        self.dq = {}
        for q in ("sp", "act", "pool"):
            lst = []
            for i in range(n_dma_sems):
                key = "d_%s_%d" % (q, i)
                self.semobj[key] = stack.enter_context(nc.semaphore(key))
                self.cnt[key] = 0
                lst.append(key)
            self.dq[q] = [lst, 0]
        self.seen = {n: {} for n in self.ENG}
        self.prog = {n: [] for n in self.ENG}
        self.ninst = 0

    def flush(self):
        b = self.block
        starters = {"pe": b.tensor, "act": b.scalar, "dve": b.vector, "pool": b.gpsimd, "sp": b.sync}
        for n in self.ENG:
            lst = self.prog[n]
            if not lst:
                continue

            def body(eng, lst=lst):
                for f in lst:
                    f(eng)
            starters[n](body)
            self.prog[n] = []

    def barrier(self):
        toks = dict(self.cnt)
        for n in self.ENG:
            self._need(n, {k: v for k, v in toks.items() if v > 0}, force_own=False)

    def sb(self, name, shape, dt, stack=None):
        self.uid = getattr(self, "uid", 0) + 1
        name = "%s_%d" % (name, self.uid)
        return T((stack or self.stack).enter_context(self.nc.sbuf_tensor(name, list(shape), dt)), name)

    def ps(self, name, shape, dt=F32, stack=None):
        self.uid = getattr(self, "uid", 0) + 1
        name = "%s_%d" % (name, self.uid)
        return T((stack or self.stack).enter_context(self.nc.psum_tensor(name, list(shape), dt)), name)

    def _need(self, eng, toks, force_own=True):
        seen = self.seen[eng]
        own = "e_" + eng
        for k, v in toks.items():
            if k == own and (eng == "pe" or not force_own):
                continue
            if seen.get(k, 0) >= v:
                continue
            self.prog[eng].append(lambda e, s=self.semobj[k], v=v: e.wait_ge(s, v))
            seen[k] = v
            self.ninst += 1

    def _deps(self, eng, reads, writes):
        toks = {}
        for t in reads:
            for k, v in t.writes.items():
                if toks.get(k, 0) < v:
                    toks[k] = v
        for t in writes:
            for k, v in t.writes.items():
                if toks.get(k, 0) < v:
                    toks[k] = v
            for k, v in t.reads.items():
                if toks.get(k, 0) < v:
                    toks[k] = v
        self._need(eng, toks)

    def op(self, eng, fn, reads=(), writes=()):
        self._deps(eng, reads, writes)
        key = "e_" + eng
        self.cnt[key] += 1
        v = self.cnt[key]
        self.prog[eng].append(lambda e, fn=fn, s=self.semobj[key]: fn(e).then_inc(s, 1))
        for t in reads:
            t.reads[key] = v
        for t in writes:
            t.writes[key] = v
        self.ninst += 1

    def dma(self, q, fn, reads=(), writes=()):
        self._deps(q, reads, writes)
        lst, i = self.dq[q]
        key = lst[i % len(lst)]
        self.dq[q][1] = i + 1
        if self.cnt[key] > 0 and self.seen[q].get(key, 0) < self.cnt[key]:
            self.prog[q].append(lambda e, s=self.semobj[key], v=self.cnt[key]: e.wait_ge(s, v))
            self.seen[q][key] = self.cnt[key]
        self.cnt[key] += 16
        v = self.cnt[key]
        self.prog[q].append(lambda e, fn=fn, s=self.semobj[key]: fn(e).then_inc(s, 16))
        for t in reads:
            t.reads[key] = v
        for t in writes:
            t.writes[key] = v
        self.ninst += 1

    def wait_all(self, eng, tiles):
        toks = {}
        for t in tiles:
            for d in (t.writes, t.reads):
                for k, v in d.items():
                    if toks.get(k, 0) < v:
                        toks[k] = v
        self._need(eng, toks)


class Rot:
    def __init__(self, c, name, shape, dt, n, st, psum=False):
        self.tiles = [(c.ps if psum else c.sb)("%s%d" % (name, i), shape, dt, st) for i in range(n)]
        self.i = 0

    def get(self):
        t = self.tiles[self.i % len(self.tiles)]
        self.i += 1
        return t


def act(c, out_t, out_ap, in_t, in_ap, func, extra_r=(), extra_w=(), **kw):
    c.op("act", lambda e: e.activation(out=out_ap, in_=in_ap, func=func, **kw),
         reads=[in_t] + list(extra_r), writes=[out_t] + list(extra_w))


def ts(c, eng, out_t, out_ap, in_t, in_ap, s1, s2, op0, op1=None, extra_r=()):
    if op1 is None:
        c.op(eng, lambda e: e.tensor_scalar(out=out_ap, in0=in_ap, scalar1=s1, scalar2=None, op0=op0),
             reads=[in_t] + list(extra_r), writes=[out_t])
    else:
        c.op(eng, lambda e: e.tensor_scalar(out=out_ap, in0=in_ap, scalar1=s1, scalar2=s2, op0=op0, op1=op1),
             reads=[in_t] + list(extra_r), writes=[out_t])


def tt(c, eng, out_t, out_ap, a_t, a_ap, b_t, b_ap, op):
    c.op(eng, lambda e: e.tensor_tensor(out=out_ap, in0=a_ap, in1=b_ap, op=op), reads=[a_t, b_t], writes=[out_t])


def stt(c, out_t, out_ap, a_t, a_ap, scalar, b_t, b_ap, op0, op1, extra_r=(), extra_w=(), **kw):
    c.op("dve", lambda e: e.scalar_tensor_tensor(out=out_ap, in0=a_ap, scalar=scalar, in1=b_ap, op0=op0, op1=op1, **kw),
         reads=[a_t, b_t] + list(extra_r), writes=[out_t] + list(extra_w))


def cp(c, eng, out_t, out_ap, in_t, in_ap):
    if eng == "act":
        c.op("act", lambda e: e.copy(out=out_ap, in_=in_ap), reads=[in_t], writes=[out_t])
    else:
        c.op(eng, lambda e: e.tensor_copy(out=out_ap, in_=in_ap), reads=[in_t], writes=[out_t])


def mm(c, out_t, out_ap, l_t, l_ap, r_t, r_ap, start, stop):
    c.op("pe", lambda e: e.matmul(out_ap, lhsT=l_ap, rhs=r_ap, start=start, stop=stop, skip_group_check=True),
         reads=[l_t, r_t], writes=[out_t])


def tr(c, out_t, out_ap, in_t, in_ap, id_t, id_ap):
    c.op("pe", lambda e: e.transpose(out=out_ap, in_=in_ap, identity=id_ap), reads=[in_t, id_t], writes=[out_t])


def rstd_from_ss(c, ss_t, ss_ap, n, rms_t, rms_ap, rstd_t, rstd_ap):
    ts(c, "dve", rms_t, rms_ap, ss_t, ss_ap, 1.0 / n, EPS, ALU.mult, ALU.add)
    act(c, rms_t, rms_ap, rms_t, rms_ap, AF.Sqrt)
    c.op("dve", lambda e: e.reciprocal(out=rstd_ap, in_=rms_ap), reads=[rms_t], writes=[rstd_t])


def trig_tables(c, st, posf_t, nblk, invf_t, cos_t, sin_t):
    ang = c.sb("tg_ang", [128, nblk, 32], F32, st)
    ki = c.sb("tg_ki", [128, nblk, 32], I32, st)
    kf = c.sb("tg_kf", [128, nblk, 32], F32, st)
    fl = c.sb("tg_fl", [128, nblk, 32], F32, st)
    tt(c, "dve", ang, ang[:], posf_t, posf_t[:, 0:nblk].unsqueeze(2).to_broadcast([128, nblk, 32]),
       invf_t, invf_t[:, :].unsqueeze(1).to_broadcast([128, nblk, 32]), ALU.mult)
    for (dst, shift) in ((sin_t, 0.0), (cos_t, 0.25)):
        ts(c, "dve", kf, kf[:], ang, ang[:], 1.0 / (2 * math.pi), shift, ALU.mult, ALU.add)
        cp(c, "dve", ki, ki[:], kf, kf[:])
        cp(c, "dve", fl, fl[:], ki, ki[:])
        tt(c, "dve", kf, kf[:], kf, kf[:], fl, fl[:], ALU.subtract)
        ts(c, "dve", fl, fl[:], kf, kf[:], 0.5, None, ALU.is_gt)
        tt(c, "dve", kf, kf[:], kf, kf[:], fl, fl[:], ALU.subtract)
        ts(c, "dve", fl, fl[:], kf, kf[:], -0.5, None, ALU.is_lt)
        tt(c, "dve", kf, kf[:], kf, kf[:], fl, fl[:], ALU.add)
        act(c, dst, dst[:], kf, kf[:], AF.Sin, scale=TWO_PI)


def rope(c, eng, out_t, out_ap3, x_t, x_ap3, cos_t, cos_ap3, sin_t, sin_ap3, tmp_t, tmp_ap3, nh):
    x1, x2 = x_ap3[:, :, 0:32], x_ap3[:, :, 32:64]
    o1, o2 = out_ap3[:, :, 0:32], out_ap3[:, :, 32:64]
    t1 = tmp_ap3[:, :, 0:32]
    t2 = tmp_ap3[:, :, 32:64]
    tt(c, eng, tmp_t, t1, x_t, x2, sin_t, sin_ap3, ALU.mult)
    tt(c, eng, tmp_t, t2, x_t, x1, sin_t, sin_ap3, ALU.mult)
    tt(c, eng, out_t, o1, x_t, x1, cos_t, cos_ap3, ALU.mult)
    tt(c, eng, out_t, o2, x_t, x2, cos_t, cos_ap3, ALU.mult)
    tt(c, eng, out_t, o1, out_t, o1, tmp_t, t1, ALU.subtract)
    tt(c, eng, out_t, o2, out_t, o2, tmp_t, t2, ALU.add)


def norm_transpose(c, st, x_dram_ap, g1_t, ident_t, xt_rot, ub, uT, pT_rot, ssq, rms, rstd, junk):
    xt = xt_rot.get()
    c.dma("sp", lambda e: e.dma_start(out=xt[:], in_=x_dram_ap), writes=[xt])
    for kb in range(4):
        act(c, junk, junk[:], xt, xt[:, kb, :], AF.Square, extra_w=[ssq], accum_out=ssq[:, kb:kb + 1])
    import os
    NT = int(os.environ.get("NT", "9"))
    rstd_from_ss(c, ssq, ssq[:, 0:4], D, rms, rms[:, 0:4], rstd, rstd[:, 0:4])
    for kb in range(4):
        if NT >= 1:
            stt(c, ub, ub[:, kb, :], xt, xt[:, kb, :], rstd[:, kb:kb + 1], g1_t, g1_t[:], ALU.mult, ALU.mult, extra_r=[rstd])
        pT = pT_rot.get()
        if NT >= 2:
            for k in range(8):
                tr(c, pT, pT[:, k, :], ub, ub[:, kb, k * 128:(k + 1) * 128], ident_t, ident_t[:])
        if NT >= 3:
            cp(c, "dve", uT, uT[:, :, kb * 128:(kb + 1) * 128], pT, pT[:])
    return xt


def build(dbg=None):
    nc = bass.Bass("TRN2", target_bir_lowering=False)
    dbg = dbg or {}
    last_phase = dbg.get("last_phase", 9)

    def din(name, shape, dt=F32):
        return T(nc.dram_tensor(name, list(shape), dt, kind="ExternalInput"), name)

    def dscr(name, shape, dt):
        kind = "ExternalOutput" if name in dbg.get("dump", ()) else "Internal"
        return T(nc.dram_tensor(name, list(shape), dt, kind=kind), name)

    xs = din("xs", [S, D])
    xo = din("xo", [NOWN * UT, D])
    pos_s = din("pos_s", [128, 64], I32)
    pos_o = din("pos_o", [128, 32], I32)
    invf = din("invf", [128, 32])
    ident_d = din("ident", [128, 128])
    g1 = din("g1", [128, D])
    w_in = din("w_in", [D, 4672])
    convw = din("convw", [128, 8, 4])
    convb = din("convb", [128, 8])
    lru_wa = din("lru_wa", [8, 128, 128])
    lru_wx = din("lru_wx", [8, 128, 128])
    lru_ba = din("lru_ba", [128, 8])
    lru_bx = din("lru_bx", [128, 8])
    lru_lam = din("lru_lam", [128, 8])
    w_rnn_o = din("w_rnn_o", [D, D])
    gq = din("gq", [128, 256])
    w_uq = din("w_uq", [256, 1536])
    gkv = din("gkv", [128, 256])
    w_ukv = din("w_ukv", [256, 2048])
    gqk_q = din("gqk_q", [128, 192])
    gqk_k_pe = din("gqk_k_pe", [128, 64])
    gqk_k_col = din("gqk_k_col", [128, 1])
    w_mla_o = din("w_mla_o", [D, D])
    w_out = din("w_out", [D, D])
    g2 = din("g2", [128, D])
    wr = din("wr", [D, 72])
    br = din("br", [128, 72])
    w1r = din("w1r", [64 * 128, 2048])
    w3r = din("w3r", [64 * 128, 2048])
    w2r = din("w2r", [64 * 128, 2048])
    masks = din("masks", [NOWN, 2, 128, 4 * 512], BF16)
    hidx = din("hidx", [128, 64], I32)
    pidx = din("pidx", [128, 1])
    ustrict = din("ustrict", [128, 128])
    thr = din("thr", [128, 128])
    out = T(nc.dram_tensor("out", [NOWN * UT, D], F32, kind="ExternalOutput"), "out")

    KT_d = dscr("KT_d", [H, 128, S], BF16)
    V_d = dscr("V_d", [H, NU, 128, 512], BF16)
    hT_d = dscr("hT_d", [NU * 1024, 512], BF16)
    sA_d = dscr("sA_d", [NOWN, 128, 8 * 512], BF16)
    OT_d = dscr("OT_d", [NOWN, 128, 8 * 512], BF16)
    h_d = dscr("h_d", [NOWN * UT, D], F32)
    u2_d = dscr("u2_d", [NOWN * UT, D], BF16)
    xpad_d = dscr("xpad_d", [MPAD, D], BF16)
    ypad_d = dscr("ypad_d", [MPAD, D], F32)
    wcat_d = dscr("wcat_d", [64 * 128, 6144], BF16)
    dbg_d = dscr("dbg_d", [128, 8192], F32)
    w_in_b = dscr("w_in_b", [D, 4672], BF16)
    w_rnn_o_b = dscr("w_rnn_o_b", [D, D], BF16)
    w_mla_o_b = dscr("w_mla_o_b", [D, D], BF16)
    w_out_b = dscr("w_out_b", [D, D], BF16)
    w_uq_b = dscr("w_uq_b", [256, 1536], BF16)
    wr_b = dscr("wr_b", [D, 72], BF16)

    sk_dump = dscr("sk_dump", [128, 512], F32)
    with ExitStack() as gst:
        c = Ctx(nc, gst, None)
        ident = c.sb("identb", [128, 128], BF16)
        identf = c.sb("identf", [128, 128], F32)
        KP = c.sb("KP", [64, S], BF16)
        SK = c.sb("SK", [128, 64, 8], F32)
        c.dma("pool", lambda e: e.dma_start(out=ident[:], in_=ident_d[:, :]), writes=[ident])
        c.dma("sp", lambda e: e.dma_start(out=identf[:], in_=ident_d[:, :]), writes=[identf])

        block = gst.enter_context(nc.Block())
        c.block = block
        if False:
            for (src, dst) in ((w1r, w1b_d), (w3r, w3b_d), (w2r, w2b_d)):
                for i in range(16):
                    c.dma("pool", lambda e, src=src, dst=dst, i=i: e.dma_start(
                        out=dst[i * 512:(i + 1) * 512, :], in_=src[i * 512:(i + 1) * 512, :]),
                        reads=[src], writes=[dst])

        if last_phase >= 1:
            with ExitStack() as st:
                phase1(c, st, locals())
                c.barrier()
                c.flush()
        S1all = c.sb("S1all", [128, 32, 64], F32)
        S2all = c.sb("S2all", [128, 32, 64], F32)
        GT = c.sb("GT", [128, 32, 2], F32)
        G_ = dict(locals())
        for (pn, fn) in ((2, phase2a), (3, phase2b), (4, phase2c), (5, phase3)):
            if last_phase >= pn:
                with ExitStack() as st:
                    fn(c, st, G_)
                    c.barrier()
                    c.flush()
        c.wait_all("sp", [out, KT_d, V_d, hT_d, dbg_d, sk_dump])
        c.flush()
    return nc


def phase1(c, st, G):
    xs, w_in, ident, KP, SK = G["xs"], G["w_in"], G["ident"], G["KP"], G["SK"]
    KT_d, V_d, hT_d = G["KT_d"], G["V_d"], G["hT_d"]
    Wxr = c.sb("Wxr", [128, 8, 1024], BF16, st)
    Wkv = c.sb("Wkv", [128, 8, 320], BF16, st)
    Wa = c.sb("Wa", [128, 8, 128], BF16, st)
    Wx = c.sb("Wx", [128, 8, 128], BF16, st)
    Wukv = c.sb("Wukv", [128, 2, 2048], BF16, st)
    w_in_v = G["w_in"].t[:, :].rearrange("(k p) n -> p k n", p=128)
    for k in range(8):
        c.dma("pool", lambda e, k=k: e.dma_start(out=Wxr[:, k, :], in_=w_in_v[:, k, 0:1024]), writes=[Wxr])
    c.dma("pool", lambda e: e.dma_start(out=Wkv[:], in_=w_in_v[:, :, 2304:2624]), writes=[Wkv])
    c.dma("pool", lambda e: e.dma_start(out=Wa[:], in_=G["lru_wa"].t[:, :, :].rearrange("n k j -> k n j")), writes=[Wa])
    c.dma("pool", lambda e: e.dma_start(out=Wx[:], in_=G["lru_wx"].t[:, :, :].rearrange("n k j -> k n j")), writes=[Wx])
    for kc in range(2):
        c.dma("pool", lambda e, kc=kc: e.dma_start(out=Wukv[:, kc, :], in_=G["w_ukv"].t[kc * 128:(kc + 1) * 128, :]), writes=[Wukv])
    def small(name, src, shape, dt=F32):
        t = c.sb(name, shape, dt, st)
        c.dma("sp", lambda e: e.dma_start(out=t[:], in_=src.t[tuple(slice(None) for _ in shape)]), writes=[t])
        return t
    g1 = small("g1s", G["g1"], [128, D])
    cw = small("cw", G["convw"], [128, 8, 4])
    cb = small("cb", G["convb"], [128, 8])
    ba = small("ba", G["lru_ba"], [128, 8])
    bx = small("bx", G["lru_bx"], [128, 8])
    lam = small("lam", G["lru_lam"], [128, 8])
    gkv = small("gkvs", G["gkv"], [128, 256])
    gkpe = small("gkpe", G["gqk_k_pe"], [128, 64])
    gkcol = small("gkcol", G["gqk_k_col"], [128, 1])
    invf = small("invfs", G["invf"], [128, 32])
    posi = small("posi", G["pos_s"], [128, 64], I32)
    posf = c.sb("posf", [128, 64], F32, st)
    cp(c, "dve", posf, posf[:], posi, posi[:])
    cosT = c.sb("cosT", [128, 64, 32], F32, st)
    sinT = c.sb("sinT", [128, 64, 32], F32, st)
    with ExitStack() as st2:
        trig_tables(c, st2, posf, 64, invf, cosT, sinT)
        c.barrier()
        c.flush()
    cl = c.sb("cl", [128, 8], F32, st)
    act(c, cl, cl[:], lam, lam[:], AF.Exp, scale=-1.0)
    ts(c, "dve", cl, cl[:], cl, cl[:], 1.0, None, ALU.add)
    act(c, cl, cl[:], cl, cl[:], AF.Ln)
    ts(c, "dve", cl, cl[:], cl, cl[:], -8.0, None, ALU.mult)

    xt_rot = Rot(c, "xt", [128, 4, D], F32, 1, st)
    ub = c.sb("ub", [128, 4, D], BF16, st)
    uT = c.sb("uT", [128, 8, UT], BF16, st)
    junk = c.sb("junk", [128, D], F32, st)
    ssq = c.sb("ssq", [128, 4], F32, st)
    rms = c.sb("rms", [128, 4], F32, st)
    rstd = c.sb("rstd", [128, 4], F32, st)
    pT_rot = Rot(c, "pT", [128, 8, 128], BF16, 2, st, psum=True)
    KTs_rot = Rot(c, "KTs", [128, 8, UT], BF16, 1, st)
    Vs_rot = Rot(c, "Vs", [128, 8, 128], BF16, 2, st)
    sv = c.sb("sv", [128, 16], F32, st)
    SS0 = c.sb("SS0", [128, 8], F32, st)
    ckvg = c.sb("ckvg", [128, 256], BF16, st)
    ckvT = c.sb("ckvT", [128, 2, 128], BF16, st)
    kpg = c.sb("kpg", [128, 1, 64], F32, st)
    kpr = c.sb("kpr", [128, 1, 64], F32, st)
    kpt = c.sb("kpt", [128, 1, 64], F32, st)
    kpb = c.sb("kpb", [128, 64], BF16, st)
    k0b = c.sb("k0b", [128, 8, 128], BF16, st)

    hT_v = hT_d.t[:, :].rearrange("(u c p) t -> u p c t", c=8, p=128)
    LT = []
    for ln in range(2):
        LT.append(dict(
            xc=c.sb("xcL", [128, UT], F32, st), xcb=c.sb("xcbL", [128, UT], BF16, st), rr=c.sb("rrL", [128, UT], F32, st),
            ii=c.sb("iiL", [128, UT], F32, st), aa=c.sb("aaL", [128, UT], F32, st), mm_=c.sb("mmL", [128, UT], F32, st),
            bi=c.sb("biL", [128, UT], F32, st), hf=c.sb("hfL", [128, UT], F32, st),
            p0=c.ps("pL0", [128, 512], F32, st), p1=c.ps("pL1", [128, 512], F32, st)))
    xrs = [c.sb("xrc", [128, UT + 3], F32, st) for _ in range(8)]
    carries = [c.sb("carryc", [128, 1], F32, st) for _ in range(8)]
    hTbs = [c.sb("hTbc", [128, UT], BF16, st) for _ in range(8)]
    for t_ in xrs + carries:
        c.op("dve", lambda e, t_=t_: e.memset(t_[:], 0.0), writes=[t_])
    pKV = [c.ps("pKV0", [128, 512], F32, st), c.ps("pKV1", [128, 512], F32, st)]
    junk2 = c.sb("junk2", [128, 320], F32, st)

    def rnn_chunk(ch, u, L):
        xc, xcb, rr, ii, aa, mm_, bi, hf = L["xc"], L["xcb"], L["rr"], L["ii"], L["aa"], L["mm_"], L["bi"], L["hf"]
        pa, pb = L["p0"], L["p1"]
        xr, carry, hTb = xrs[ch], carries[ch], hTbs[ch]
        for k in range(8):
            mm(c, pa, pa[:], Wxr, Wxr[:, k, ch * 128:(ch + 1) * 128], uT, uT[:, k, :], k == 0, k == 7)
        if u > 0:
            cp(c, "pool", xr, xr[:, 0:3], xr, xr[:, UT:UT + 3])
        yield
        cp(c, "act", xr, xr[:, 3:UT + 3], pa, pa[:])
        yield
        ts(c, "dve", xc, xc[:], xr, xr[:, 0:UT], cw[:, ch, 0:1], cb[:, ch:ch + 1], ALU.mult, ALU.add, extra_r=[cw, cb])
        for k in range(1, 4):
            stt(c, xc, xc[:], xr, xr[:, k:k + UT], cw[:, ch, k:k + 1], xc, xc[:], ALU.mult, ALU.add, extra_r=[cw])
        yield
        cp(c, "pool", xcb, xcb[:], xc, xc[:])
        yield
        mm(c, pa, pa[:], Wa, Wa[:, ch, :], xcb, xcb[:], True, True)
        mm(c, pb, pb[:], Wx, Wx[:, ch, :], xcb, xcb[:], True, True)
        yield
        act(c, rr, rr[:], pa, pa[:], AF.Sigmoid, extra_r=[ba], bias=ba[:, ch:ch + 1])
        act(c, ii, ii[:], pb, pb[:], AF.Sigmoid, extra_r=[bx], bias=bx[:, ch:ch + 1])
        yield
        act(c, aa, aa[:], rr, rr[:], AF.Exp, extra_r=[cl], scale=cl[:, ch:ch + 1])
        tt(c, "dve", bi, bi[:], xc, xc[:], ii, ii[:], ALU.mult)
        yield
        tt(c, "pool", mm_, mm_[:], aa, aa[:], aa, aa[:], ALU.mult)
        ts(c, "pool", mm_, mm_[:], mm_, mm_[:], -1.0, 1.0, ALU.mult, ALU.add)
        yield
        act(c, mm_, mm_[:], mm_, mm_[:], AF.Sqrt)
        yield
        tt(c, "dve", bi, bi[:], bi, bi[:], mm_, mm_[:], ALU.mult)
        c.op("dve", lambda e: e.tensor_tensor_scan(out=hf[:], data0=aa[:], data1=bi[:], initial=carry[:, 0:1],
                                                   op0=ALU.mult, op1=ALU.add), reads=[aa, bi, carry], writes=[hf])
        cp(c, "dve", carry, carry[:, 0:1], hf, hf[:, UT - 1:UT])
        yield
        cp(c, "act", hTb, hTb[:], hf, hf[:])
        c.dma("sp", lambda e: e.dma_start(out=hT_v[u][:, ch, :], in_=hTb[:]), reads=[hTb], writes=[hT_d])
        yield

    def kv_block(kb, u, KTs):
        blk = u * 4 + kb
        pc = pKV[0]
        for k in range(8):
            mm(c, pc, pc[:, 0:320], uT, uT[:, k, kb * 128:(kb + 1) * 128], Wkv, Wkv[:, k, :], k == 0, k == 7)
        yield
        act(c, junk2, junk2[:, 0:256], pc, pc[:, 0:256], AF.Square, extra_w=[sv], accum_out=sv[:, 0:1])
        act(c, junk2, junk2[:, 256:320], pc, pc[:, 256:320], AF.Square, extra_w=[sv], accum_out=sv[:, 1:2])
        yield
        rstd_from_ss(c, sv, sv[:, 0:1], 256, sv, sv[:, 2:3], sv, sv[:, 3:4])
        tt(c, "dve", ckvg, ckvg[:], pc, pc[:, 0:256], gkv, gkv[:], ALU.mult)
        tt(c, "dve", kpg, kpg[:, 0, :], pc, pc[:, 256:320], gkpe, gkpe[:], ALU.mult)
        yield
        pT = pT_rot.get()
        for kc in range(2):
            tr(c, pT, pT[:, kc, :], ckvg, ckvg[:, kc * 128:(kc + 1) * 128], ident, ident[:])
        yield
        cp(c, "dve", ckvT, ckvT[:], pT, pT[:, 0:2, :])
        rope(c, "pool", kpr, kpr[:], kpg, kpg[:], cosT, cosT[:, blk:blk + 1, :], sinT, sinT[:, blk:blk + 1, :], kpt, kpt[:], 1)
        yield
        ts(c, "dve", kpb, kpb[:], kpr, kpr[:, 0, :], sv[:, 2:3], None, ALU.mult, extra_r=[sv])
        pT2 = pT_rot.get()
        tr(c, pT2, pT2[0:64, 0, :], kpb, kpb[:], ident, ident[:])
        yield
        cp(c, "act", KP, KP[:, blk * 128:(blk + 1) * 128], pT2, pT2[0:64, 0, :])
        Vs = Vs_rot.get()
        for n in range(4):
            pk = pKV[(n + 1) % 2]
            for kc in range(2):
                mm(c, pk, pk[:], ckvT, ckvT[:, kc, :], Wukv, Wukv[:, kc, n * 512:(n + 1) * 512], kc == 0, kc == 1)
            yield
            for hh in range(2):
                h = n * 2 + hh
                act(c, junk2, junk2[:, 0:128], pk, pk[:, hh * 256:hh * 256 + 128], AF.Square, extra_w=[SS0], accum_out=SS0[:, h:h + 1])
                cp(c, "act", k0b, k0b[:, h, :], pk, pk[:, hh * 256:hh * 256 + 128])
                act(c, Vs, Vs[:, h, :], pk, pk[:, hh * 256 + 128:hh * 256 + 256], AF.Copy, extra_r=[sv], scale=sv[:, 3:4])
            yield
        c.dma("sp", lambda e, Vs=Vs: e.dma_start(
            out=V_d.t[:, u, :, kb * 128:(kb + 1) * 128].rearrange("h p d -> p h d"), in_=Vs[:]), reads=[Vs], writes=[V_d])
        pT3 = pT_rot.get()
        for h in range(8):
            tr(c, pT3, pT3[:, h, :], k0b, k0b[:, h, :], ident, ident[:])
        yield
        for h in range(8):
            act(c, KTs, KTs[:, h, kb * 128:(kb + 1) * 128], pT3, pT3[:, h, :], AF.Copy, extra_r=[gkcol], scale=gkcol[:, 0:1])
        yield
        tt(c, "dve", sv, sv[:, 4:5], sv, sv[:, 3:4], sv, sv[:, 3:4], ALU.mult)
        ts(c, "dve", SS0, SS0[:], SS0, SS0[:], sv[:, 4:5], sv[:, 1:2], ALU.mult, ALU.add, extra_r=[sv])
        ts(c, "dve", SS0, SS0[:], SS0, SS0[:], 1.0 / 192, EPS, ALU.mult, ALU.add)
        yield
        act(c, SS0, SS0[:], SS0, SS0[:], AF.Sqrt)
        yield
        c.op("dve", lambda e: e.reciprocal(out=SS0[:], in_=SS0[:]), reads=[SS0], writes=[SS0])
        ts(c, "dve", SK, SK[:, blk, :], SS0, SS0[:], sv[:, 3:4], QSCALE, ALU.mult, ALU.mult, extra_r=[sv])
        yield

    def chain(gens):
        for g in gens:
            yield from g

    for u in range(G["dbg"].get("nu", NU)):
        xsrc = xs.t[u * UT:(u + 1) * UT, :].rearrange("(kb p) d -> p kb d", p=128)
        norm_transpose(c, st, xsrc, g1, ident, xt_rot, ub, uT, pT_rot, ssq, rms, rstd, junk)
        KTs = KTs_rot.get()
        lanes = [chain([rnn_chunk(ch, u, LT[0]) for ch in (0, 2, 4, 6)]),
                 chain([rnn_chunk(ch, u, LT[1]) for ch in (1, 3, 5, 7)]),
                 chain([kv_block(kb, u, KTs) for kb in range(4)])]
        while lanes:
            for g in list(lanes):
                try:
                    next(g)
                except StopIteration:
                    lanes.remove(g)
        c.dma("sp", lambda e, u=u, KTs=KTs: e.dma_start(
            out=KT_d.t[:, :, u * UT:(u + 1) * UT].rearrange("h p t -> p h t"), in_=KTs[:]), reads=[KTs], writes=[KT_d])
        if u == 0:
            jobs = []
            for r0 in (0, 512):
                for (c0, c1) in ((1024, 2304), (2624, 3648), (3648, 4672)):
                    jobs.append((G["w_in"], G["w_in_b"], r0, r0 + 512, c0, c1))
                for nm in ("w_rnn_o", "w_mla_o", "w_out"):
                    jobs.append((G[nm], G[nm + "_b"], r0, r0 + 512, 0, D))
            jobs.append((G["w_uq"], G["w_uq_b"], 0, 256, 0, 1536))
            jobs.append((G["wr"], G["wr_b"], 0, D, 0, 72))
            for (src, dst, r0, r1, c0, c1) in jobs:
                c.dma("pool", lambda e, src=src, dst=dst, r0=r0, r1=r1, c0=c0, c1=c1: e.dma_start(
                    out=dst.t[r0:r1, c0:c1], in_=src.t[r0:r1, c0:c1]), reads=[src], writes=[dst])
        if u == 0 and G["last_phase"] >= 5:
            for (src, col) in ((G["w1r"], 0), (G["w3r"], 2048), (G["w2r"], 4096)):
                for i in range(16):
                    c.dma("pool", lambda e, src=src, col=col, i=i: e.dma_start(
                        out=G["wcat_d"].t[i * 512:(i + 1) * 512, col:col + 2048], in_=src.t[i * 512:(i + 1) * 512, :]),
                        reads=[src], writes=[G["wcat_d"]])
        if u % 4 == 3:
            c.flush()
    if "dbg_d" in G["dbg"].get("dump", ()):
        dbt = c.sb("dbt", [128, 8192], F32, st)
        c.op("dve", lambda e: e.memset(dbt[:], 0.0), writes=[dbt])
        cp(c, "dve", dbt, dbt[0:64, :], KP, KP[:, :])
        c.dma("sp", lambda e: e.dma_start(out=G["dbg_d"].t[:, :], in_=dbt[:]), reads=[dbt], writes=[G["dbg_d"]])
        sk_d = G["sk_dump"]
        c.dma("sp", lambda e: e.dma_start(out=sk_d.t[:, :], in_=SK[:].rearrange("p a b -> p (a b)")), reads=[SK], writes=[sk_d])


def run_lanes(lanes):
    lanes = list(lanes)
    while lanes:
        for g in list(lanes):
            try:
                next(g)
            except StopIteration:
                lanes.remove(g)


def chain_gens(gens):
    for g in gens:
        yield from g


def load_w(c, st, name, src_ap_fn, nk, ncols, q="pool", src_t=None):
    t = c.sb(name, [128, nk, ncols], BF16, st)
    for k in range(nk):
        c.dma(q, lambda e, k=k: e.dma_start(out=t[:, k, :], in_=src_ap_fn(k)), reads=([src_t] if src_t is not None else []), writes=[t])
    return t


def small_t(c, st, name, src, shape, dt=F32):
    t = c.sb(name, shape, dt, st)
    c.dma("sp", lambda e: e.dma_start(out=t[:], in_=src.t[tuple(slice(None) for _ in shape)]), writes=[t])
    return t


def norm_tiles(c, st):
    return dict(xt_rot=Rot(c, "xt", [128, 4, D], F32, 1, st), ub=c.sb("ub", [128, 4, D], BF16, st),
                uT=c.sb("uT", [128, 8, UT], BF16, st), junk=c.sb("junk", [128, D], F32, st),
                ssq=c.sb("ssq", [128, 4], F32, st), rms=c.sb("rms", [128, 4], F32, st), rstd=c.sb("rstd", [128, 4], F32, st))


def do_norm(c, st, N, xsrc, g1, ident, pT_rot):
    return norm_transpose(c, st, xsrc, g1, ident, N["xt_rot"], N["ub"], N["uT"], pT_rot, N["ssq"], N["rms"], N["rstd"], N["junk"])


def phase2a(c, st, G):
    xo, ident, hT_d, sA_d = G["xo"], G["ident"], G["hT_d"], G["sA_d"]
    w_in_v = G["w_in_b"].t[:, :].rearrange("(k p) n -> p k n", p=128)
    wro_v = G["w_rnn_o_b"].t[:, :].rearrange("(k p) n -> p k n", p=128)
    Wy = load_w(c, st, "Wy", lambda k: w_in_v[:, k, 1024:2048], 8, 1024, "sp", G["w_in_b"])
    Wga = load_w(c, st, "Wga", lambda k: w_in_v[:, k, 2624:3648], 8, 1024, "sp", G["w_in_b"])
    Wro = load_w(c, st, "Wro", lambda k: wro_v[:, k, :], 8, 1024, "sp", G["w_rnn_o_b"])
    g1 = small_t(c, st, "g1s", G["g1"], [128, D])
    hidx = small_t(c, st, "hidx", G["hidx"], [128, 64], I32)
    N = norm_tiles(c, st)
    uT = N["uT"]
    pT_rot = Rot(c, "pT", [128, 8, 128], BF16, 2, st, psum=True)
    pA_rot = Rot(c, "pA", [128, 512], F32, 4, st, psum=True)
    hs = c.sb("hs", [128, 8, UT], BF16, st)
    LA = [dict(ys=c.sb("ys", [128, UT], F32, st), y2=c.sb("y2", [128, UT], F32, st), sg=c.sb("sg", [128, UT], F32, st),
               p0=pA_rot.tiles[2 * ln], p1=pA_rot.tiles[2 * ln + 1]) for ln in range(2)]
    zT = c.sb("zT", [128, 8, UT], BF16, st)
    sAT = c.sb("sAT", [128, 8, UT], BF16, st)
    for i in range(NOWN):
        xsrc = xo.t[i * UT:(i + 1) * UT, :].rearrange("(kb p) d -> p kb d", p=128)
        do_norm(c, st, N, xsrc, g1, ident, pT_rot)
        for ch in range(8):
            c.dma("pool", lambda e, ch=ch, i=i: e.indirect_dma_start(
                out=hs[:, ch, :], out_offset=None, in_=hT_d.t[:, :],
                in_offset=bass.IndirectOffsetOnAxis(ap=hidx[:, i * 8 + ch:i * 8 + ch + 1], axis=0)),
                reads=[hidx, hT_d], writes=[hs])
        def gelu_chunk(ch, L):
            ys, y2, sg, pa = L["ys"], L["y2"], L["sg"], L["p0"]
            for k in range(8):
                mm(c, pa, pa[:], Wy, Wy[:, k, ch * 128:(ch + 1) * 128], uT, uT[:, k, :], k == 0, k == 7)
            yield
            cp(c, "act", ys, ys[:], pa, pa[:])
            yield
            tt(c, "pool", y2, y2[:], ys, ys[:], ys, ys[:], ALU.mult)
            ts(c, "pool", y2, y2[:], y2, y2[:], 0.044715, 1.0, ALU.mult, ALU.add)
            tt(c, "pool", y2, y2[:], y2, y2[:], ys, ys[:], ALU.mult)
            yield
            act(c, sg, sg[:], y2, y2[:], AF.Sigmoid, scale=1.5957691216)
            yield
            tt(c, "dve", sg, sg[:], sg, sg[:], ys, ys[:], ALU.mult)
            tt(c, "dve", zT, zT[:, ch, :], sg, sg[:], hs, hs[:, ch, :], ALU.mult)
            yield

        def a_chunk(co, L):
            sg, pa, pg = L["sg"], L["p0"], L["p1"]
            for k in range(8):
                mm(c, pa, pa[:], Wro, Wro[:, k, co * 128:(co + 1) * 128], zT, zT[:, k, :], k == 0, k == 7)
            yield
            for k in range(8):
                mm(c, pg, pg[:], Wga, Wga[:, k, co * 128:(co + 1) * 128], uT, uT[:, k, :], k == 0, k == 7)
            yield
            act(c, sg, sg[:], pg, pg[:], AF.Sigmoid)
            yield
            tt(c, "dve", sAT, sAT[:, co, :], pa, pa[:], sg, sg[:], ALU.mult)
            yield

        run_lanes([chain_gens([gelu_chunk(ch, LA[ln]) for ch in range(ln, 8, 2)]) for ln in range(2)])
        run_lanes([chain_gens([a_chunk(co, LA[ln]) for co in range(ln, 8, 2)]) for ln in range(2)])
        c.dma("sp", lambda e, i=i: e.dma_start(out=sA_d.t[i], in_=sAT[:].rearrange("p a b -> p (a b)")), reads=[sAT], writes=[sA_d])
        c.flush()


def phase2b(c, st, G):
    xo, ident, KP, SK = G["xo"], G["ident"], G["KP"], G["SK"]
    KT_d, V_d, OT_d, masks = G["KT_d"], G["V_d"], G["OT_d"], G["masks"]
    w_in_v = G["w_in_b"].t[:, :].rearrange("(k p) n -> p k n", p=128)
    Wcq = load_w(c, st, "Wcq", lambda k: w_in_v[:, k, 2048:2304], 8, 256, "sp", G["w_in_b"])
    Wuq = load_w(c, st, "Wuq", lambda k: G["w_uq_b"].t[k * 128:(k + 1) * 128, :], 2, 1536, "sp", G["w_uq_b"])
    g1 = small_t(c, st, "g1s", G["g1"], [128, D])
    gq = small_t(c, st, "gqs", G["gq"], [128, 256])
    gqk = small_t(c, st, "gqk", G["gqk_q"], [128, 192])
    invf = small_t(c, st, "invfs", G["invf"], [128, 32])
    posi = small_t(c, st, "posi", G["pos_o"], [128, 32], I32)
    posf = c.sb("posf", [128, 32], F32, st)
    cp(c, "dve", posf, posf[:], posi, posi[:])
    cosO = c.sb("cosO", [128, 32, 32], F32, st)
    sinO = c.sb("sinO", [128, 32, 32], F32, st)
    with ExitStack() as st2:
        trig_tables(c, st2, posf, 32, invf, cosO, sinO)
        c.barrier()
        c.flush()
    N = norm_tiles(c, st)
    uT = N["uT"]
    junk = N["junk"]
    pT_rot = Rot(c, "pT", [128, 8, 128], BF16, 1, st, psum=True)
    pS_rot = Rot(c, "pS", [128, 512], F32, 3, st, psum=True)
    pO = [c.ps("pO%d" % q, [128, 512], F32, st) for q in range(4)]
    onesb = c.sb("onesb2", [128, 128], BF16, st)
    c.op("dve", lambda e: e.memset(onesb[:], 1.0), writes=[onesb])
    rinv = c.sb("rinv", [128, 512], F32, st)
    sv = c.sb("sv", [128, 16], F32, st)
    SSQ = c.sb("SSQ", [128, 8], F32, st)
    FQ = c.sb("FQ", [128, 8], F32, st)
    cqg = c.sb("cqg", [128, 256], BF16, st)
    cqT = c.sb("cqT", [128, 2, 128], BF16, st)
    q0s = c.sb("q0s", [128, 8, 192], F32, st)
    qr = c.sb("qr", [128, 8, 64], F32, st)
    qtmp = c.sb("qtmp", [128, 8, 64], F32, st)
    qbn = c.sb("qbn", [128, 8, 128], BF16, st)
    qbp = c.sb("qbp", [128, 8, 64], BF16, st)
    QT = c.sb("QT", [128, 8, UT], BF16, st)
    QP = c.sb("QP", [64, 8, UT], BF16, st)
    msk = [c.sb("msk%d" % w, [128, 4, 512], BF16, st) for w in range(2)]
    KT_rot = Rot(c, "KTt", [128, 512], BF16, 3, st)
    V_rot = Rot(c, "Vt", [128, 4, 130], BF16, 3, st)
    for vt in V_rot.tiles:
        c.op("dve", lambda e, vt=vt: e.memset(vt[:], 1.0), writes=[vt])
    PT_rot = Rot(c, "PT", [128, 512], BF16, 3, st)
    rs = c.sb("rs", [128, 4], F32, st)
    ob = c.sb("ob", [128, 128], BF16, st)
    OT = c.sb("OT", [128, 8, UT], BF16, st)
    q0f = q0s[:].rearrange("p a b -> p (a b)")
    for i in range(NOWN):
        xsrc = xo.t[i * UT:(i + 1) * UT, :].rearrange("(kb p) d -> p kb d", p=128)
        do_norm(c, st, N, xsrc, g1, ident, pT_rot)
        for w in range(2):
            c.dma("sp", lambda e, w=w, i=i: e.dma_start(out=msk[w][:].rearrange("p a b -> p (a b)"), in_=masks.t[i, w]), writes=[msk[w]])
        for kb in range(4):
            blk = i * 4 + kb
            pc = pO[0]
            for k in range(8):
                mm(c, pc, pc[:, 0:256], uT, uT[:, k, kb * 128:(kb + 1) * 128], Wcq, Wcq[:, k, :], k == 0, k == 7)
            act(c, junk, junk[:, 0:256], pc, pc[:, 0:256], AF.Square, extra_w=[sv], accum_out=sv[:, 0:1])
            rstd_from_ss(c, sv, sv[:, 0:1], 256, sv, sv[:, 2:3], sv, sv[:, 3:4])
            tt(c, "dve", cqg, cqg[:], pc, pc[:, 0:256], gq, gq[:], ALU.mult)
            pT = pT_rot.get()
            for kc in range(2):
                tr(c, pT, pT[:, kc, :], cqg, cqg[:, kc * 128:(kc + 1) * 128], ident, ident[:])
            cp(c, "dve", cqT, cqT[:], pT, pT[:, 0:2, :])
            for n in range(3):
                pq = pO[1 + n]
                for kc in range(2):
                    mm(c, pq, pq[:], cqT, cqT[:, kc, :], Wuq, Wuq[:, kc, n * 512:(n + 1) * 512], kc == 0, kc == 1)
                cp(c, "act", q0s, q0f[:, n * 512:(n + 1) * 512], pq, pq[:])
            for h in range(8):
                act(c, junk, junk[:, 0:192], q0s, q0s[:, h, :], AF.Square, extra_w=[SSQ], accum_out=SSQ[:, h:h + 1])
            tt(c, "dve", sv, sv[:, 4:5], sv, sv[:, 3:4], sv, sv[:, 3:4], ALU.mult)
            ts(c, "dve", FQ, FQ[:], SSQ, SSQ[:], sv[:, 4:5], 1.0 / 192, ALU.mult, ALU.mult, extra_r=[sv])
            ts(c, "dve", FQ, FQ[:], FQ, FQ[:], EPS, None, ALU.add)
            act(c, FQ, FQ[:], FQ, FQ[:], AF.Sqrt)
            c.op("dve", lambda e: e.reciprocal(out=FQ[:], in_=FQ[:]), reads=[FQ], writes=[FQ])
            ts(c, "dve", FQ, FQ[:], FQ, FQ[:], sv[:, 3:4], None, ALU.mult, extra_r=[sv])
            tt(c, "dve", q0s, q0s[:], q0s, q0s[:], FQ, FQ[:].unsqueeze(2).to_broadcast([128, 8, 192]), ALU.mult)
            tt(c, "pool", q0s, q0s[:], q0s, q0s[:], gqk, gqk[:].unsqueeze(1).to_broadcast([128, 8, 192]), ALU.mult)
            rope(c, "pool", qr, qr[:], q0s, q0s[:, :, 128:192], cosO, cosO[:, blk:blk + 1, :].to_broadcast([128, 8, 32]),
                 sinO, sinO[:, blk:blk + 1, :].to_broadcast([128, 8, 32]), qtmp, qtmp[:], 8)
            cp(c, "dve", qbn, qbn[:], q0s, q0s[:, :, 0:128])
            cp(c, "dve", qbp, qbp[:], qr, qr[:])
            pT = pT_rot.get()
            for h in range(8):
                tr(c, pT, pT[:, h, :], qbn, qbn[:, h, :], ident, ident[:])
            cp(c, "dve", QT, QT[:, :, kb * 128:(kb + 1) * 128], pT, pT[:])
            pT = pT_rot.get()
            for h in range(8):
                tr(c, pT, pT[0:64, h, :], qbp, qbp[:, h, :], ident, ident[:])
            cp(c, "dve", QP, QP[:, :, kb * 128:(kb + 1) * 128], pT, pT[0:64, :, :])
        E = EXT[i]
        for h in range(8):
            tiles = [(ku, kb) for ku in range(E) for kb in range(4)]
            kv = {}

            def load_kv(ku, h=h):
                KTt = KT_rot.get()
                Vt = V_rot.get()
                c.dma("sp", lambda e, KTt=KTt: e.dma_start(out=KTt[:], in_=KT_d.t[h, :, ku * 512:(ku + 1) * 512]),
                      reads=[KT_d], writes=[KTt])
                c.dma("sp", lambda e, Vt=Vt: e.dma_start(
                    out=Vt[:, :, 0:128], in_=V_d.t[h, ku].rearrange("p (kb d) -> p kb d", d=128)), reads=[V_d], writes=[Vt])
                kv[ku] = (KTt, Vt)

            def qk(t, h=h):
                ku, kb = tiles[t]
                if kb == 0 and ku + 1 < E:
                    load_kv(ku + 1)
                KTt = kv[ku][0]
                kblk = ku * 4 + kb
                pS = pS_rot.get()
                mm(c, pS, pS[:], KTt, KTt[:, kb * 128:(kb + 1) * 128], QT, QT[:, h, :], True, False)
                mm(c, pS, pS[:], KP, KP[:, kblk * 128:(kblk + 1) * 128], QP, QP[:, h, :], False, True)
                return pS

            load_kv(0)
            pSq = [qk(0)]
            if len(tiles) > 1:
                pSq.append(qk(1))
            for t in range(len(tiles)):
                ku, kb = tiles[t]
                kblk = ku * 4 + kb
                pS = pSq.pop(0)
                if t + 2 < len(tiles):
                    pSq.append(qk(t + 2))
                Vt = kv[ku][1]
                PT = PT_rot.get()
                act(c, PT, PT[:], pS, pS[:], AF.Exp, extra_r=[SK], scale=SK[:, kblk, h:h + 1])
                if ku >= E - 2:
                    w = ku - (E - 2)
                    tt(c, "pool", PT, PT[:], PT, PT[:], msk[w], msk[w][:, kb, :], ALU.mult)
                pOT, pRS = pO[2 * (h % 2)], pO[2 * (h % 2) + 1]
                mm(c, pOT, pOT[:], Vt, Vt[:, kb, 0:128], PT, PT[:], t == 0, t == len(tiles) - 1)
                mm(c, pRS, pRS[:], onesb, onesb[:], PT, PT[:], t == 0, t == len(tiles) - 1)
            pOT, pRS = pO[2 * (h % 2)], pO[2 * (h % 2) + 1]
            c.op("dve", lambda e, pRS=pRS: e.reciprocal(out=rinv[:], in_=pRS[:]), reads=[pRS], writes=[rinv])
            tt(c, "dve", OT, OT[:, h, :], pOT, pOT[:], rinv, rinv[:], ALU.mult)
            c.flush()
        c.dma("sp", lambda e, i=i: e.dma_start(out=OT_d.t[i], in_=OT[:].rearrange("p a b -> p (a b)")), reads=[OT], writes=[OT_d])


def phase2c(c, st, G):
    xo, ident, OT_d, sA_d, h_d, u2_d = G["xo"], G["ident"], G["OT_d"], G["sA_d"], G["h_d"], G["u2_d"]
    S1all, S2all, GT = G["S1all"], G["S2all"], G["GT"]
    w_in_v = G["w_in_b"].t[:, :].rearrange("(k p) n -> p k n", p=128)
    wmo_v = G["w_mla_o_b"].t[:, :].rearrange("(k p) n -> p k n", p=128)
    wo_v = G["w_out_b"].t[:, :].rearrange("(k p) n -> p k n", p=128)
    wr_v = G["wr_b"].t[:, :].rearrange("(k p) n -> p k n", p=128)
    Wgb = load_w(c, st, "Wgb", lambda k: w_in_v[:, k, 3648:4672], 8, 1024, "sp", G["w_in_b"])
    Wmo = load_w(c, st, "Wmo", lambda k: wmo_v[:, k, :], 8, 1024, "sp", G["w_mla_o_b"])
    Wo = load_w(c, st, "Wo", lambda k: wo_v[:, k, :], 8, 1024, "sp", G["w_out_b"])
    Wr = load_w(c, st, "Wr", lambda k: wr_v[:, k, :], 8, 72, "sp", G["wr_b"])
    g1 = small_t(c, st, "g1s", G["g1"], [128, D])
    g2 = small_t(c, st, "g2s", G["g2"], [128, D])
    brt = small_t(c, st, "brt", G["br"], [128, 72])
    N = norm_tiles(c, st)
    uT = N["uT"]
    junk = N["junk"]
    pT_rot = Rot(c, "pT", [128, 8, 128], BF16, 2, st, psum=True)
    pA_rot = Rot(c, "pA", [128, 512], F32, 4, st, psum=True)
    OT = c.sb("OT", [128, 8, UT], BF16, st)
    sAT = c.sb("sAT", [128, 8, UT], BF16, st)
    LC = [dict(sg=c.sb("sg", [128, UT], F32, st), p0=pA_rot.tiles[2 * ln], p1=pA_rot.tiles[2 * ln + 1]) for ln in range(2)]
    mT = c.sb("mT", [128, 8, UT], BF16, st)
    hrow = c.sb("hrow", [128, D], F32, st)
    u2b = c.sb("u2b", [128, D], BF16, st)
    u2T = c.sb("u2T", [128, 8, UT], BF16, st)
    sv = c.sb("sv", [128, 16], F32, st)
    lg = c.sb("lg", [128, 72], F32, st)
    m8 = c.sb("m8", [128, 8], F32, st)
    oh = c.sb("oh", [128, 8], F32, st)
    em = c.sb("em", [128, 8, 8], F32, st)
    s1 = c.sb("s1", [128, 64], F32, st)
    s2 = c.sb("s2", [128, 64], F32, st)
    emf = em[:].rearrange("p a b -> p (a b)")
    for i in range(NOWN):
        xsrc = xo.t[i * UT:(i + 1) * UT, :].rearrange("(kb p) d -> p kb d", p=128)
        xt = do_norm(c, st, N, xsrc, g1, ident, pT_rot)
        c.dma("sp", lambda e, i=i: e.dma_start(out=OT[:].rearrange("p a b -> p (a b)"), in_=OT_d.t[i]), reads=[OT_d], writes=[OT])
        c.dma("sp", lambda e, i=i: e.dma_start(out=sAT[:].rearrange("p a b -> p (a b)"), in_=sA_d.t[i]), reads=[sA_d], writes=[sAT])
        def merge_chunk(co, L):
            sg, pb, pg = L["sg"], L["p0"], L["p1"]
            for k in range(8):
                mm(c, pb, pb[:], Wmo, Wmo[:, k, co * 128:(co + 1) * 128], OT, OT[:, k, :], k == 0, k == 7)
            yield
            for k in range(8):
                mm(c, pg, pg[:], Wgb, Wgb[:, k, co * 128:(co + 1) * 128], uT, uT[:, k, :], k == 0, k == 7)
            yield
            act(c, sg, sg[:], pg, pg[:], AF.Sigmoid)
            yield
            tt(c, "dve", sg, sg[:], pb, pb[:], sg, sg[:], ALU.mult)
            tt(c, "dve", mT, mT[:, co, :], sg, sg[:], sAT, sAT[:, co, :], ALU.add)
            yield

        run_lanes([chain_gens([merge_chunk(co, LC[ln]) for co in range(ln, 8, 2)]) for ln in range(2)])
        for kb in range(4):
            blk = i * 4 + kb
            for n in range(2):
                ph = pA_rot.get()
                for k in range(8):
                    mm(c, ph, ph[:], mT, mT[:, k, kb * 128:(kb + 1) * 128], Wo, Wo[:, k, n * 512:(n + 1) * 512], k == 0, k == 7)
                tt(c, "dve", hrow, hrow[:, n * 512:(n + 1) * 512], ph, ph[:], xt, xt[:, kb, n * 512:(n + 1) * 512], ALU.add)
            c.dma("sp", lambda e, blk=blk: e.dma_start(out=h_d.t[blk * 128:(blk + 1) * 128, :], in_=hrow[:]), reads=[hrow], writes=[h_d])
            act(c, junk, junk[:], hrow, hrow[:], AF.Square, extra_w=[sv], accum_out=sv[:, 0:1])
            rstd_from_ss(c, sv, sv[:, 0:1], D, sv, sv[:, 2:3], sv, sv[:, 3:4])
            stt(c, u2b, u2b[:], hrow, hrow[:], sv[:, 3:4], g2, g2[:], ALU.mult, ALU.mult, extra_r=[sv])
            c.dma("sp", lambda e, blk=blk: e.dma_start(out=u2_d.t[blk * 128:(blk + 1) * 128, :], in_=u2b[:]), reads=[u2b], writes=[u2_d])
            pT = pT_rot.get()
            for k in range(8):
                tr(c, pT, pT[:, k, :], u2b, u2b[:, k * 128:(k + 1) * 128], ident, ident[:])
            cp(c, "dve", u2T, u2T[:, :, kb * 128:(kb + 1) * 128], pT, pT[:])
            pl = pA_rot.get()
            for k in range(8):
                mm(c, pl, pl[:, 0:72], u2T, u2T[:, k, kb * 128:(kb + 1) * 128], Wr, Wr[:, k, :], k == 0, k == 7)
            tt(c, "dve", lg, lg[:], pl, pl[:, 0:72], brt, brt[:], ALU.add)
            c.op("dve", lambda e: e.max(out=m8[:], in_=lg[:, 0:8]), reads=[lg], writes=[m8])
            ts(c, "dve", oh, oh[:], lg, lg[:, 0:8], m8[:, 0:1], None, ALU.is_ge, extra_r=[m8])
            ts(c, "dve", sv, sv[:, 5:6], m8, m8[:, 0:1], -1.0, None, ALU.mult)
            act(c, junk, junk[:, 0:8], lg, lg[:, 0:8], AF.Exp, extra_r=[sv], extra_w=[sv], bias=sv[:, 5:6], accum_out=sv[:, 6:7])
            c.op("dve", lambda e: e.reciprocal(out=sv[:, 7:8], in_=sv[:, 6:7]), reads=[sv], writes=[sv])
            ts(c, "dve", oh, oh[:], oh, oh[:], -1.0, 1e9, ALU.add, ALU.mult)
            tt(c, "dve", em, em[:], lg, lg[:, 8:72].rearrange("p (a b) -> p a b", b=8), oh, oh[:].unsqueeze(2).to_broadcast([128, 8, 8]), ALU.add)
            c.op("dve", lambda e: e.max(out=m8[:], in_=emf), reads=[em], writes=[m8])
            ts(c, "dve", s1, s1[:], em, emf, m8[:, 0:1], None, ALU.is_ge, extra_r=[m8])
            ts(c, "dve", s2, s2[:], em, emf, m8[:, 1:2], None, ALU.is_ge, extra_r=[m8])
            tt(c, "dve", sv, sv[:, 8:9], m8, m8[:, 0:1], m8, m8[:, 1:2], ALU.subtract)
            act(c, sv, sv[:, 9:10], sv, sv[:, 8:9], AF.Sigmoid)
            tt(c, "dve", sv, sv[:, 10:11], sv, sv[:, 9:10], sv, sv[:, 7:8], ALU.mult)
            tt(c, "dve", sv, sv[:, 11:12], sv, sv[:, 7:8], sv, sv[:, 10:11], ALU.subtract)
            tt(c, "dve", sv, sv[:, 12:13], sv, sv[:, 10:11], sv, sv[:, 11:12], ALU.subtract)
            cp(c, "dve", S1all, S1all[:, blk, :], s1, s1[:])
            tt(c, "dve", S2all, S2all[:, blk, :], s2, s2[:], s1, s1[:], ALU.subtract)
            cp(c, "dve", GT, GT[:, blk, 0:2], sv, sv[:, 10:12])
        c.flush()


def phase3(c, st, G):
    ident, h_d, u2_d, out = G["ident"], G["h_d"], G["u2_d"], G["out"]
    S1all, S2all, GT = G["S1all"], G["S2all"], G["GT"]
    xpad_d, ypad_d, w1r, w3r, w2r = G["xpad_d"], G["ypad_d"], G["w1r"], G["w3r"], G["w2r"]
    ustr = c.sb("ustr", [128, 128], BF16, st)
    c.dma("pool", lambda e: e.dma_start(out=ustr[:], in_=G["ustrict"].t[:, :]), writes=[ustr])
    onesb = c.sb("onesb", [128, 128], BF16, st)
    c.op("dve", lambda e: e.memset(onesb[:], 1.0), writes=[onesb])
    thr = small_t(c, st, "thr", G["thr"], [128, 128])
    pidx = small_t(c, st, "pidx", G["pidx"], [128, 1])
    pA_rot = Rot(c, "pA3", [128, 512], F32, 2, st, psum=True)
    A = c.sb("A3", [128, 64], BF16, st)
    Acum = c.sb("Acum", [128, 64], BF16, st)
    c.op("dve", lambda e: e.memset(Acum[:], 0.0), writes=[Acum])
    RK = c.sb("RK", [128, 32, 64], F32, st)
    for b in range(32):
        tt(c, "dve", A, A[:], S1all, S1all[:, b, :], S2all, S2all[:, b, :], ALU.add)
        pr = pA_rot.get()
        mm(c, pr, pr[:, 0:64], ustr, ustr[:], A, A[:], True, False)
        mm(c, pr, pr[:, 0:64], onesb, onesb[:], Acum, Acum[:], False, True)
        cp(c, "act", RK, RK[:, b, :], pr, pr[:, 0:64])
        tt(c, "dve", Acum, Acum[:], Acum, Acum[:], A, A[:], ALU.add)
    pr = pA_rot.get()
    mm(c, pr, pr[:, 0:64], onesb, onesb[:], Acum, Acum[:], True, True)
    tot = c.sb("tot", [128, 64], F32, st)
    f1 = c.sb("f1", [128, 64], F32, st)
    f2 = c.sb("f2", [128, 64], F32, st)
    ki = c.sb("ki3", [128, 64], I32, st)
    cp(c, "act", tot, tot[:], pr, pr[:, 0:64])
    ts(c, "dve", f1, f1[:], tot, tot[:], 127.0, 1.0 / 128, ALU.add, ALU.mult)
    cp(c, "dve", ki, ki[:], f1, f1[:])
    cp(c, "dve", f2, f2[:], ki, ki[:])
    tt(c, "dve", f1, f1[:], f2, f2[:], f1, f1[:], ALU.is_gt)
    tt(c, "dve", f2, f2[:], f2, f2[:], f1, f1[:], ALU.subtract)
    padded = c.sb("padded", [128, 64], F32, st)
    ts(c, "dve", padded, padded[:], f2, f2[:], 128.0, None, ALU.mult)
    pend = c.sb("pend", [128, 64], F32, st)
    pstart = c.sb("pstart", [128, 64], F32, st)
    ones64 = c.sb("ones64", [128, 64], F32, st)
    c.op("dve", lambda e: e.memset(ones64[:], 1.0), writes=[ones64])
    c.op("dve", lambda e: e.tensor_tensor_scan(out=pend[:], data0=ones64[:], data1=padded[:], initial=0.0, op0=ALU.mult, op1=ALU.add),
         reads=[ones64, padded], writes=[pend])
    tt(c, "dve", pstart, pstart[:], pend, pend[:], padded, padded[:], ALU.subtract)
    DST = c.sb("DST", [128, 64], F32, st)
    DSTi = c.sb("DSTi", [128, 64], I32, st)
    junk = c.sb("junk3", [128, 64], F32, st)
    for b in range(32):
        tt(c, "dve", f1, f1[:], RK, RK[:, b, :], pstart, pstart[:], ALU.add)
        stt(c, junk, junk[:], f1, f1[:], 1.0, S1all, S1all[:, b, :], ALU.mult, ALU.mult, extra_w=[DST], accum_out=DST[:, 2 * b:2 * b + 1])
        stt(c, junk, junk[:], f1, f1[:], 1.0, S2all, S2all[:, b, :], ALU.mult, ALU.mult, extra_w=[DST], accum_out=DST[:, 2 * b + 1:2 * b + 2])
    cp(c, "dve", DSTi, DSTi[:], DST, DST[:])
    u2r_rot = Rot(c, "u2r", [128, D], BF16, 2, st)
    for b in range(32):
        u2r = u2r_rot.get()
        c.dma("sp", lambda e, b=b, u2r=u2r: e.dma_start(out=u2r[:], in_=u2_d.t[b * 128:(b + 1) * 128, :]), reads=[u2_d], writes=[u2r])
        for sl in range(2):
            c.dma("pool", lambda e, b=b, sl=sl, u2r=u2r: e.indirect_dma_start(
                out=xpad_d.t[:, :], out_offset=bass.IndirectOffsetOnAxis(ap=DSTi[:, 2 * b + sl:2 * b + sl + 1], axis=0),
                in_=u2r[:], in_offset=None), reads=[u2r, DSTi], writes=[xpad_d])
    cmp = c.sb("cmp3", [128, 128, 64], F32, st)
    tt(c, "dve", cmp, cmp[:], pend, pend[:].unsqueeze(1).to_broadcast([128, 128, 64]),
       thr, thr[:].unsqueeze(2).to_broadcast([128, 128, 64]), ALU.is_le)
    BE = c.sb("BE", [128, 128], F32, st)
    c.op("dve", lambda e: e.tensor_reduce(out=BE[:], in_=cmp[:], axis=mybir.AxisListType.X, op=ALU.add), reads=[cmp], writes=[BE])
    ts(c, "dve", BE, BE[:], BE, BE[:], 63.0, 128.0, ALU.min, ALU.mult)
    ts(c, "dve", BE, BE[:], BE, BE[:], pidx[:, 0:1], None, ALU.add, extra_r=[pidx])
    WIDX = c.sb("WIDX", [128, 128], I32, st)
    cp(c, "dve", WIDX, WIDX[:], BE, BE[:])
    c.flush()
    Wc_rot = Rot(c, "Wcat", [128, 6144], BF16, 3, st)
    wcat_d = G["wcat_d"]
    xr_rot = Rot(c, "xrow", [128, D], BF16, 3, st)
    xT = c.sb("xT3", [128, 8, 128], BF16, st)
    pH1 = Rot(c, "pH1", [128, 512], F32, 1, st, psum=True)
    pH3 = Rot(c, "pH3", [128, 512], F32, 1, st, psum=True)
    pY_rot = Rot(c, "pY", [128, 512], F32, 2, st, psum=True)
    pT_rot = Rot(c, "pT3", [128, 8, 128], BF16, 2, st, psum=True)
    sg = c.sb("sg3", [128, 256], F32, st)
    hg = c.sb("hg3", [128, 256], BF16, st)
    hgT = c.sb("hgT3", [128, 2, 128], BF16, st)
    yrow_rot = Rot(c, "yrow", [128, D], F32, 2, st)
    stg = {}

    def fetch(i):
        Wc, xr = Wc_rot.get(), xr_rot.get()
        c.dma("pool", lambda e, Wc=Wc, i=i: e.indirect_dma_start(
            out=Wc[:], out_offset=None, in_=wcat_d.t[:, :],
            in_offset=bass.IndirectOffsetOnAxis(ap=WIDX[:, i:i + 1], axis=0)), reads=[WIDX, wcat_d], writes=[Wc])
        c.dma("sp", lambda e, xr=xr, i=i: e.dma_start(out=xr[:], in_=xpad_d.t[i * 128:(i + 1) * 128, :]), reads=[xpad_d], writes=[xr])
        stg[i] = (Wc, xr)

    fetch(0)
    fetch(1)
    for i in range(NBLK):
        if i + 2 < NBLK:
            fetch(i + 2)
        Wc, xr = stg.pop(i)
        pT = pT_rot.get()
        for k in range(8):
            tr(c, pT, pT[:, k, :], xr, xr[:, k * 128:(k + 1) * 128], ident, ident[:])
        cp(c, "dve", xT, xT[:], pT, pT[:])
        p1 = pH1.get()
        p3 = pH3.get()
        for k in range(8):
            mm(c, p1, p1[:, 0:256], xT, xT[:, k, :], Wc, Wc[:, k * 256:(k + 1) * 256], k == 0, k == 7)
        for k in range(8):
            mm(c, p3, p3[:, 0:256], xT, xT[:, k, :], Wc, Wc[:, 2048 + k * 256:2048 + (k + 1) * 256], k == 0, k == 7)
        act(c, sg, sg[:], p1, p1[:, 0:256], AF.Sigmoid)
        tt(c, "dve", sg, sg[:], p1, p1[:, 0:256], sg, sg[:], ALU.mult)
        tt(c, "dve", hg, hg[:], p3, p3[:, 0:256], sg, sg[:], ALU.mult)
        pT = pT_rot.get()
        for kc in range(2):
            tr(c, pT, pT[:, kc, :], hg, hg[:, kc * 128:(kc + 1) * 128], ident, ident[:])
        cp(c, "dve", hgT, hgT[:], pT, pT[:, 0:2, :])
        yrow = yrow_rot.get()
        for n in range(2):
            py = pY_rot.get()
            for kc in range(2):
                mm(c, py, py[:], hgT, hgT[:, kc, :], Wc, Wc[:, 4096 + kc * 1024 + n * 512:4096 + kc * 1024 + (n + 1) * 512], kc == 0, kc == 1)
            cp(c, "act" if n == 0 else "dve", yrow, yrow[:, n * 512:(n + 1) * 512], py, py[:])
        c.dma("sp", lambda e, i=i, yrow=yrow: e.dma_start(out=ypad_d.t[i * 128:(i + 1) * 128, :], in_=yrow[:]), reads=[yrow], writes=[ypad_d])
        if i % 16 == 15:
            c.flush()
    y1_rot = Rot(c, "y1", [128, D], F32, 2, st)
    y2_rot = Rot(c, "y2", [128, D], F32, 2, st)
    hr_rot = Rot(c, "hr3", [128, D], F32, 2, st)
    for b in range(32):
        y1, y2, hr = y1_rot.get(), y2_rot.get(), hr_rot.get()
        for (dst, sl) in ((y1, 0), (y2, 1)):
            c.dma("pool", lambda e, dst=dst, sl=sl, b=b: e.indirect_dma_start(
                out=dst[:], out_offset=None, in_=ypad_d.t[:, :],
                in_offset=bass.IndirectOffsetOnAxis(ap=DSTi[:, 2 * b + sl:2 * b + sl + 1], axis=0)), reads=[DSTi, ypad_d], writes=[dst])
        c.dma("sp", lambda e, b=b, hr=hr: e.dma_start(out=hr[:], in_=h_d.t[b * 128:(b + 1) * 128, :]), reads=[h_d], writes=[hr])
        stt(c, hr, hr[:], y1, y1[:], GT[:, b, 0:1], hr, hr[:], ALU.mult, ALU.add, extra_r=[GT])
        stt(c, hr, hr[:], y2, y2[:], GT[:, b, 1:2], hr, hr[:], ALU.mult, ALU.add, extra_r=[GT])
        c.dma("sp", lambda e, b=b, hr=hr: e.dma_start(out=out.t[b * 128:(b + 1) * 128, :], in_=hr[:]), reads=[hr], writes=[out])
    c.flush()


def phase3_dense(c, st, G):
    ident, h_d, u2T_d, Gall, out = G["ident"], G["h_d"], G["u2T_d"], G["Gall"], G["out"]
    w1r, w3r, w2r = G["w1r"], G["w3r"], G["w2r"]
    NB = 16
    yacc = c.sb("yacc", [128, NB, D], F32, st)
    u2T = c.sb("u2Ta", [128, 8, NB * 128], BF16, st)
    W13_rot = Rot(c, "W13", [128, 8, 512], BF16, 2, st)
    W2_rot = Rot(c, "W2", [128, 2, 1024], BF16, 2, st)
    pH_rot = Rot(c, "pH", [128, 512], F32, 2, st, psum=True)
    pY_rot = Rot(c, "pY", [128, 512], F32, 4, st, psum=True)
    pT_rot = Rot(c, "pT", [128, 8, 128], BF16, 2, st, psum=True)
    sg = c.sb("sg3", [128, 256], F32, st)
    hg = c.sb("hg", [128, 256], BF16, st)
    hgT = c.sb("hgT", [128, 2, 128], BF16, st)
    hrow = c.sb("hrow3", [128, D], F32, st)
    for half in range(2):
        for j in range(4):
            c.dma("sp", lambda e, j=j, half=half: e.dma_start(
                out=u2T[:, :, j * 512:(j + 1) * 512], in_=u2T_d.t[half * 4 + j].rearrange("p (a b) -> p a b", b=512)),
                reads=[u2T_d], writes=[u2T])
        for ex in range(64):
            W13 = W13_rot.get()
            W2 = W2_rot.get()
            c.dma("pool", lambda e, ex=ex, W13=W13: e.dma_start(
                out=W13[:, :, 0:256], in_=w1r.t[ex * 128:(ex + 1) * 128, :].rearrange("p (a b) -> p a b", b=256)), writes=[W13])
            c.dma("pool", lambda e, ex=ex, W13=W13: e.dma_start(
                out=W13[:, :, 256:512], in_=w3r.t[ex * 128:(ex + 1) * 128, :].rearrange("p (a b) -> p a b", b=256)), writes=[W13])
            c.dma("pool", lambda e, ex=ex, W2=W2: e.dma_start(
                out=W2[:].rearrange("p a b -> p (a b)"), in_=w2r.t[ex * 128:(ex + 1) * 128, :]), writes=[W2])
            for b in range(NB):
                blk = half * NB + b
                ph = pH_rot.get()
                for k in range(8):
                    mm(c, ph, ph[:], u2T, u2T[:, k, b * 128:(b + 1) * 128], W13, W13[:, k, :], k == 0, k == 7)
                act(c, sg, sg[:], ph, ph[:, 0:256], AF.Sigmoid)
                tt(c, "dve", sg, sg[:], ph, ph[:, 0:256], sg, sg[:], ALU.mult)
                tt(c, "dve", sg, sg[:], ph, ph[:, 256:512], sg, sg[:], ALU.mult)
                ts(c, "dve", hg, hg[:], sg, sg[:], Gall[:, blk, ex:ex + 1], None, ALU.mult, extra_r=[Gall])
                pT = pT_rot.get()
                for kc in range(2):
                    tr(c, pT, pT[:, kc, :], hg, hg[:, kc * 128:(kc + 1) * 128], ident, ident[:])
                cp(c, "dve", hgT, hgT[:], pT, pT[:, 0:2, :])
                for n in range(2):
                    py = pY_rot.get()
                    for kc in range(2):
                        mm(c, py, py[:], hgT, hgT[:, kc, :], W2, W2[:, kc, n * 512:(n + 1) * 512], kc == 0, kc == 1)
                    if ex == 0:
                        cp(c, "act", yacc, yacc[:, b, n * 512:(n + 1) * 512], py, py[:])
                    else:
                        tt(c, "dve", yacc, yacc[:, b, n * 512:(n + 1) * 512], py, py[:], yacc, yacc[:, b, n * 512:(n + 1) * 512], ALU.add)
            if ex % 4 == 3:
                c.flush()
        for b in range(NB):
            blk = half * NB + b
            c.dma("sp", lambda e, blk=blk: e.dma_start(out=hrow[:], in_=h_d.t[blk * 128:(blk + 1) * 128, :]), reads=[h_d], writes=[hrow])
            tt(c, "dve", yacc, yacc[:, b, :], yacc, yacc[:, b, :], hrow, hrow[:], ALU.add)
            c.dma("sp", lambda e, blk=blk, b=b: e.dma_start(out=out.t[blk * 128:(blk + 1) * 128, :], in_=yacc[:, b, :]), reads=[yacc], writes=[out])
        c.flush()


def _prep_core(inp, cidx):
    b, j = cidx // 2, cidx % 2
    f = np.float32
    x = inp["x"]
    own = OWN[j]
    rep = lambda v, n=128: np.ascontiguousarray(np.broadcast_to(np.asarray(v, f).reshape(1, -1), (n, np.asarray(v).size)))
    colmaj = lambda v: np.ascontiguousarray(np.asarray(v, f).reshape(8, 128).T)
    m = {}
    m["xs"] = np.ascontiguousarray(x[b])
    m["xo"] = np.ascontiguousarray(np.concatenate([x[b, u * UT:(u + 1) * UT] for u in own], 0))
    pos = np.asarray(inp["positions"][b], np.int32)
    m["pos_s"] = np.ascontiguousarray(pos.reshape(64, 128).T)
    pown = np.concatenate([pos[u * UT:(u + 1) * UT] for u in own])
    m["pos_o"] = np.ascontiguousarray(pown.reshape(32, 128).T)
    invf = (10000.0 ** (-np.arange(0, 64, 2, dtype=np.float32) / 64)).astype(f)
    m["invf"] = rep(invf)
    m["ident"] = np.eye(128, dtype=f)
    m["g1"] = rep(inp["norm1_g"][0])
    m["w_in"] = np.ascontiguousarray(inp["w_in"][0])
    m["convw"] = np.ascontiguousarray(np.asarray(inp["conv_w"][0], f).reshape(4, 8, 128).transpose(2, 1, 0))
    m["convb"] = colmaj(inp["conv_b"][0])
    m["lru_wa"] = np.ascontiguousarray(inp["lru_wa"][0])
    m["lru_wx"] = np.ascontiguousarray(inp["lru_wx"][0])
    m["lru_ba"] = colmaj(inp["lru_ba"][0])
    m["lru_bx"] = colmaj(inp["lru_bx"][0])
    m["lru_lam"] = colmaj(inp["lru_lambda"][0])
    m["w_rnn_o"] = np.ascontiguousarray(inp["w_rnn_o"][0])
    m["gq"] = rep(inp["q_norm_g"][0])
    m["w_uq"] = np.ascontiguousarray(inp["w_uq"][0])
    m["gkv"] = rep(inp["kv_norm_g"][0])
    m["w_ukv"] = np.ascontiguousarray(inp["w_ukv"][0])
    m["gqk_q"] = rep(inp["qk_norm_q_g"][0])
    gk = np.asarray(inp["qk_norm_k_g"][0], f)
    m["gqk_k_pe"] = rep(gk[128:192])
    m["gqk_k_col"] = np.ascontiguousarray(gk[0:128].reshape(128, 1))
    m["w_mla_o"] = np.ascontiguousarray(inp["w_mla_o"][0])
    m["w_out"] = np.ascontiguousarray(inp["w_out"][0])
    m["g2"] = rep(inp["norm2_g"][0])
    m["wr"] = np.ascontiguousarray(np.concatenate([inp["router_wg"][0], inp["router_we"][0]], 1))
    m["br"] = rep(np.concatenate([inp["router_bg"][0], inp["router_be"][0]]))
    m["w1r"] = np.ascontiguousarray(np.asarray(inp["exp_w1"][0], f).reshape(64, 8, 128, 256).transpose(0, 2, 1, 3).reshape(64 * 128, 2048))
    m["w3r"] = np.ascontiguousarray(np.asarray(inp["exp_w3"][0], f).reshape(64, 8, 128, 256).transpose(0, 2, 1, 3).reshape(64 * 128, 2048))
    m["w2r"] = np.ascontiguousarray(np.asarray(inp["exp_w2"][0], f).reshape(64, 2, 128, 1024).transpose(0, 2, 1, 3).reshape(64 * 128, 2048))
    kk = (np.arange(4)[None, :, None] * 128 + np.arange(128)[:, None, None])
    qq = np.arange(512)[None, None, :]
    diag = (kk <= qq).astype(f)
    full = np.ones_like(diag)
    zero = np.zeros_like(diag)
    mk = np.zeros((NOWN, 2, 128, 4, 512), f)
    for i in range(NOWN):
        for w in range(2):
            ku = EXT[i] - 2 + w
            mk[i, w] = full if ku < own[i] else (diag if ku == own[i] else zero)
    m["masks"] = mk.reshape(NOWN, 2, 128, 2048).astype(ml_dtypes.bfloat16)
    hid = np.zeros((128, 64), np.int32)
    for i in range(NOWN):
        for ch in range(8):
            hid[:, i * 8 + ch] = own[i] * 1024 + ch * 128 + np.arange(128)
    m["hidx"] = hid
    m["pidx"] = np.arange(128, dtype=f).reshape(128, 1)
    m["ustrict"] = np.triu(np.ones((128, 128), f), 1)
    m["thr"] = np.ascontiguousarray(np.broadcast_to((np.arange(128, dtype=f) * 128).reshape(1, 128), (128, 128)))
    return m


_NC_CACHE = {}


def kernel(**inputs):
    inp = {k: np.asarray(v) for k, v in inputs.items()}
    if "nc" not in _NC_CACHE:
        _NC_CACHE["nc"] = build()
    nc = _NC_CACHE["nc"]
    in_maps = [_prep_core(inp, cidx) for cidx in range(8)]
    res = run_bass_kernel_spmd(nc, in_maps, core_ids=list(range(8)))
    outp = np.zeros((4, S, D), np.float32)
    for cidx in range(8):
        b, j = cidx // 2, cidx % 2
        o = res.results[cidx]["out"]
        for i, u in enumerate(OWN[j]):
            outp[b, u * UT:(u + 1) * UT] = o[i * UT:(i + 1) * UT]
    return outp
```

```python
import math
import numpy as np
from contextlib import ExitStack
import ml_dtypes
import concourse.bass as bass
import concourse.mybir as mybir
from concourse.bass_utils import run_bass_kernel_spmd

F32 = mybir.dt.float32
BF16 = mybir.dt.bfloat16
I32 = mybir.dt.int32
AF = mybir.ActivationFunctionType
ALU = mybir.AluOpType

D = 1024
S = 8192
NU = 16
UT = 512
NOWN = 8
H = 8
EPS = 1e-6
OWN = {0: [0, 3, 4, 7, 8, 11, 12, 15], 1: [1, 2, 5, 6, 9, 10, 13, 14]}
EXT = [4 * (i // 2) + (2 if i % 2 == 0 else 4) for i in range(NOWN)]
MPAD = 16384
NBLK = MPAD // 128
TWO_PI = 6.283185
QSCALE = 192.0 ** -0.5


class T:
    __slots__ = ("t", "name", "writes", "reads")

    def __init__(self, t, name=""):
        self.t = t
        self.name = name
        self.writes = {}
        self.reads = {}

    def __getitem__(self, idx):
        return self.t[idx]


class Ctx:
    ENG = ("pe", "act", "dve", "pool", "sp")

    def __init__(self, nc, stack, block, n_dma_sems=10):
        self.nc = nc
        self.stack = stack
        self.block = block
        self.cnt = {}
        self.semobj = {}
        for n in self.ENG:
            self.semobj["e_" + n] = stack.enter_context(nc.semaphore("s_" + n))
            self.cnt["e_" + n] = 0
        self.dq = {}
        for q in ("sp", "act", "pool"):
            lst = []
            for i in range(n_dma_sems):
                key = "d_%s_%d" % (q, i)
                self.semobj[key] = stack.enter_context(nc.semaphore(key))
                self.cnt[key] = 0
                lst.append(key)
            self.dq[q] = [lst, 0]
        self.seen = {n: {} for n in self.ENG}
        self.prog = {n: [] for n in self.ENG}
        self.ninst = 0

    def flush(self):
        b = self.block
        starters = {"pe": b.tensor, "act": b.scalar, "dve": b.vector, "pool": b.gpsimd, "sp": b.sync}
        for n in self.ENG:
            lst = self.prog[n]
            if not lst:
                continue

            def body(eng, lst=lst):
                for f in lst:
                    f(eng)
            starters[n](body)
            self.prog[n] = []

    def barrier(self):
        toks = dict(self.cnt)
        for n in self.ENG:
            self._need(n, {k: v for k, v in toks.items() if v > 0}, force_own=False)

    def sb(self, name, shape, dt, stack=None):
        self.uid = getattr(self, "uid", 0) + 1
        name = "%s_%d" % (name, self.uid)
        return T((stack or self.stack).enter_context(self.nc.sbuf_tensor(name, list(shape), dt)), name)

    def ps(self, name, shape, dt=F32, stack=None):
        self.uid = getattr(self, "uid", 0) + 1
        name = "%s_%d" % (name, self.uid)
        return T((stack or self.stack).enter_context(self.nc.psum_tensor(name, list(shape), dt)), name)

    def _need(self, eng, toks, force_own=True):
        seen = self.seen[eng]
        own = "e_" + eng
        for k, v in toks.items():
            if k == own and (eng == "pe" or not force_own):
                continue
            if seen.get(k, 0) >= v:
                continue
            self.prog[eng].append(lambda e, s=self.semobj[k], v=v: e.wait_ge(s, v))
            seen[k] = v
            self.ninst += 1

    def _deps(self, eng, reads, writes):
        toks = {}
        for t in reads:
            for k, v in t.writes.items():
                if toks.get(k, 0) < v:
                    toks[k] = v
        for t in writes:
            for k, v in t.writes.items():
                if toks.get(k, 0) < v:
                    toks[k] = v
            for k, v in t.reads.items():
                if toks.get(k, 0) < v:
                    toks[k] = v
        self._need(eng, toks)

    def op(self, eng, fn, reads=(), writes=()):
        self._deps(eng, reads, writes)
        key = "e_" + eng
        self.cnt[key] += 1
        v = self.cnt[key]
        self.prog[eng].append(lambda e, fn=fn, s=self.semobj[key]: fn(e).then_inc(s, 1))
        for t in reads:
            t.reads[key] = v
        for t in writes:
            t.writes[key] = v
        self.ninst += 1

    def dma(self, q, fn, reads=(), writes=()):
        self._deps(q, reads, writes)
        lst, i = self.dq[q]
        key = lst[i % len(lst)]
        self.dq[q][1] = i + 1
        if self.cnt[key] > 0 and self.seen[q].get(key, 0) < self.cnt[key]:
            self.prog[q].append(lambda e, s=self.semobj[key], v=self.cnt[key]: e.wait_ge(s, v))
            self.seen[q][key] = self.cnt[key]
        self.cnt[key] += 16
        v = self.cnt[key]
        self.prog[q].append(lambda e, fn=fn, s=self.semobj[key]: fn(e).then_inc(s, 16))
        for t in reads:
            t.reads[key] = v
        for t in writes:
            t.writes[key] = v
        self.ninst += 1

    def wait_all(self, eng, tiles):
        toks = {}
        for t in tiles:
            for d in (t.writes, t.reads):
                for k, v in d.items():
                    if toks.get(k, 0) < v:
                        toks[k] = v
        self._need(eng, toks)


class Rot:
    def __init__(self, c, name, shape, dt, n, st, psum=False):
        self.tiles = [(c.ps if psum else c.sb)("%s%d" % (name, i), shape, dt, st) for i in range(n)]
        self.i = 0

    def get(self):
        t = self.tiles[self.i % len(self.tiles)]
        self.i += 1
        return t


def act(c, out_t, out_ap, in_t, in_ap, func, extra_r=(), extra_w=(), **kw):
    c.op("act", lambda e: e.activation(out=out_ap, in_=in_ap, func=func, **kw),
         reads=[in_t] + list(extra_r), writes=[out_t] + list(extra_w))


def ts(c, eng, out_t, out_ap, in_t, in_ap, s1, s2, op0, op1=None, extra_r=()):
    if op1 is None:
        c.op(eng, lambda e: e.tensor_scalar(out=out_ap, in0=in_ap, scalar1=s1, scalar2=None, op0=op0),
             reads=[in_t] + list(extra_r), writes=[out_t])
    else:
        c.op(eng, lambda e: e.tensor_scalar(out=out_ap, in0=in_ap, scalar1=s1, scalar2=s2, op0=op0, op1=op1),
             reads=[in_t] + list(extra_r), writes=[out_t])


def tt(c, eng, out_t, out_ap, a_t, a_ap, b_t, b_ap, op):
    c.op(eng, lambda e: e.tensor_tensor(out=out_ap, in0=a_ap, in1=b_ap, op=op), reads=[a_t, b_t], writes=[out_t])


def stt(c, out_t, out_ap, a_t, a_ap, scalar, b_t, b_ap, op0, op1, extra_r=(), extra_w=(), **kw):
    c.op("dve", lambda e: e.scalar_tensor_tensor(out=out_ap, in0=a_ap, scalar=scalar, in1=b_ap, op0=op0, op1=op1, **kw),
         reads=[a_t, b_t] + list(extra_r), writes=[out_t] + list(extra_w))


def cp(c, eng, out_t, out_ap, in_t, in_ap):
    if eng == "act":
        c.op("act", lambda e: e.copy(out=out_ap, in_=in_ap), reads=[in_t], writes=[out_t])
    else:
        c.op(eng, lambda e: e.tensor_copy(out=out_ap, in_=in_ap), reads=[in_t], writes=[out_t])


def mm(c, out_t, out_ap, l_t, l_ap, r_t, r_ap, start, stop):
    c.op("pe", lambda e: e.matmul(out_ap, lhsT=l_ap, rhs=r_ap, start=start, stop=stop, skip_group_check=True),
         reads=[l_t, r_t], writes=[out_t])


def tr(c, out_t, out_ap, in_t, in_ap, id_t, id_ap):
    c.op("pe", lambda e: e.transpose(out=out_ap, in_=in_ap, identity=id_ap), reads=[in_t, id_t], writes=[out_t])


def rstd_from_ss(c, ss_t, ss_ap, n, rms_t, rms_ap, rstd_t, rstd_ap):
    ts(c, "dve", rms_t, rms_ap, ss_t, ss_ap, 1.0 / n, EPS, ALU.mult, ALU.add)
    act(c, rms_t, rms_ap, rms_t, rms_ap, AF.Sqrt)
    c.op("dve", lambda e: e.reciprocal(out=rstd_ap, in_=rms_ap), reads=[rms_t], writes=[rstd_t])


def trig_tables(c, st, posf_t, nblk, invf_t, cos_t, sin_t):
    ang = c.sb("tg_ang", [128, nblk, 32], F32, st)
    ki = c.sb("tg_ki", [128, nblk, 32], I32, st)
    kf = c.sb("tg_kf", [128, nblk, 32], F32, st)
    fl = c.sb("tg_fl", [128, nblk, 32], F32, st)
    tt(c, "dve", ang, ang[:], posf_t, posf_t[:, 0:nblk].unsqueeze(2).to_broadcast([128, nblk, 32]),
       invf_t, invf_t[:, :].unsqueeze(1).to_broadcast([128, nblk, 32]), ALU.mult)
    for (dst, shift) in ((sin_t, 0.0), (cos_t, 0.25)):
        ts(c, "dve", kf, kf[:], ang, ang[:], 1.0 / (2 * math.pi), shift, ALU.mult, ALU.add)
        cp(c, "dve", ki, ki[:], kf, kf[:])
        cp(c, "dve", fl, fl[:], ki, ki[:])
        tt(c, "dve", kf, kf[:], kf, kf[:], fl, fl[:], ALU.subtract)
        ts(c, "dve", fl, fl[:], kf, kf[:], 0.5, None, ALU.is_gt)
        tt(c, "dve", kf, kf[:], kf, kf[:], fl, fl[:], ALU.subtract)
        ts(c, "dve", fl, fl[:], kf, kf[:], -0.5, None, ALU.is_lt)
        tt(c, "dve", kf, kf[:], kf, kf[:], fl, fl[:], ALU.add)
        act(c, dst, dst[:], kf, kf[:], AF.Sin, scale=TWO_PI)


def rope(c, eng, out_t, out_ap3, x_t, x_ap3, cos_t, cos_ap3, sin_t, sin_ap3, tmp_t, tmp_ap3, nh):
    x1, x2 = x_ap3[:, :, 0:32], x_ap3[:, :, 32:64]
    o1, o2 = out_ap3[:, :, 0:32], out_ap3[:, :, 32:64]
    t1 = tmp_ap3[:, :, 0:32]
    t2 = tmp_ap3[:, :, 32:64]
    tt(c, eng, tmp_t, t1, x_t, x2, sin_t, sin_ap3, ALU.mult)
    tt(c, eng, tmp_t, t2, x_t, x1, sin_t, sin_ap3, ALU.mult)
    tt(c, eng, out_t, o1, x_t, x1, cos_t, cos_ap3, ALU.mult)
    tt(c, eng, out_t, o2, x_t, x2, cos_t, cos_ap3, ALU.mult)
    tt(c, eng, out_t, o1, out_t, o1, tmp_t, t1, ALU.subtract)
    tt(c, eng, out_t, o2, out_t, o2, tmp_t, t2, ALU.add)


def norm_transpose(c, st, x_dram_ap, g1_t, ident_t, xt_rot, ub, uT, pT_rot, ssq, rms, rstd, junk):
    xt = xt_rot.get()
    c.dma("sp", lambda e: e.dma_start(out=xt[:], in_=x_dram_ap), writes=[xt])
    for kb in range(4):
        act(c, junk, junk[:], xt, xt[:, kb, :], AF.Square, extra_w=[ssq], accum_out=ssq[:, kb:kb + 1])
    import os
    NT = int(os.environ.get("NT", "9"))
    rstd_from_ss(c, ssq, ssq[:, 0:4], D, rms, rms[:, 0:4], rstd, rstd[:, 0:4])
    for kb in range(4):
        if NT >= 1:
            stt(c, ub, ub[:, kb, :], xt, xt[:, kb, :], rstd[:, kb:kb + 1], g1_t, g1_t[:], ALU.mult, ALU.mult, extra_r=[rstd])
        pT = pT_rot.get()
        if NT >= 2:
            for k in range(8):
                tr(c, pT, pT[:, k, :], ub, ub[:, kb, k * 128:(k + 1) * 128], ident_t, ident_t[:])
        if NT >= 3:
            cp(c, "dve", uT, uT[:, :, kb * 128:(kb + 1) * 128], pT, pT[:])
    return xt


def build(dbg=None):
    nc = bass.Bass("TRN2", target_bir_lowering=False)
    dbg = dbg or {}
    last_phase = dbg.get("last_phase", 9)

    def din(name, shape, dt=F32):
        return T(nc.dram_tensor(name, list(shape), dt, kind="ExternalInput"), name)

    def dscr(name, shape, dt):
        kind = "ExternalOutput" if name in dbg.get("dump", ()) else "Internal"
        return T(nc.dram_tensor(name, list(shape), dt, kind=kind), name)

    xs = din("xs", [S, D])
    xo = din("xo", [NOWN * UT, D])
    pos_s = din("pos_s", [128, 64], I32)
    pos_o = din("pos_o", [128, 32], I32)
    invf = din("invf", [128, 32])
    ident_d = din("ident", [128, 128])
    g1 = din("g1", [128, D])
    w_in = din("w_in", [D, 4672])
    convw = din("convw", [128, 8, 4])
    convb = din("convb", [128, 8])
    lru_wa = din("lru_wa", [8, 128, 128])
    lru_wx = din("lru_wx", [8, 128, 128])
    lru_ba = din("lru_ba", [128, 8])
    lru_bx = din("lru_bx", [128, 8])
    lru_lam = din("lru_lam", [128, 8])
    w_rnn_o = din("w_rnn_o", [D, D])
    gq = din("gq", [128, 256])
    w_uq = din("w_uq", [256, 1536])
    gkv = din("gkv", [128, 256])
    w_ukv = din("w_ukv", [256, 2048])
    gqk_q = din("gqk_q", [128, 192])
    gqk_k_pe = din("gqk_k_pe", [128, 64])
    gqk_k_col = din("gqk_k_col", [128, 1])
    w_mla_o = din("w_mla_o", [D, D])
    w_out = din("w_out", [D, D])
    g2 = din("g2", [128, D])
    wr = din("wr", [D, 72])
    br = din("br", [128, 72])
    w1r = din("w1r", [64 * 128, 2048])
    w3r = din("w3r", [64 * 128, 2048])
    w2r = din("w2r", [64 * 128, 2048])
    masks = din("masks", [NOWN, 2, 128, 4 * 512], BF16)
    hidx = din("hidx", [128, 64], I32)
    pidx = din("pidx", [128, 1])
    ustrict = din("ustrict", [128, 128])
    thr = din("thr", [128, 128])
    out = T(nc.dram_tensor("out", [NOWN * UT, D], F32, kind="ExternalOutput"), "out")

    KT_d = dscr("KT_d", [H, 128, S], BF16)
    V_d = dscr("V_d", [H, NU, 128, 512], BF16)
    hT_d = dscr("hT_d", [NU * 1024, 512], BF16)
    sA_d = dscr("sA_d", [NOWN, 128, 8 * 512], BF16)
    OT_d = dscr("OT_d", [NOWN, 128, 8 * 512], BF16)
    h_d = dscr("h_d", [NOWN * UT, D], F32)
    u2_d = dscr("u2_d", [NOWN * UT, D], BF16)
    xpad_d = dscr("xpad_d", [MPAD, D], BF16)
    ypad_d = dscr("ypad_d", [MPAD, D], F32)
    wcat_d = dscr("wcat_d", [64 * 128, 6144], BF16)
    dbg_d = dscr("dbg_d", [128, 8192], F32)

    sk_dump = dscr("sk_dump", [128, 512], F32)
    with ExitStack() as gst:
        c = Ctx(nc, gst, None)
        ident = c.sb("identb", [128, 128], BF16)
        identf = c.sb("identf", [128, 128], F32)
        KP = c.sb("KP", [64, S], BF16)
        SK = c.sb("SK", [128, 64, 8], F32)
        c.dma("pool", lambda e: e.dma_start(out=ident[:], in_=ident_d[:, :]), writes=[ident])
        c.dma("sp", lambda e: e.dma_start(out=identf[:], in_=ident_d[:, :]), writes=[identf])

        block = gst.enter_context(nc.Block())
        c.block = block
        if False:
            for (src, dst) in ((w1r, w1b_d), (w3r, w3b_d), (w2r, w2b_d)):
                for i in range(16):
                    c.dma("pool", lambda e, src=src, dst=dst, i=i: e.dma_start(
                        out=dst[i * 512:(i + 1) * 512, :], in_=src[i * 512:(i + 1) * 512, :]),
                        reads=[src], writes=[dst])

        if last_phase >= 1:
            with ExitStack() as st:
                phase1(c, st, locals())
                c.barrier()
                c.flush()
        S1all = c.sb("S1all", [128, 32, 64], F32)
        S2all = c.sb("S2all", [128, 32, 64], F32)
        GT = c.sb("GT", [128, 32, 2], F32)
        G_ = dict(locals())
        for (pn, fn) in ((2, phase2a), (3, phase2b), (4, phase2c), (5, phase3)):
            if last_phase >= pn:
                with ExitStack() as st:
                    fn(c, st, G_)
                    c.barrier()
                    c.flush()
        c.wait_all("sp", [out, KT_d, V_d, hT_d, dbg_d, sk_dump])
        c.flush()
    return nc


def phase1(c, st, G):
    xs, w_in, ident, KP, SK = G["xs"], G["w_in"], G["ident"], G["KP"], G["SK"]
    KT_d, V_d, hT_d = G["KT_d"], G["V_d"], G["hT_d"]
    Wxr = c.sb("Wxr", [128, 8, 1024], BF16, st)
    Wkv = c.sb("Wkv", [128, 8, 320], BF16, st)
    Wa = c.sb("Wa", [128, 8, 128], BF16, st)
    Wx = c.sb("Wx", [128, 8, 128], BF16, st)
    Wukv = c.sb("Wukv", [128, 2, 2048], BF16, st)
    w_in_v = G["w_in"].t[:, :].rearrange("(k p) n -> p k n", p=128)
    for k in range(8):
        c.dma("pool", lambda e, k=k: e.dma_start(out=Wxr[:, k, :], in_=w_in_v[:, k, 0:1024]), writes=[Wxr])
    c.dma("pool", lambda e: e.dma_start(out=Wkv[:], in_=w_in_v[:, :, 2304:2624]), writes=[Wkv])
    c.dma("pool", lambda e: e.dma_start(out=Wa[:], in_=G["lru_wa"].t[:, :, :].rearrange("n k j -> k n j")), writes=[Wa])
    c.dma("pool", lambda e: e.dma_start(out=Wx[:], in_=G["lru_wx"].t[:, :, :].rearrange("n k j -> k n j")), writes=[Wx])
    for kc in range(2):
        c.dma("pool", lambda e, kc=kc: e.dma_start(out=Wukv[:, kc, :], in_=G["w_ukv"].t[kc * 128:(kc + 1) * 128, :]), writes=[Wukv])
    def small(name, src, shape, dt=F32):
        t = c.sb(name, shape, dt, st)
        c.dma("sp", lambda e: e.dma_start(out=t[:], in_=src.t[tuple(slice(None) for _ in shape)]), writes=[t])
        return t
    g1 = small("g1s", G["g1"], [128, D])
    cw = small("cw", G["convw"], [128, 8, 4])
    cb = small("cb", G["convb"], [128, 8])
    ba = small("ba", G["lru_ba"], [128, 8])
    bx = small("bx", G["lru_bx"], [128, 8])
    lam = small("lam", G["lru_lam"], [128, 8])
    gkv = small("gkvs", G["gkv"], [128, 256])
    gkpe = small("gkpe", G["gqk_k_pe"], [128, 64])
    gkcol = small("gkcol", G["gqk_k_col"], [128, 1])
    invf = small("invfs", G["invf"], [128, 32])
    posi = small("posi", G["pos_s"], [128, 64], I32)
    posf = c.sb("posf", [128, 64], F32, st)
    cp(c, "dve", posf, posf[:], posi, posi[:])
    cosT = c.sb("cosT", [128, 64, 32], F32, st)
    sinT = c.sb("sinT", [128, 64, 32], F32, st)
    with ExitStack() as st2:
        trig_tables(c, st2, posf, 64, invf, cosT, sinT)
        c.barrier()
        c.flush()
    cl = c.sb("cl", [128, 8], F32, st)
    act(c, cl, cl[:], lam, lam[:], AF.Exp, scale=-1.0)
    ts(c, "dve", cl, cl[:], cl, cl[:], 1.0, None, ALU.add)
    act(c, cl, cl[:], cl, cl[:], AF.Ln)
    ts(c, "dve", cl, cl[:], cl, cl[:], -8.0, None, ALU.mult)

    xt_rot = Rot(c, "xt", [128, 4, D], F32, 1, st)
    ub = c.sb("ub", [128, 4, D], BF16, st)
    uT = c.sb("uT", [128, 8, UT], BF16, st)
    junk = c.sb("junk", [128, D], F32, st)
    ssq = c.sb("ssq", [128, 4], F32, st)
    rms = c.sb("rms", [128, 4], F32, st)
    rstd = c.sb("rstd", [128, 4], F32, st)
    pT_rot = Rot(c, "pT", [128, 8, 128], BF16, 2, st, psum=True)
    KTs_rot = Rot(c, "KTs", [128, 8, UT], BF16, 1, st)
    Vs_rot = Rot(c, "Vs", [128, 8, 128], BF16, 2, st)
    sv = c.sb("sv", [128, 16], F32, st)
    SS0 = c.sb("SS0", [128, 8], F32, st)
    ckvg = c.sb("ckvg", [128, 256], BF16, st)
    ckvT = c.sb("ckvT", [128, 2, 128], BF16, st)
    kpg = c.sb("kpg", [128, 1, 64], F32, st)
    kpr = c.sb("kpr", [128, 1, 64], F32, st)
    kpt = c.sb("kpt", [128, 1, 64], F32, st)
    kpb = c.sb("kpb", [128, 64], BF16, st)
    k0b = c.sb("k0b", [128, 8, 128], BF16, st)

    hT_v = hT_d.t[:, :].rearrange("(u c p) t -> u p c t", c=8, p=128)
    LT = []
    for ln in range(2):
        LT.append(dict(
            xc=c.sb("xcL", [128, UT], F32, st), xcb=c.sb("xcbL", [128, UT], BF16, st), rr=c.sb("rrL", [128, UT], F32, st),
            ii=c.sb("iiL", [128, UT], F32, st), aa=c.sb("aaL", [128, UT], F32, st), mm_=c.sb("mmL", [128, UT], F32, st),
            bi=c.sb("biL", [128, UT], F32, st), hf=c.sb("hfL", [128, UT], F32, st),
            p0=c.ps("pL0", [128, 512], F32, st), p1=c.ps("pL1", [128, 512], F32, st)))
    xrs = [c.sb("xrc", [128, UT + 3], F32, st) for _ in range(8)]
    carries = [c.sb("carryc", [128, 1], F32, st) for _ in range(8)]
    hTbs = [c.sb("hTbc", [128, UT], BF16, st) for _ in range(8)]
    for t_ in xrs + carries:
        c.op("dve", lambda e, t_=t_: e.memset(t_[:], 0.0), writes=[t_])
    pKV = [c.ps("pKV0", [128, 512], F32, st), c.ps("pKV1", [128, 512], F32, st)]
    junk2 = c.sb("junk2", [128, 320], F32, st)

    def rnn_chunk(ch, u, L):
        xc, xcb, rr, ii, aa, mm_, bi, hf = L["xc"], L["xcb"], L["rr"], L["ii"], L["aa"], L["mm_"], L["bi"], L["hf"]
        pa, pb = L["p0"], L["p1"]
        xr, carry, hTb = xrs[ch], carries[ch], hTbs[ch]
        for k in range(8):
            mm(c, pa, pa[:], Wxr, Wxr[:, k, ch * 128:(ch + 1) * 128], uT, uT[:, k, :], k == 0, k == 7)
        if u > 0:
            cp(c, "pool", xr, xr[:, 0:3], xr, xr[:, UT:UT + 3])
        yield
        cp(c, "act", xr, xr[:, 3:UT + 3], pa, pa[:])
        yield
        ts(c, "dve", xc, xc[:], xr, xr[:, 0:UT], cw[:, ch, 0:1], cb[:, ch:ch + 1], ALU.mult, ALU.add, extra_r=[cw, cb])
        for k in range(1, 4):
            stt(c, xc, xc[:], xr, xr[:, k:k + UT], cw[:, ch, k:k + 1], xc, xc[:], ALU.mult, ALU.add, extra_r=[cw])
        yield
        cp(c, "pool", xcb, xcb[:], xc, xc[:])
        yield
        mm(c, pa, pa[:], Wa, Wa[:, ch, :], xcb, xcb[:], True, True)
        mm(c, pb, pb[:], Wx, Wx[:, ch, :], xcb, xcb[:], True, True)
        yield
        act(c, rr, rr[:], pa, pa[:], AF.Sigmoid, extra_r=[ba], bias=ba[:, ch:ch + 1])
        act(c, ii, ii[:], pb, pb[:], AF.Sigmoid, extra_r=[bx], bias=bx[:, ch:ch + 1])
        yield
        act(c, aa, aa[:], rr, rr[:], AF.Exp, extra_r=[cl], scale=cl[:, ch:ch + 1])
        tt(c, "dve", bi, bi[:], xc, xc[:], ii, ii[:], ALU.mult)
        yield
        tt(c, "pool", mm_, mm_[:], aa, aa[:], aa, aa[:], ALU.mult)
        ts(c, "pool", mm_, mm_[:], mm_, mm_[:], -1.0, 1.0, ALU.mult, ALU.add)
        yield
        act(c, mm_, mm_[:], mm_, mm_[:], AF.Sqrt)
        yield
        tt(c, "dve", bi, bi[:], bi, bi[:], mm_, mm_[:], ALU.mult)
        c.op("dve", lambda e: e.tensor_tensor_scan(out=hf[:], data0=aa[:], data1=bi[:], initial=carry[:, 0:1],
                                                   op0=ALU.mult, op1=ALU.add), reads=[aa, bi, carry], writes=[hf])
        cp(c, "dve", carry, carry[:, 0:1], hf, hf[:, UT - 1:UT])
        yield
        cp(c, "act", hTb, hTb[:], hf, hf[:])
        c.dma("sp", lambda e: e.dma_start(out=hT_v[u][:, ch, :], in_=hTb[:]), reads=[hTb], writes=[hT_d])
        yield

    def kv_block(kb, u, KTs):
        blk = u * 4 + kb
        pc = pKV[0]
        for k in range(8):
            mm(c, pc, pc[:, 0:320], uT, uT[:, k, kb * 128:(kb + 1) * 128], Wkv, Wkv[:, k, :], k == 0, k == 7)
        yield
        act(c, junk2, junk2[:, 0:256], pc, pc[:, 0:256], AF.Square, extra_w=[sv], accum_out=sv[:, 0:1])
        act(c, junk2, junk2[:, 256:320], pc, pc[:, 256:320], AF.Square, extra_w=[sv], accum_out=sv[:, 1:2])
        yield
        rstd_from_ss(c, sv, sv[:, 0:1], 256, sv, sv[:, 2:3], sv, sv[:, 3:4])
        tt(c, "dve", ckvg, ckvg[:], pc, pc[:, 0:256], gkv, gkv[:], ALU.mult)
        tt(c, "dve", kpg, kpg[:, 0, :], pc, pc[:, 256:320], gkpe, gkpe[:], ALU.mult)
        yield
        pT = pT_rot.get()
        for kc in range(2):
            tr(c, pT, pT[:, kc, :], ckvg, ckvg[:, kc * 128:(kc + 1) * 128], ident, ident[:])
        yield
        cp(c, "dve", ckvT, ckvT[:], pT, pT[:, 0:2, :])
        rope(c, "pool", kpr, kpr[:], kpg, kpg[:], cosT, cosT[:, blk:blk + 1, :], sinT, sinT[:, blk:blk + 1, :], kpt, kpt[:], 1)
        yield
        ts(c, "dve", kpb, kpb[:], kpr, kpr[:, 0, :], sv[:, 2:3], None, ALU.mult, extra_r=[sv])
        pT2 = pT_rot.get()
        tr(c, pT2, pT2[0:64, 0, :], kpb, kpb[:], ident, ident[:])
        yield
        cp(c, "act", KP, KP[:, blk * 128:(blk + 1) * 128], pT2, pT2[0:64, 0, :])
        Vs = Vs_rot.get()
        for n in range(4):
            pk = pKV[(n + 1) % 2]
            for kc in range(2):
                mm(c, pk, pk[:], ckvT, ckvT[:, kc, :], Wukv, Wukv[:, kc, n * 512:(n + 1) * 512], kc == 0, kc == 1)
            yield
            for hh in range(2):
                h = n * 2 + hh
                act(c, junk2, junk2[:, 0:128], pk, pk[:, hh * 256:hh * 256 + 128], AF.Square, extra_w=[SS0], accum_out=SS0[:, h:h + 1])
                cp(c, "act", k0b, k0b[:, h, :], pk, pk[:, hh * 256:hh * 256 + 128])
                act(c, Vs, Vs[:, h, :], pk, pk[:, hh * 256 + 128:hh * 256 + 256], AF.Copy, extra_r=[sv], scale=sv[:, 3:4])
            yield
        c.dma("sp", lambda e, Vs=Vs: e.dma_start(
            out=V_d.t[:, u, :, kb * 128:(kb + 1) * 128].rearrange("h p d -> p h d"), in_=Vs[:]), reads=[Vs], writes=[V_d])
        pT3 = pT_rot.get()
        for h in range(8):
            tr(c, pT3, pT3[:, h, :], k0b, k0b[:, h, :], ident, ident[:])
        yield
        for h in range(8):
            act(c, KTs, KTs[:, h, kb * 128:(kb + 1) * 128], pT3, pT3[:, h, :], AF.Copy, extra_r=[gkcol], scale=gkcol[:, 0:1])
        yield
        tt(c, "dve", sv, sv[:, 4:5], sv, sv[:, 3:4], sv, sv[:, 3:4], ALU.mult)
        ts(c, "dve", SS0, SS0[:], SS0, SS0[:], sv[:, 4:5], sv[:, 1:2], ALU.mult, ALU.add, extra_r=[sv])
        ts(c, "dve", SS0, SS0[:], SS0, SS0[:], 1.0 / 192, EPS, ALU.mult, ALU.add)
        yield
        act(c, SS0, SS0[:], SS0, SS0[:], AF.Sqrt)
        yield
        c.op("dve", lambda e: e.reciprocal(out=SS0[:], in_=SS0[:]), reads=[SS0], writes=[SS0])
        ts(c, "dve", SK, SK[:, blk, :], SS0, SS0[:], sv[:, 3:4], QSCALE, ALU.mult, ALU.mult, extra_r=[sv])
        yield

    def chain(gens):
        for g in gens:
            yield from g

    for u in range(G["dbg"].get("nu", NU)):
        xsrc = xs.t[u * UT:(u + 1) * UT, :].rearrange("(kb p) d -> p kb d", p=128)
        norm_transpose(c, st, xsrc, g1, ident, xt_rot, ub, uT, pT_rot, ssq, rms, rstd, junk)
        KTs = KTs_rot.get()
        lanes = [chain([rnn_chunk(ch, u, LT[0]) for ch in (0, 2, 4, 6)]),
                 chain([rnn_chunk(ch, u, LT[1]) for ch in (1, 3, 5, 7)]),
                 chain([kv_block(kb, u, KTs) for kb in range(4)])]
        while lanes:
            for g in list(lanes):
                try:
                    next(g)
                except StopIteration:
                    lanes.remove(g)
        c.dma("sp", lambda e, u=u, KTs=KTs: e.dma_start(
            out=KT_d.t[:, :, u * UT:(u + 1) * UT].rearrange("h p t -> p h t"), in_=KTs[:]), reads=[KTs], writes=[KT_d])
        if u == 0 and G["last_phase"] >= 5:
            for (src, col) in ((G["w1r"], 0), (G["w3r"], 2048), (G["w2r"], 4096)):
                for i in range(16):
                    c.dma("pool", lambda e, src=src, col=col, i=i: e.dma_start(
                        out=G["wcat_d"].t[i * 512:(i + 1) * 512, col:col + 2048], in_=src.t[i * 512:(i + 1) * 512, :]),
                        reads=[src], writes=[G["wcat_d"]])
        if u % 4 == 3:
            c.flush()
    if "dbg_d" in G["dbg"].get("dump", ()):
        dbt = c.sb("dbt", [128, 8192], F32, st)
        c.op("dve", lambda e: e.memset(dbt[:], 0.0), writes=[dbt])
        cp(c, "dve", dbt, dbt[0:64, :], KP, KP[:, :])
        c.dma("sp", lambda e: e.dma_start(out=G["dbg_d"].t[:, :], in_=dbt[:]), reads=[dbt], writes=[G["dbg_d"]])
        sk_d = G["sk_dump"]
        c.dma("sp", lambda e: e.dma_start(out=sk_d.t[:, :], in_=SK[:].rearrange("p a b -> p (a b)")), reads=[SK], writes=[sk_d])


def run_lanes(lanes):
    lanes = list(lanes)
    while lanes:
        for g in list(lanes):
            try:
                next(g)
            except StopIteration:
                lanes.remove(g)


def chain_gens(gens):
    for g in gens:
        yield from g


def load_w(c, st, name, src_ap_fn, nk, ncols):
    t = c.sb(name, [128, nk, ncols], BF16, st)
    for k in range(nk):
        c.dma("pool", lambda e, k=k: e.dma_start(out=t[:, k, :], in_=src_ap_fn(k)), writes=[t])
    return t


def small_t(c, st, name, src, shape, dt=F32):
    t = c.sb(name, shape, dt, st)
    c.dma("sp", lambda e: e.dma_start(out=t[:], in_=src.t[tuple(slice(None) for _ in shape)]), writes=[t])
    return t


def norm_tiles(c, st):
    return dict(xt_rot=Rot(c, "xt", [128, 4, D], F32, 1, st), ub=c.sb("ub", [128, 4, D], BF16, st),
                uT=c.sb("uT", [128, 8, UT], BF16, st), junk=c.sb("junk", [128, D], F32, st),
                ssq=c.sb("ssq", [128, 4], F32, st), rms=c.sb("rms", [128, 4], F32, st), rstd=c.sb("rstd", [128, 4], F32, st))


def do_norm(c, st, N, xsrc, g1, ident, pT_rot):
    return norm_transpose(c, st, xsrc, g1, ident, N["xt_rot"], N["ub"], N["uT"], pT_rot, N["ssq"], N["rms"], N["rstd"], N["junk"])


def phase2a(c, st, G):
    xo, ident, hT_d, sA_d = G["xo"], G["ident"], G["hT_d"], G["sA_d"]
    w_in_v = G["w_in"].t[:, :].rearrange("(k p) n -> p k n", p=128)
    wro_v = G["w_rnn_o"].t[:, :].rearrange("(k p) n -> p k n", p=128)
    Wy = load_w(c, st, "Wy", lambda k: w_in_v[:, k, 1024:2048], 8, 1024)
    Wga = load_w(c, st, "Wga", lambda k: w_in_v[:, k, 2624:3648], 8, 1024)
    Wro = load_w(c, st, "Wro", lambda k: wro_v[:, k, :], 8, 1024)
    g1 = small_t(c, st, "g1s", G["g1"], [128, D])
    hidx = small_t(c, st, "hidx", G["hidx"], [128, 64], I32)
    N = norm_tiles(c, st)
    uT = N["uT"]
    pT_rot = Rot(c, "pT", [128, 8, 128], BF16, 2, st, psum=True)
    pA_rot = Rot(c, "pA", [128, 512], F32, 4, st, psum=True)
    hs = c.sb("hs", [128, 8, UT], BF16, st)
    LA = [dict(ys=c.sb("ys", [128, UT], F32, st), y2=c.sb("y2", [128, UT], F32, st), sg=c.sb("sg", [128, UT], F32, st),
               p0=pA_rot.tiles[2 * ln], p1=pA_rot.tiles[2 * ln + 1]) for ln in range(2)]
    zT = c.sb("zT", [128, 8, UT], BF16, st)
    sAT = c.sb("sAT", [128, 8, UT], BF16, st)
    for i in range(NOWN):
        xsrc = xo.t[i * UT:(i + 1) * UT, :].rearrange("(kb p) d -> p kb d", p=128)
        do_norm(c, st, N, xsrc, g1, ident, pT_rot)
        for ch in range(8):
            c.dma("pool", lambda e, ch=ch, i=i: e.indirect_dma_start(
                out=hs[:, ch, :], out_offset=None, in_=hT_d.t[:, :],
                in_offset=bass.IndirectOffsetOnAxis(ap=hidx[:, i * 8 + ch:i * 8 + ch + 1], axis=0)),
                reads=[hidx, hT_d], writes=[hs])
        def gelu_chunk(ch, L):
            ys, y2, sg, pa = L["ys"], L["y2"], L["sg"], L["p0"]
            for k in range(8):
                mm(c, pa, pa[:], Wy, Wy[:, k, ch * 128:(ch + 1) * 128], uT, uT[:, k, :], k == 0, k == 7)
            yield
            cp(c, "act", ys, ys[:], pa, pa[:])
            yield
            tt(c, "pool", y2, y2[:], ys, ys[:], ys, ys[:], ALU.mult)
            ts(c, "pool", y2, y2[:], y2, y2[:], 0.044715, 1.0, ALU.mult, ALU.add)
            tt(c, "pool", y2, y2[:], y2, y2[:], ys, ys[:], ALU.mult)
            yield
            act(c, sg, sg[:], y2, y2[:], AF.Sigmoid, scale=1.5957691216)
            yield
            tt(c, "dve", sg, sg[:], sg, sg[:], ys, ys[:], ALU.mult)
            tt(c, "dve", zT, zT[:, ch, :], sg, sg[:], hs, hs[:, ch, :], ALU.mult)
            yield

        def a_chunk(co, L):
            sg, pa, pg = L["sg"], L["p0"], L["p1"]
            for k in range(8):
                mm(c, pa, pa[:], Wro, Wro[:, k, co * 128:(co + 1) * 128], zT, zT[:, k, :], k == 0, k == 7)
            yield
            for k in range(8):
                mm(c, pg, pg[:], Wga, Wga[:, k, co * 128:(co + 1) * 128], uT, uT[:, k, :], k == 0, k == 7)
            yield
            act(c, sg, sg[:], pg, pg[:], AF.Sigmoid)
            yield
            tt(c, "dve", sAT, sAT[:, co, :], pa, pa[:], sg, sg[:], ALU.mult)
            yield

        run_lanes([chain_gens([gelu_chunk(ch, LA[ln]) for ch in range(ln, 8, 2)]) for ln in range(2)])
        run_lanes([chain_gens([a_chunk(co, LA[ln]) for co in range(ln, 8, 2)]) for ln in range(2)])
        c.dma("sp", lambda e, i=i: e.dma_start(out=sA_d.t[i], in_=sAT[:].rearrange("p a b -> p (a b)")), reads=[sAT], writes=[sA_d])
        c.flush()


def phase2b(c, st, G):
    xo, ident, KP, SK = G["xo"], G["ident"], G["KP"], G["SK"]
    KT_d, V_d, OT_d, masks = G["KT_d"], G["V_d"], G["OT_d"], G["masks"]
    w_in_v = G["w_in"].t[:, :].rearrange("(k p) n -> p k n", p=128)
    Wcq = load_w(c, st, "Wcq", lambda k: w_in_v[:, k, 2048:2304], 8, 256)
    Wuq = load_w(c, st, "Wuq", lambda k: G["w_uq"].t[k * 128:(k + 1) * 128, :], 2, 1536)
    g1 = small_t(c, st, "g1s", G["g1"], [128, D])
    gq = small_t(c, st, "gqs", G["gq"], [128, 256])
    gqk = small_t(c, st, "gqk", G["gqk_q"], [128, 192])
    invf = small_t(c, st, "invfs", G["invf"], [128, 32])
    posi = small_t(c, st, "posi", G["pos_o"], [128, 32], I32)
    posf = c.sb("posf", [128, 32], F32, st)
    cp(c, "dve", posf, posf[:], posi, posi[:])
    cosO = c.sb("cosO", [128, 32, 32], F32, st)
    sinO = c.sb("sinO", [128, 32, 32], F32, st)
    with ExitStack() as st2:
        trig_tables(c, st2, posf, 32, invf, cosO, sinO)
        c.barrier()
        c.flush()
    N = norm_tiles(c, st)
    uT = N["uT"]
    junk = N["junk"]
    pT_rot = Rot(c, "pT", [128, 8, 128], BF16, 1, st, psum=True)
    pS_rot = Rot(c, "pS", [128, 512], F32, 3, st, psum=True)
    pO = [c.ps("pO%d" % q, [128, 512], F32, st) for q in range(4)]
    onesb = c.sb("onesb2", [128, 128], BF16, st)
    c.op("dve", lambda e: e.memset(onesb[:], 1.0), writes=[onesb])
    rinv = c.sb("rinv", [128, 512], F32, st)
    sv = c.sb("sv", [128, 16], F32, st)
    SSQ = c.sb("SSQ", [128, 8], F32, st)
    FQ = c.sb("FQ", [128, 8], F32, st)
    cqg = c.sb("cqg", [128, 256], BF16, st)
    cqT = c.sb("cqT", [128, 2, 128], BF16, st)
    q0s = c.sb("q0s", [128, 8, 192], F32, st)
    qr = c.sb("qr", [128, 8, 64], F32, st)
    qtmp = c.sb("qtmp", [128, 8, 64], F32, st)
    qbn = c.sb("qbn", [128, 8, 128], BF16, st)
    qbp = c.sb("qbp", [128, 8, 64], BF16, st)
    QT = c.sb("QT", [128, 8, UT], BF16, st)
    QP = c.sb("QP", [64, 8, UT], BF16, st)
    msk = [c.sb("msk%d" % w, [128, 4, 512], BF16, st) for w in range(2)]
    KT_rot = Rot(c, "KTt", [128, 512], BF16, 3, st)
    V_rot = Rot(c, "Vt", [128, 4, 130], BF16, 3, st)
    for vt in V_rot.tiles:
        c.op("dve", lambda e, vt=vt: e.memset(vt[:], 1.0), writes=[vt])
    PT_rot = Rot(c, "PT", [128, 512], BF16, 3, st)
    rs = c.sb("rs", [128, 4], F32, st)
    ob = c.sb("ob", [128, 128], BF16, st)
    OT = c.sb("OT", [128, 8, UT], BF16, st)
    q0f = q0s[:].rearrange("p a b -> p (a b)")
    for i in range(NOWN):
        xsrc = xo.t[i * UT:(i + 1) * UT, :].rearrange("(kb p) d -> p kb d", p=128)
        do_norm(c, st, N, xsrc, g1, ident, pT_rot)
        for w in range(2):
            c.dma("sp", lambda e, w=w, i=i: e.dma_start(out=msk[w][:].rearrange("p a b -> p (a b)"), in_=masks.t[i, w]), writes=[msk[w]])
        for kb in range(4):
            blk = i * 4 + kb
            pc = pO[0]
            for k in range(8):
                mm(c, pc, pc[:, 0:256], uT, uT[:, k, kb * 128:(kb + 1) * 128], Wcq, Wcq[:, k, :], k == 0, k == 7)
            act(c, junk, junk[:, 0:256], pc, pc[:, 0:256], AF.Square, extra_w=[sv], accum_out=sv[:, 0:1])
            rstd_from_ss(c, sv, sv[:, 0:1], 256, sv, sv[:, 2:3], sv, sv[:, 3:4])
            tt(c, "dve", cqg, cqg[:], pc, pc[:, 0:256], gq, gq[:], ALU.mult)
            pT = pT_rot.get()
            for kc in range(2):
                tr(c, pT, pT[:, kc, :], cqg, cqg[:, kc * 128:(kc + 1) * 128], ident, ident[:])
            cp(c, "dve", cqT, cqT[:], pT, pT[:, 0:2, :])
            for n in range(3):
                pq = pO[1 + n]
                for kc in range(2):
                    mm(c, pq, pq[:], cqT, cqT[:, kc, :], Wuq, Wuq[:, kc, n * 512:(n + 1) * 512], kc == 0, kc == 1)
                cp(c, "act", q0s, q0f[:, n * 512:(n + 1) * 512], pq, pq[:])
            for h in range(8):
                act(c, junk, junk[:, 0:192], q0s, q0s[:, h, :], AF.Square, extra_w=[SSQ], accum_out=SSQ[:, h:h + 1])
            tt(c, "dve", sv, sv[:, 4:5], sv, sv[:, 3:4], sv, sv[:, 3:4], ALU.mult)
            ts(c, "dve", FQ, FQ[:], SSQ, SSQ[:], sv[:, 4:5], 1.0 / 192, ALU.mult, ALU.mult, extra_r=[sv])
            ts(c, "dve", FQ, FQ[:], FQ, FQ[:], EPS, None, ALU.add)
            act(c, FQ, FQ[:], FQ, FQ[:], AF.Sqrt)
            c.op("dve", lambda e: e.reciprocal(out=FQ[:], in_=FQ[:]), reads=[FQ], writes=[FQ])
            ts(c, "dve", FQ, FQ[:], FQ, FQ[:], sv[:, 3:4], None, ALU.mult, extra_r=[sv])
            tt(c, "dve", q0s, q0s[:], q0s, q0s[:], FQ, FQ[:].unsqueeze(2).to_broadcast([128, 8, 192]), ALU.mult)
            tt(c, "pool", q0s, q0s[:], q0s, q0s[:], gqk, gqk[:].unsqueeze(1).to_broadcast([128, 8, 192]), ALU.mult)
            rope(c, "pool", qr, qr[:], q0s, q0s[:, :, 128:192], cosO, cosO[:, blk:blk + 1, :].to_broadcast([128, 8, 32]),
                 sinO, sinO[:, blk:blk + 1, :].to_broadcast([128, 8, 32]), qtmp, qtmp[:], 8)
            cp(c, "dve", qbn, qbn[:], q0s, q0s[:, :, 0:128])
            cp(c, "dve", qbp, qbp[:], qr, qr[:])
            pT = pT_rot.get()
            for h in range(8):
                tr(c, pT, pT[:, h, :], qbn, qbn[:, h, :], ident, ident[:])
            cp(c, "dve", QT, QT[:, :, kb * 128:(kb + 1) * 128], pT, pT[:])
            pT = pT_rot.get()
            for h in range(8):
                tr(c, pT, pT[0:64, h, :], qbp, qbp[:, h, :], ident, ident[:])
            cp(c, "dve", QP, QP[:, :, kb * 128:(kb + 1) * 128], pT, pT[0:64, :, :])
        E = EXT[i]
        for h in range(8):
            tiles = [(ku, kb) for ku in range(E) for kb in range(4)]
            kv = {}

            def load_kv(ku, h=h):
                KTt = KT_rot.get()
                Vt = V_rot.get()
                c.dma("sp", lambda e, KTt=KTt: e.dma_start(out=KTt[:], in_=KT_d.t[h, :, ku * 512:(ku + 1) * 512]),
                      reads=[KT_d], writes=[KTt])
                c.dma("sp", lambda e, Vt=Vt: e.dma_start(
                    out=Vt[:, :, 0:128], in_=V_d.t[h, ku].rearrange("p (kb d) -> p kb d", d=128)), reads=[V_d], writes=[Vt])
                kv[ku] = (KTt, Vt)

            def qk(t, h=h):
                ku, kb = tiles[t]
                if kb == 0 and ku + 1 < E:
                    load_kv(ku + 1)
                KTt = kv[ku][0]
                kblk = ku * 4 + kb
                pS = pS_rot.get()
                mm(c, pS, pS[:], KTt, KTt[:, kb * 128:(kb + 1) * 128], QT, QT[:, h, :], True, False)
                mm(c, pS, pS[:], KP, KP[:, kblk * 128:(kblk + 1) * 128], QP, QP[:, h, :], False, True)
                return pS

            load_kv(0)
            pSq = [qk(0)]
            if len(tiles) > 1:
                pSq.append(qk(1))
            for t in range(len(tiles)):
                ku, kb = tiles[t]
                kblk = ku * 4 + kb
                pS = pSq.pop(0)
                if t + 2 < len(tiles):
                    pSq.append(qk(t + 2))
                Vt = kv[ku][1]
                PT = PT_rot.get()
                act(c, PT, PT[:], pS, pS[:], AF.Exp, extra_r=[SK], scale=SK[:, kblk, h:h + 1])
                if ku >= E - 2:
                    w = ku - (E - 2)
                    tt(c, "pool", PT, PT[:], PT, PT[:], msk[w], msk[w][:, kb, :], ALU.mult)
                pOT, pRS = pO[2 * (h % 2)], pO[2 * (h % 2) + 1]
                mm(c, pOT, pOT[:], Vt, Vt[:, kb, 0:128], PT, PT[:], t == 0, t == len(tiles) - 1)
                mm(c, pRS, pRS[:], onesb, onesb[:], PT, PT[:], t == 0, t == len(tiles) - 1)
            pOT, pRS = pO[2 * (h % 2)], pO[2 * (h % 2) + 1]
            c.op("dve", lambda e, pRS=pRS: e.reciprocal(out=rinv[:], in_=pRS[:]), reads=[pRS], writes=[rinv])
            tt(c, "dve", OT, OT[:, h, :], pOT, pOT[:], rinv, rinv[:], ALU.mult)
            c.flush()
        c.dma("sp", lambda e, i=i: e.dma_start(out=OT_d.t[i], in_=OT[:].rearrange("p a b -> p (a b)")), reads=[OT], writes=[OT_d])


def phase2c(c, st, G):
    xo, ident, OT_d, sA_d, h_d, u2_d = G["xo"], G["ident"], G["OT_d"], G["sA_d"], G["h_d"], G["u2_d"]
    S1all, S2all, GT = G["S1all"], G["S2all"], G["GT"]
    w_in_v = G["w_in"].t[:, :].rearrange("(k p) n -> p k n", p=128)
    wmo_v = G["w_mla_o"].t[:, :].rearrange("(k p) n -> p k n", p=128)
    wo_v = G["w_out"].t[:, :].rearrange("(k p) n -> p k n", p=128)
    wr_v = G["wr"].t[:, :].rearrange("(k p) n -> p k n", p=128)
    Wgb = load_w(c, st, "Wgb", lambda k: w_in_v[:, k, 3648:4672], 8, 1024)
    Wmo = load_w(c, st, "Wmo", lambda k: wmo_v[:, k, :], 8, 1024)
    Wo = load_w(c, st, "Wo", lambda k: wo_v[:, k, :], 8, 1024)
    Wr = load_w(c, st, "Wr", lambda k: wr_v[:, k, :], 8, 72)
    g1 = small_t(c, st, "g1s", G["g1"], [128, D])
    g2 = small_t(c, st, "g2s", G["g2"], [128, D])
    brt = small_t(c, st, "brt", G["br"], [128, 72])
    N = norm_tiles(c, st)
    uT = N["uT"]
    junk = N["junk"]
    pT_rot = Rot(c, "pT", [128, 8, 128], BF16, 2, st, psum=True)
    pA_rot = Rot(c, "pA", [128, 512], F32, 4, st, psum=True)
    OT = c.sb("OT", [128, 8, UT], BF16, st)
    sAT = c.sb("sAT", [128, 8, UT], BF16, st)
    LC = [dict(sg=c.sb("sg", [128, UT], F32, st), p0=pA_rot.tiles[2 * ln], p1=pA_rot.tiles[2 * ln + 1]) for ln in range(2)]
    mT = c.sb("mT", [128, 8, UT], BF16, st)
    hrow = c.sb("hrow", [128, D], F32, st)
    u2b = c.sb("u2b", [128, D], BF16, st)
    u2T = c.sb("u2T", [128, 8, UT], BF16, st)
    sv = c.sb("sv", [128, 16], F32, st)
    lg = c.sb("lg", [128, 72], F32, st)
    m8 = c.sb("m8", [128, 8], F32, st)
    oh = c.sb("oh", [128, 8], F32, st)
    em = c.sb("em", [128, 8, 8], F32, st)
    s1 = c.sb("s1", [128, 64], F32, st)
    s2 = c.sb("s2", [128, 64], F32, st)
    emf = em[:].rearrange("p a b -> p (a b)")
    for i in range(NOWN):
        xsrc = xo.t[i * UT:(i + 1) * UT, :].rearrange("(kb p) d -> p kb d", p=128)
        xt = do_norm(c, st, N, xsrc, g1, ident, pT_rot)
        c.dma("sp", lambda e, i=i: e.dma_start(out=OT[:].rearrange("p a b -> p (a b)"), in_=OT_d.t[i]), reads=[OT_d], writes=[OT])
        c.dma("sp", lambda e, i=i: e.dma_start(out=sAT[:].rearrange("p a b -> p (a b)"), in_=sA_d.t[i]), reads=[sA_d], writes=[sAT])
        def merge_chunk(co, L):
            sg, pb, pg = L["sg"], L["p0"], L["p1"]
            for k in range(8):
                mm(c, pb, pb[:], Wmo, Wmo[:, k, co * 128:(co + 1) * 128], OT, OT[:, k, :], k == 0, k == 7)
            yield
            for k in range(8):
                mm(c, pg, pg[:], Wgb, Wgb[:, k, co * 128:(co + 1) * 128], uT, uT[:, k, :], k == 0, k == 7)
            yield
            act(c, sg, sg[:], pg, pg[:], AF.Sigmoid)
            yield
            tt(c, "dve", sg, sg[:], pb, pb[:], sg, sg[:], ALU.mult)
            tt(c, "dve", mT, mT[:, co, :], sg, sg[:], sAT, sAT[:, co, :], ALU.add)
            yield

        run_lanes([chain_gens([merge_chunk(co, LC[ln]) for co in range(ln, 8, 2)]) for ln in range(2)])
        for kb in range(4):
            blk = i * 4 + kb
            for n in range(2):
                ph = pA_rot.get()
                for k in range(8):
                    mm(c, ph, ph[:], mT, mT[:, k, kb * 128:(kb + 1) * 128], Wo, Wo[:, k, n * 512:(n + 1) * 512], k == 0, k == 7)
                tt(c, "dve", hrow, hrow[:, n * 512:(n + 1) * 512], ph, ph[:], xt, xt[:, kb, n * 512:(n + 1) * 512], ALU.add)
            c.dma("sp", lambda e, blk=blk: e.dma_start(out=h_d.t[blk * 128:(blk + 1) * 128, :], in_=hrow[:]), reads=[hrow], writes=[h_d])
            act(c, junk, junk[:], hrow, hrow[:], AF.Square, extra_w=[sv], accum_out=sv[:, 0:1])
            rstd_from_ss(c, sv, sv[:, 0:1], D, sv, sv[:, 2:3], sv, sv[:, 3:4])
            stt(c, u2b, u2b[:], hrow, hrow[:], sv[:, 3:4], g2, g2[:], ALU.mult, ALU.mult, extra_r=[sv])
            c.dma("sp", lambda e, blk=blk: e.dma_start(out=u2_d.t[blk * 128:(blk + 1) * 128, :], in_=u2b[:]), reads=[u2b], writes=[u2_d])
            pT = pT_rot.get()
            for k in range(8):
                tr(c, pT, pT[:, k, :], u2b, u2b[:, k * 128:(k + 1) * 128], ident, ident[:])
            cp(c, "dve", u2T, u2T[:, :, kb * 128:(kb + 1) * 128], pT, pT[:])
            pl = pA_rot.get()
            for k in range(8):
                mm(c, pl, pl[:, 0:72], u2T, u2T[:, k, kb * 128:(kb + 1) * 128], Wr, Wr[:, k, :], k == 0, k == 7)
            tt(c, "dve", lg, lg[:], pl, pl[:, 0:72], brt, brt[:], ALU.add)
            c.op("dve", lambda e: e.max(out=m8[:], in_=lg[:, 0:8]), reads=[lg], writes=[m8])
            ts(c, "dve", oh, oh[:], lg, lg[:, 0:8], m8[:, 0:1], None, ALU.is_ge, extra_r=[m8])
            ts(c, "dve", sv, sv[:, 5:6], m8, m8[:, 0:1], -1.0, None, ALU.mult)
            act(c, junk, junk[:, 0:8], lg, lg[:, 0:8], AF.Exp, extra_r=[sv], extra_w=[sv], bias=sv[:, 5:6], accum_out=sv[:, 6:7])
            c.op("dve", lambda e: e.reciprocal(out=sv[:, 7:8], in_=sv[:, 6:7]), reads=[sv], writes=[sv])
            ts(c, "dve", oh, oh[:], oh, oh[:], -1.0, 1e9, ALU.add, ALU.mult)
            tt(c, "dve", em, em[:], lg, lg[:, 8:72].rearrange("p (a b) -> p a b", b=8), oh, oh[:].unsqueeze(2).to_broadcast([128, 8, 8]), ALU.add)
            c.op("dve", lambda e: e.max(out=m8[:], in_=emf), reads=[em], writes=[m8])
            ts(c, "dve", s1, s1[:], em, emf, m8[:, 0:1], None, ALU.is_ge, extra_r=[m8])
            ts(c, "dve", s2, s2[:], em, emf, m8[:, 1:2], None, ALU.is_ge, extra_r=[m8])
            tt(c, "dve", sv, sv[:, 8:9], m8, m8[:, 0:1], m8, m8[:, 1:2], ALU.subtract)
            act(c, sv, sv[:, 9:10], sv, sv[:, 8:9], AF.Sigmoid)
            tt(c, "dve", sv, sv[:, 10:11], sv, sv[:, 9:10], sv, sv[:, 7:8], ALU.mult)
            tt(c, "dve", sv, sv[:, 11:12], sv, sv[:, 7:8], sv, sv[:, 10:11], ALU.subtract)
            tt(c, "dve", sv, sv[:, 12:13], sv, sv[:, 10:11], sv, sv[:, 11:12], ALU.subtract)
            cp(c, "dve", S1all, S1all[:, blk, :], s1, s1[:])
            tt(c, "dve", S2all, S2all[:, blk, :], s2, s2[:], s1, s1[:], ALU.subtract)
            cp(c, "dve", GT, GT[:, blk, 0:2], sv, sv[:, 10:12])
        c.flush()


def phase3(c, st, G):
    ident, h_d, u2_d, out = G["ident"], G["h_d"], G["u2_d"], G["out"]
    S1all, S2all, GT = G["S1all"], G["S2all"], G["GT"]
    xpad_d, ypad_d, w1r, w3r, w2r = G["xpad_d"], G["ypad_d"], G["w1r"], G["w3r"], G["w2r"]
    ustr = c.sb("ustr", [128, 128], BF16, st)
    c.dma("pool", lambda e: e.dma_start(out=ustr[:], in_=G["ustrict"].t[:, :]), writes=[ustr])
    onesb = c.sb("onesb", [128, 128], BF16, st)
    c.op("dve", lambda e: e.memset(onesb[:], 1.0), writes=[onesb])
    thr = small_t(c, st, "thr", G["thr"], [128, 128])
    pidx = small_t(c, st, "pidx", G["pidx"], [128, 1])
    pA_rot = Rot(c, "pA3", [128, 512], F32, 2, st, psum=True)
    A = c.sb("A3", [128, 64], BF16, st)
    Acum = c.sb("Acum", [128, 64], BF16, st)
    c.op("dve", lambda e: e.memset(Acum[:], 0.0), writes=[Acum])
    RK = c.sb("RK", [128, 32, 64], F32, st)
    for b in range(32):
        tt(c, "dve", A, A[:], S1all, S1all[:, b, :], S2all, S2all[:, b, :], ALU.add)
        pr = pA_rot.get()
        mm(c, pr, pr[:, 0:64], ustr, ustr[:], A, A[:], True, False)
        mm(c, pr, pr[:, 0:64], onesb, onesb[:], Acum, Acum[:], False, True)
        cp(c, "act", RK, RK[:, b, :], pr, pr[:, 0:64])
        tt(c, "dve", Acum, Acum[:], Acum, Acum[:], A, A[:], ALU.add)
    pr = pA_rot.get()
    mm(c, pr, pr[:, 0:64], onesb, onesb[:], Acum, Acum[:], True, True)
    tot = c.sb("tot", [128, 64], F32, st)
    f1 = c.sb("f1", [128, 64], F32, st)
    f2 = c.sb("f2", [128, 64], F32, st)
    ki = c.sb("ki3", [128, 64], I32, st)
    cp(c, "act", tot, tot[:], pr, pr[:, 0:64])
    ts(c, "dve", f1, f1[:], tot, tot[:], 127.0, 1.0 / 128, ALU.add, ALU.mult)
    cp(c, "dve", ki, ki[:], f1, f1[:])
    cp(c, "dve", f2, f2[:], ki, ki[:])
    tt(c, "dve", f1, f1[:], f2, f2[:], f1, f1[:], ALU.is_gt)
    tt(c, "dve", f2, f2[:], f2, f2[:], f1, f1[:], ALU.subtract)
    padded = c.sb("padded", [128, 64], F32, st)
    ts(c, "dve", padded, padded[:], f2, f2[:], 128.0, None, ALU.mult)
    pend = c.sb("pend", [128, 64], F32, st)
    pstart = c.sb("pstart", [128, 64], F32, st)
    ones64 = c.sb("ones64", [128, 64], F32, st)
    c.op("dve", lambda e: e.memset(ones64[:], 1.0), writes=[ones64])
    c.op("dve", lambda e: e.tensor_tensor_scan(out=pend[:], data0=ones64[:], data1=padded[:], initial=0.0, op0=ALU.mult, op1=ALU.add),
         reads=[ones64, padded], writes=[pend])
    tt(c, "dve", pstart, pstart[:], pend, pend[:], padded, padded[:], ALU.subtract)
    DST = c.sb("DST", [128, 64], F32, st)
    DSTi = c.sb("DSTi", [128, 64], I32, st)
    junk = c.sb("junk3", [128, 64], F32, st)
    for b in range(32):
        tt(c, "dve", f1, f1[:], RK, RK[:, b, :], pstart, pstart[:], ALU.add)
        stt(c, junk, junk[:], f1, f1[:], 1.0, S1all, S1all[:, b, :], ALU.mult, ALU.mult, extra_w=[DST], accum_out=DST[:, 2 * b:2 * b + 1])
        stt(c, junk, junk[:], f1, f1[:], 1.0, S2all, S2all[:, b, :], ALU.mult, ALU.mult, extra_w=[DST], accum_out=DST[:, 2 * b + 1:2 * b + 2])
    cp(c, "dve", DSTi, DSTi[:], DST, DST[:])
    u2r_rot = Rot(c, "u2r", [128, D], BF16, 2, st)
    for b in range(32):
        u2r = u2r_rot.get()
        c.dma("sp", lambda e, b=b, u2r=u2r: e.dma_start(out=u2r[:], in_=u2_d.t[b * 128:(b + 1) * 128, :]), reads=[u2_d], writes=[u2r])
        for sl in range(2):
            c.dma("pool", lambda e, b=b, sl=sl, u2r=u2r: e.indirect_dma_start(
                out=xpad_d.t[:, :], out_offset=bass.IndirectOffsetOnAxis(ap=DSTi[:, 2 * b + sl:2 * b + sl + 1], axis=0),
                in_=u2r[:], in_offset=None), reads=[u2r, DSTi], writes=[xpad_d])
    cmp = c.sb("cmp3", [128, 128, 64], F32, st)
    tt(c, "dve", cmp, cmp[:], pend, pend[:].unsqueeze(1).to_broadcast([128, 128, 64]),
       thr, thr[:].unsqueeze(2).to_broadcast([128, 128, 64]), ALU.is_le)
    BE = c.sb("BE", [128, 128], F32, st)
    c.op("dve", lambda e: e.tensor_reduce(out=BE[:], in_=cmp[:], axis=mybir.AxisListType.X, op=ALU.add), reads=[cmp], writes=[BE])
    ts(c, "dve", BE, BE[:], BE, BE[:], 63.0, 128.0, ALU.min, ALU.mult)
    ts(c, "dve", BE, BE[:], BE, BE[:], pidx[:, 0:1], None, ALU.add, extra_r=[pidx])
    WIDX = c.sb("WIDX", [128, 128], I32, st)
    cp(c, "dve", WIDX, WIDX[:], BE, BE[:])
    c.flush()
    Wc_rot = Rot(c, "Wcat", [128, 6144], BF16, 4, st)
    wcat_d = G["wcat_d"]
    xr_rot = Rot(c, "xrow", [128, D], BF16, 4, st)
    LM = [dict(xT=c.sb("xT3", [128, 8, 128], BF16, st), sg=c.sb("sg3", [128, 256], F32, st), hg=c.sb("hg3", [128, 256], BF16, st),
               hgT=c.sb("hgT3", [128, 2, 128], BF16, st), p1=c.ps("pH1", [128, 512], F32, st), p3=c.ps("pH3", [128, 512], F32, st),
               py=pA_rot.tiles[ln_], pT=c.ps("pT3", [128, 8, 128], BF16, st)) for ln_ in range(2)]
    yrow_rot = Rot(c, "yrow", [128, D], F32, 2, st)
    stg = {}

    def fetch(i):
        Wc, xr = Wc_rot.get(), xr_rot.get()
        c.dma("pool", lambda e, Wc=Wc, i=i: e.indirect_dma_start(
            out=Wc[:], out_offset=None, in_=wcat_d.t[:, :],
            in_offset=bass.IndirectOffsetOnAxis(ap=WIDX[:, i:i + 1], axis=0)), reads=[WIDX, wcat_d], writes=[Wc])
        c.dma("sp", lambda e, xr=xr, i=i: e.dma_start(out=xr[:], in_=xpad_d.t[i * 128:(i + 1) * 128, :]), reads=[xpad_d], writes=[xr])
        stg[i] = (Wc, xr)

    def moe_block(i, L):
        xT, sg, hg, hgT, p1, p3, py, pT = L["xT"], L["sg"], L["hg"], L["hgT"], L["p1"], L["p3"], L["py"], L["pT"]
        if i + 2 < NBLK:
            fetch(i + 2)
        Wc, xr = stg.pop(i)
        for k in range(8):
            tr(c, pT, pT[:, k, :], xr, xr[:, k * 128:(k + 1) * 128], ident, ident[:])
        yield
        cp(c, "dve", xT, xT[:], pT, pT[:])
        yield
        for k in range(8):
            mm(c, p1, p1[:, 0:256], xT, xT[:, k, :], Wc, Wc[:, k * 256:(k + 1) * 256], k == 0, k == 7)
        yield
        for k in range(8):
            mm(c, p3, p3[:, 0:256], xT, xT[:, k, :], Wc, Wc[:, 2048 + k * 256:2048 + (k + 1) * 256], k == 0, k == 7)
        act(c, sg, sg[:], p1, p1[:, 0:256], AF.Sigmoid)
        yield
        tt(c, "dve", sg, sg[:], p1, p1[:, 0:256], sg, sg[:], ALU.mult)
        tt(c, "dve", hg, hg[:], p3, p3[:, 0:256], sg, sg[:], ALU.mult)
        yield
        for kc in range(2):
            tr(c, pT, pT[:, kc, :], hg, hg[:, kc * 128:(kc + 1) * 128], ident, ident[:])
        yield
        cp(c, "dve", hgT, hgT[:], pT, pT[:, 0:2, :])
        yield
        yrow = yrow_rot.get()
        for n in range(2):
            for kc in range(2):
                mm(c, py, py[:], hgT, hgT[:, kc, :], Wc, Wc[:, 4096 + kc * 1024 + n * 512:4096 + kc * 1024 + (n + 1) * 512], kc == 0, kc == 1)
            yield
            cp(c, "act" if n == 0 else "dve", yrow, yrow[:, n * 512:(n + 1) * 512], py, py[:])
            yield
        c.dma("sp", lambda e, i=i, yrow=yrow: e.dma_start(out=ypad_d.t[i * 128:(i + 1) * 128, :], in_=yrow[:]), reads=[yrow], writes=[ypad_d])
        yield

    fetch(0)
    fetch(1)
    for i0 in range(0, NBLK, 16):
        run_lanes([chain_gens([moe_block(i, LM[ln]) for i in range(i0 + ln, i0 + 16, 2)]) for ln in range(2)])
        c.flush()
    y1_rot = Rot(c, "y1", [128, D], F32, 2, st)
    y2_rot = Rot(c, "y2", [128, D], F32, 2, st)
    hr_rot = Rot(c, "hr3", [128, D], F32, 2, st)
    for b in range(32):
        y1, y2, hr = y1_rot.get(), y2_rot.get(), hr_rot.get()
        for (dst, sl) in ((y1, 0), (y2, 1)):
            c.dma("pool", lambda e, dst=dst, sl=sl, b=b: e.indirect_dma_start(
                out=dst[:], out_offset=None, in_=ypad_d.t[:, :],
                in_offset=bass.IndirectOffsetOnAxis(ap=DSTi[:, 2 * b + sl:2 * b + sl + 1], axis=0)), reads=[DSTi, ypad_d], writes=[dst])
        c.dma("sp", lambda e, b=b, hr=hr: e.dma_start(out=hr[:], in_=h_d.t[b * 128:(b + 1) * 128, :]), reads=[h_d], writes=[hr])
        stt(c, hr, hr[:], y1, y1[:], GT[:, b, 0:1], hr, hr[:], ALU.mult, ALU.add, extra_r=[GT])
        stt(c, hr, hr[:], y2, y2[:], GT[:, b, 1:2], hr, hr[:], ALU.mult, ALU.add, extra_r=[GT])
        c.dma("sp", lambda e, b=b, hr=hr: e.dma_start(out=out.t[b * 128:(b + 1) * 128, :], in_=hr[:]), reads=[hr], writes=[out])
    c.flush()


def phase3_dense(c, st, G):
    ident, h_d, u2T_d, Gall, out = G["ident"], G["h_d"], G["u2T_d"], G["Gall"], G["out"]
    w1r, w3r, w2r = G["w1r"], G["w3r"], G["w2r"]
    NB = 16
    yacc = c.sb("yacc", [128, NB, D], F32, st)
    u2T = c.sb("u2Ta", [128, 8, NB * 128], BF16, st)
    W13_rot = Rot(c, "W13", [128, 8, 512], BF16, 2, st)
    W2_rot = Rot(c, "W2", [128, 2, 1024], BF16, 2, st)
    pH_rot = Rot(c, "pH", [128, 512], F32, 2, st, psum=True)
    pY_rot = Rot(c, "pY", [128, 512], F32, 4, st, psum=True)
    pT_rot = Rot(c, "pT", [128, 8, 128], BF16, 2, st, psum=True)
    sg = c.sb("sg3", [128, 256], F32, st)
    hg = c.sb("hg", [128, 256], BF16, st)
    hgT = c.sb("hgT", [128, 2, 128], BF16, st)
    hrow = c.sb("hrow3", [128, D], F32, st)
    for half in range(2):
        for j in range(4):
            c.dma("sp", lambda e, j=j, half=half: e.dma_start(
                out=u2T[:, :, j * 512:(j + 1) * 512], in_=u2T_d.t[half * 4 + j].rearrange("p (a b) -> p a b", b=512)),
                reads=[u2T_d], writes=[u2T])
        for ex in range(64):
            W13 = W13_rot.get()
            W2 = W2_rot.get()
            c.dma("pool", lambda e, ex=ex, W13=W13: e.dma_start(
                out=W13[:, :, 0:256], in_=w1r.t[ex * 128:(ex + 1) * 128, :].rearrange("p (a b) -> p a b", b=256)), writes=[W13])
            c.dma("pool", lambda e, ex=ex, W13=W13: e.dma_start(
                out=W13[:, :, 256:512], in_=w3r.t[ex * 128:(ex + 1) * 128, :].rearrange("p (a b) -> p a b", b=256)), writes=[W13])
            c.dma("pool", lambda e, ex=ex, W2=W2: e.dma_start(
                out=W2[:].rearrange("p a b -> p (a b)"), in_=w2r.t[ex * 128:(ex + 1) * 128, :]), writes=[W2])
            for b in range(NB):
                blk = half * NB + b
                ph = pH_rot.get()
                for k in range(8):
                    mm(c, ph, ph[:], u2T, u2T[:, k, b * 128:(b + 1) * 128], W13, W13[:, k, :], k == 0, k == 7)
                act(c, sg, sg[:], ph, ph[:, 0:256], AF.Sigmoid)
                tt(c, "dve", sg, sg[:], ph, ph[:, 0:256], sg, sg[:], ALU.mult)
                tt(c, "dve", sg, sg[:], ph, ph[:, 256:512], sg, sg[:], ALU.mult)
                ts(c, "dve", hg, hg[:], sg, sg[:], Gall[:, blk, ex:ex + 1], None, ALU.mult, extra_r=[Gall])
                pT = pT_rot.get()
                for kc in range(2):
                    tr(c, pT, pT[:, kc, :], hg, hg[:, kc * 128:(kc + 1) * 128], ident, ident[:])
                cp(c, "dve", hgT, hgT[:], pT, pT[:, 0:2, :])
                for n in range(2):
                    py = pY_rot.get()
                    for kc in range(2):
                        mm(c, py, py[:], hgT, hgT[:, kc, :], W2, W2[:, kc, n * 512:(n + 1) * 512], kc == 0, kc == 1)
                    if ex == 0:
                        cp(c, "act", yacc, yacc[:, b, n * 512:(n + 1) * 512], py, py[:])
                    else:
                        tt(c, "dve", yacc, yacc[:, b, n * 512:(n + 1) * 512], py, py[:], yacc, yacc[:, b, n * 512:(n + 1) * 512], ALU.add)
            if ex % 4 == 3:
                c.flush()
        for b in range(NB):
            blk = half * NB + b
            c.dma("sp", lambda e, blk=blk: e.dma_start(out=hrow[:], in_=h_d.t[blk * 128:(blk + 1) * 128, :]), reads=[h_d], writes=[hrow])
            tt(c, "dve", yacc, yacc[:, b, :], yacc, yacc[:, b, :], hrow, hrow[:], ALU.add)
            c.dma("sp", lambda e, blk=blk, b=b: e.dma_start(out=out.t[blk * 128:(blk + 1) * 128, :], in_=yacc[:, b, :]), reads=[yacc], writes=[out])
        c.flush()


def _prep_core(inp, cidx):
    b, j = cidx // 2, cidx % 2
    f = np.float32
    x = inp["x"]
    own = OWN[j]
    rep = lambda v, n=128: np.ascontiguousarray(np.broadcast_to(np.asarray(v, f).reshape(1, -1), (n, np.asarray(v).size)))
    colmaj = lambda v: np.ascontiguousarray(np.asarray(v, f).reshape(8, 128).T)
    m = {}
    m["xs"] = np.ascontiguousarray(x[b])
    m["xo"] = np.ascontiguousarray(np.concatenate([x[b, u * UT:(u + 1) * UT] for u in own], 0))
    pos = np.asarray(inp["positions"][b], np.int32)
    m["pos_s"] = np.ascontiguousarray(pos.reshape(64, 128).T)
    pown = np.concatenate([pos[u * UT:(u + 1) * UT] for u in own])
    m["pos_o"] = np.ascontiguousarray(pown.reshape(32, 128).T)
    invf = (10000.0 ** (-np.arange(0, 64, 2, dtype=np.float32) / 64)).astype(f)
    m["invf"] = rep(invf)
    m["ident"] = np.eye(128, dtype=f)
    m["g1"] = rep(inp["norm1_g"][0])
    m["w_in"] = np.ascontiguousarray(inp["w_in"][0])
    m["convw"] = np.ascontiguousarray(np.asarray(inp["conv_w"][0], f).reshape(4, 8, 128).transpose(2, 1, 0))
    m["convb"] = colmaj(inp["conv_b"][0])
    m["lru_wa"] = np.ascontiguousarray(inp["lru_wa"][0])
    m["lru_wx"] = np.ascontiguousarray(inp["lru_wx"][0])
    m["lru_ba"] = colmaj(inp["lru_ba"][0])
    m["lru_bx"] = colmaj(inp["lru_bx"][0])
    m["lru_lam"] = colmaj(inp["lru_lambda"][0])
    m["w_rnn_o"] = np.ascontiguousarray(inp["w_rnn_o"][0])
    m["gq"] = rep(inp["q_norm_g"][0])
    m["w_uq"] = np.ascontiguousarray(inp["w_uq"][0])
    m["gkv"] = rep(inp["kv_norm_g"][0])
    m["w_ukv"] = np.ascontiguousarray(inp["w_ukv"][0])
    m["gqk_q"] = rep(inp["qk_norm_q_g"][0])
    gk = np.asarray(inp["qk_norm_k_g"][0], f)
    m["gqk_k_pe"] = rep(gk[128:192])
    m["gqk_k_col"] = np.ascontiguousarray(gk[0:128].reshape(128, 1))
    m["w_mla_o"] = np.ascontiguousarray(inp["w_mla_o"][0])
    m["w_out"] = np.ascontiguousarray(inp["w_out"][0])
    m["g2"] = rep(inp["norm2_g"][0])
    m["wr"] = np.ascontiguousarray(np.concatenate([inp["router_wg"][0], inp["router_we"][0]], 1))
    m["br"] = rep(np.concatenate([inp["router_bg"][0], inp["router_be"][0]]))
    m["w1r"] = np.ascontiguousarray(np.asarray(inp["exp_w1"][0], f).reshape(64, 8, 128, 256).transpose(0, 2, 1, 3).reshape(64 * 128, 2048))
    m["w3r"] = np.ascontiguousarray(np.asarray(inp["exp_w3"][0], f).reshape(64, 8, 128, 256).transpose(0, 2, 1, 3).reshape(64 * 128, 2048))
    m["w2r"] = np.ascontiguousarray(np.asarray(inp["exp_w2"][0], f).reshape(64, 2, 128, 1024).transpose(0, 2, 1, 3).reshape(64 * 128, 2048))
    kk = (np.arange(4)[None, :, None] * 128 + np.arange(128)[:, None, None])
    qq = np.arange(512)[None, None, :]
    diag = (kk <= qq).astype(f)
    full = np.ones_like(diag)
    zero = np.zeros_like(diag)
    mk = np.zeros((NOWN, 2, 128, 4, 512), f)
    for i in range(NOWN):
        for w in range(2):
            ku = EXT[i] - 2 + w
            mk[i, w] = full if ku < own[i] else (diag if ku == own[i] else zero)
    m["masks"] = mk.reshape(NOWN, 2, 128, 2048).astype(ml_dtypes.bfloat16)
    hid = np.zeros((128, 64), np.int32)
    for i in range(NOWN):
        for ch in range(8):
            hid[:, i * 8 + ch] = own[i] * 1024 + ch * 128 + np.arange(128)
    m["hidx"] = hid
    m["pidx"] = np.arange(128, dtype=f).reshape(128, 1)
    m["ustrict"] = np.triu(np.ones((128, 128), f), 1)
    m["thr"] = np.ascontiguousarray(np.broadcast_to((np.arange(128, dtype=f) * 128).reshape(1, 128), (128, 128)))
    return m


_NC_CACHE = {}


def kernel(**inputs):
    inp = {k: np.asarray(v) for k, v in inputs.items()}
    if "nc" not in _NC_CACHE:
        _NC_CACHE["nc"] = build()
    nc = _NC_CACHE["nc"]
    in_maps = [_prep_core(inp, cidx) for cidx in range(8)]
    res = run_bass_kernel_spmd(nc, in_maps, core_ids=list(range(8)))
    outp = np.zeros((4, S, D), np.float32)
    for cidx in range(8):
        b, j = cidx // 2, cidx % 2
        o = res.results[cidx]["out"]
        for i, u in enumerate(OWN[j]):
            outp[b, u * UT:(u + 1) * UT] = o[i * UT:(i + 1) * UT]
    return outp
```

```python
import math
import numpy as np
from contextlib import ExitStack
import ml_dtypes
import concourse.bass as bass
import concourse.mybir as mybir
from concourse.bass_utils import run_bass_kernel_spmd

F32 = mybir.dt.float32
BF16 = mybir.dt.bfloat16
I32 = mybir.dt.int32
AF = mybir.ActivationFunctionType
ALU = mybir.AluOpType

D = 1024
S = 8192
NU = 16
UT = 512
NOWN = 8
H = 8
EPS = 1e-6
OWN = {0: [0, 3, 4, 7, 8, 11, 12, 15], 1: [1, 2, 5, 6, 9, 10, 13, 14]}
EXT = [4 * (i // 2) + (2 if i % 2 == 0 else 4) for i in range(NOWN)]
MPAD = 16384
NBLK = MPAD // 128
TWO_PI = 6.283185
QSCALE = 192.0 ** -0.5


class T:
    __slots__ = ("t", "name", "writes", "reads")

    def __init__(self, t, name=""):
        self.t = t
        self.name = name
        self.writes = {}
        self.reads = {}

    def __getitem__(self, idx):
        return self.t[idx]


class Ctx:
    ENG = ("pe", "act", "dve", "pool", "sp")

    def __init__(self, nc, stack, block, n_dma_sems=10):
        self.nc = nc
        self.stack = stack
        self.block = block
        self.cnt = {}
        self.semobj = {}
        for n in self.ENG:
            self.semobj["e_" + n] = stack.enter_context(nc.semaphore("s_" + n))
            self.cnt["e_" + n] = 0
        self.dq = {}
        for q in ("sp", "act", "pool"):
            lst = []
            for i in range(n_dma_sems):
                key = "d_%s_%d" % (q, i)
                self.semobj[key] = stack.enter_context(nc.semaphore(key))
                self.cnt[key] = 0
                lst.append(key)
            self.dq[q] = [lst, 0]
        self.seen = {n: {} for n in self.ENG}
        self.prog = {n: [] for n in self.ENG}
        self.ninst = 0

    def flush(self):
        b = self.block
        starters = {"pe": b.tensor, "act": b.scalar, "dve": b.vector, "pool": b.gpsimd, "sp": b.sync}
        for n in self.ENG:
            lst = self.prog[n]
            if not lst:
                continue

            def body(eng, lst=lst):
                for f in lst:
                    f(eng)
            starters[n](body)
            self.prog[n] = []

    def barrier(self):
        toks = dict(self.cnt)
        for n in self.ENG:
            self._need(n, {k: v for k, v in toks.items() if v > 0}, force_own=False)

    def sb(self, name, shape, dt, stack=None):
        self.uid = getattr(self, "uid", 0) + 1
        name = "%s_%d" % (name, self.uid)
        return T((stack or self.stack).enter_context(self.nc.sbuf_tensor(name, list(shape), dt)), name)

    def ps(self, name, shape, dt=F32, stack=None):
        self.uid = getattr(self, "uid", 0) + 1
        name = "%s_%d" % (name, self.uid)
        return T((stack or self.stack).enter_context(self.nc.psum_tensor(name, list(shape), dt)), name)

    def _need(self, eng, toks, force_own=True):
        seen = self.seen[eng]
        own = "e_" + eng
        for k, v in toks.items():
            if k == own and (eng == "pe" or not force_own):
                continue
            if seen.get(k, 0) >= v:
                continue
            self.prog[eng].append(lambda e, s=self.semobj[k], v=v: e.wait_ge(s, v))
            seen[k] = v
            self.ninst += 1

    def _deps(self, eng, reads, writes):
        toks = {}
        for t in reads:
            for k, v in t.writes.items():
                if toks.get(k, 0) < v:
                    toks[k] = v
        for t in writes:
            for k, v in t.writes.items():
                if toks.get(k, 0) < v:
                    toks[k] = v
            for k, v in t.reads.items():
                if toks.get(k, 0) < v:
                    toks[k] = v
        self._need(eng, toks)

    def op(self, eng, fn, reads=(), writes=()):
        self._deps(eng, reads, writes)
        key = "e_" + eng
        self.cnt[key] += 1
        v = self.cnt[key]
        self.prog[eng].append(lambda e, fn=fn, s=self.semobj[key]: fn(e).then_inc(s, 1))
        for t in reads:
            t.reads[key] = v
        for t in writes:
            t.writes[key] = v
        self.ninst += 1

    def dma(self, q, fn, reads=(), writes=()):
        self._deps(q, reads, writes)
        lst, i = self.dq[q]
        key = lst[i % len(lst)]
        self.dq[q][1] = i + 1
        if self.cnt[key] > 0 and self.seen[q].get(key, 0) < self.cnt[key]:
            self.prog[q].append(lambda e, s=self.semobj[key], v=self.cnt[key]: e.wait_ge(s, v))
            self.seen[q][key] = self.cnt[key]
        self.cnt[key] += 16
        v = self.cnt[key]
        self.prog[q].append(lambda e, fn=fn, s=self.semobj[key]: fn(e).then_inc(s, 16))
        for t in reads:
            t.reads[key] = v
        for t in writes:
            t.writes[key] = v
        self.ninst += 1

    def wait_all(self, eng, tiles):
        toks = {}
        for t in tiles:
            for d in (t.writes, t.reads):
                for k, v in d.items():
                    if toks.get(k, 0) < v:
                        toks[k] = v
        self._need(eng, toks)


class Rot:
    def __init__(self, c, name, shape, dt, n, st, psum=False):
        self.tiles = [(c.ps if psum else c.sb)("%s%d" % (name, i), shape, dt, st) for i in range(n)]
        self.i = 0

    def get(self):
        t = self.tiles[self.i % len(self.tiles)]
        self.i += 1
        return t


def act(c, out_t, out_ap, in_t, in_ap, func, extra_r=(), extra_w=(), **kw):
    c.op("act", lambda e: e.activation(out=out_ap, in_=in_ap, func=func, **kw),
         reads=[in_t] + list(extra_r), writes=[out_t] + list(extra_w))


def ts(c, eng, out_t, out_ap, in_t, in_ap, s1, s2, op0, op1=None, extra_r=()):
    if op1 is None:
        c.op(eng, lambda e: e.tensor_scalar(out=out_ap, in0=in_ap, scalar1=s1, scalar2=None, op0=op0),
             reads=[in_t] + list(extra_r), writes=[out_t])
    else:
        c.op(eng, lambda e: e.tensor_scalar(out=out_ap, in0=in_ap, scalar1=s1, scalar2=s2, op0=op0, op1=op1),
             reads=[in_t] + list(extra_r), writes=[out_t])


def tt(c, eng, out_t, out_ap, a_t, a_ap, b_t, b_ap, op):
    c.op(eng, lambda e: e.tensor_tensor(out=out_ap, in0=a_ap, in1=b_ap, op=op), reads=[a_t, b_t], writes=[out_t])


def stt(c, out_t, out_ap, a_t, a_ap, scalar, b_t, b_ap, op0, op1, extra_r=(), extra_w=(), **kw):
    c.op("dve", lambda e: e.scalar_tensor_tensor(out=out_ap, in0=a_ap, scalar=scalar, in1=b_ap, op0=op0, op1=op1, **kw),
         reads=[a_t, b_t] + list(extra_r), writes=[out_t] + list(extra_w))


def cp(c, eng, out_t, out_ap, in_t, in_ap):
    if eng == "act":
        c.op("act", lambda e: e.copy(out=out_ap, in_=in_ap), reads=[in_t], writes=[out_t])
    else:
        c.op(eng, lambda e: e.tensor_copy(out=out_ap, in_=in_ap), reads=[in_t], writes=[out_t])


def mm(c, out_t, out_ap, l_t, l_ap, r_t, r_ap, start, stop):
    c.op("pe", lambda e: e.matmul(out_ap, lhsT=l_ap, rhs=r_ap, start=start, stop=stop, skip_group_check=True),
         reads=[l_t, r_t], writes=[out_t])


def tr(c, out_t, out_ap, in_t, in_ap, id_t, id_ap):
    c.op("pe", lambda e: e.transpose(out=out_ap, in_=in_ap, identity=id_ap), reads=[in_t, id_t], writes=[out_t])


def rstd_from_ss(c, ss_t, ss_ap, n, rms_t, rms_ap, rstd_t, rstd_ap):
    ts(c, "dve", rms_t, rms_ap, ss_t, ss_ap, 1.0 / n, EPS, ALU.mult, ALU.add)
    act(c, rms_t, rms_ap, rms_t, rms_ap, AF.Sqrt)
    c.op("dve", lambda e: e.reciprocal(out=rstd_ap, in_=rms_ap), reads=[rms_t], writes=[rstd_t])


def trig_tables(c, st, posf_t, nblk, invf_t, cos_t, sin_t):
    ang = c.sb("tg_ang", [128, nblk, 32], F32, st)
    ki = c.sb("tg_ki", [128, nblk, 32], I32, st)
    kf = c.sb("tg_kf", [128, nblk, 32], F32, st)
    fl = c.sb("tg_fl", [128, nblk, 32], F32, st)
    tt(c, "dve", ang, ang[:], posf_t, posf_t[:, 0:nblk].unsqueeze(2).to_broadcast([128, nblk, 32]),
       invf_t, invf_t[:, :].unsqueeze(1).to_broadcast([128, nblk, 32]), ALU.mult)
    for (dst, shift) in ((sin_t, 0.0), (cos_t, 0.25)):
        ts(c, "dve", kf, kf[:], ang, ang[:], 1.0 / (2 * math.pi), shift, ALU.mult, ALU.add)
        cp(c, "dve", ki, ki[:], kf, kf[:])
        cp(c, "dve", fl, fl[:], ki, ki[:])
        tt(c, "dve", kf, kf[:], kf, kf[:], fl, fl[:], ALU.subtract)
        ts(c, "dve", fl, fl[:], kf, kf[:], 0.5, None, ALU.is_gt)
        tt(c, "dve", kf, kf[:], kf, kf[:], fl, fl[:], ALU.subtract)
        ts(c, "dve", fl, fl[:], kf, kf[:], -0.5, None, ALU.is_lt)
        tt(c, "dve", kf, kf[:], kf, kf[:], fl, fl[:], ALU.add)
        act(c, dst, dst[:], kf, kf[:], AF.Sin, scale=TWO_PI)


def rope(c, eng, out_t, out_ap3, x_t, x_ap3, cos_t, cos_ap3, sin_t, sin_ap3, tmp_t, tmp_ap3, nh):
    x1, x2 = x_ap3[:, :, 0:32], x_ap3[:, :, 32:64]
    o1, o2 = out_ap3[:, :, 0:32], out_ap3[:, :, 32:64]
    t1 = tmp_ap3[:, :, 0:32]
    t2 = tmp_ap3[:, :, 32:64]
    tt(c, eng, tmp_t, t1, x_t, x2, sin_t, sin_ap3, ALU.mult)
    tt(c, eng, tmp_t, t2, x_t, x1, sin_t, sin_ap3, ALU.mult)
    tt(c, eng, out_t, o1, x_t, x1, cos_t, cos_ap3, ALU.mult)
    tt(c, eng, out_t, o2, x_t, x2, cos_t, cos_ap3, ALU.mult)
    tt(c, eng, out_t, o1, out_t, o1, tmp_t, t1, ALU.subtract)
    tt(c, eng, out_t, o2, out_t, o2, tmp_t, t2, ALU.add)


def norm_transpose(c, st, x_dram_ap, g1_t, ident_t, xt_rot, ub, uT, pT_rot, ssq, rms, rstd, junk, xt=None):
    if xt is None:
        xt = xt_rot.get()
        c.dma("sp", lambda e: e.dma_start(out=xt[:], in_=x_dram_ap), writes=[xt])
    for kb in range(4):
        act(c, junk, junk[:], xt, xt[:, kb, :], AF.Square, extra_w=[ssq], accum_out=ssq[:, kb:kb + 1])
    import os
    NT = int(os.environ.get("NT", "9"))
    rstd_from_ss(c, ssq, ssq[:, 0:4], D, rms, rms[:, 0:4], rstd, rstd[:, 0:4])
    for kb in range(4):
        if NT >= 1:
            stt(c, ub, ub[:, kb, :], xt, xt[:, kb, :], rstd[:, kb:kb + 1], g1_t, g1_t[:], ALU.mult, ALU.mult, extra_r=[rstd])
        pT = pT_rot.get()
        if NT >= 2:
            for k in range(8):
                tr(c, pT, pT[:, k, :], ub, ub[:, kb, k * 128:(k + 1) * 128], ident_t, ident_t[:])
        if NT >= 3:
            cp(c, "dve", uT, uT[:, :, kb * 128:(kb + 1) * 128], pT, pT[:])
    return xt


def build(dbg=None):
    nc = bass.Bass("TRN2", target_bir_lowering=False)
    dbg = dbg or {}
    last_phase = dbg.get("last_phase", 9)

    def din(name, shape, dt=F32):
        return T(nc.dram_tensor(name, list(shape), dt, kind="ExternalInput"), name)

    def dscr(name, shape, dt):
        kind = "ExternalOutput" if name in dbg.get("dump", ()) else "Internal"
        return T(nc.dram_tensor(name, list(shape), dt, kind=kind), name)

    xs = din("xs", [S, D])
    xo = din("xo", [NOWN * UT, D])
    pos_s = din("pos_s", [128, 64], I32)
    pos_o = din("pos_o", [128, 32], I32)
    invf = din("invf", [128, 32])
    ident_d = din("ident", [128, 128])
    g1 = din("g1", [128, D])
    w_in = din("w_in", [D, 4672])
    convw = din("convw", [128, 8, 4])
    convb = din("convb", [128, 8])
    lru_wa = din("lru_wa", [8, 128, 128])
    lru_wx = din("lru_wx", [8, 128, 128])
    lru_ba = din("lru_ba", [128, 8])
    lru_bx = din("lru_bx", [128, 8])
    lru_lam = din("lru_lam", [128, 8])
    w_rnn_o = din("w_rnn_o", [D, D])
    gq = din("gq", [128, 256])
    w_uq = din("w_uq", [256, 1536])
    gkv = din("gkv", [128, 256])
    w_ukv = din("w_ukv", [256, 2048])
    gqk_q = din("gqk_q", [128, 192])
    gqk_k_pe = din("gqk_k_pe", [128, 64])
    gqk_k_col = din("gqk_k_col", [128, 1])
    w_mla_o = din("w_mla_o", [D, D])
    w_out = din("w_out", [D, D])
    g2 = din("g2", [128, D])
    wr = din("wr", [D, 72])
    br = din("br", [128, 72])
    w1r = din("w1r", [64 * 128, 2048])
    w3r = din("w3r", [64 * 128, 2048])
    w2r = din("w2r", [64 * 128, 2048])
    masks = din("masks", [NOWN, 2, 128, 4 * 512], BF16)
    hidx = din("hidx", [128, 64], I32)
    pidx = din("pidx", [128, 1])
    ustrict = din("ustrict", [128, 128])
    thr = din("thr", [128, 128])
    out = T(nc.dram_tensor("out", [NOWN * UT, D], F32, kind="ExternalOutput"), "out")

    KT_d = dscr("KT_d", [H, 128, S], BF16)
    V_d = dscr("V_d", [H, NU, 128, 512], BF16)
    hT_d = dscr("hT_d", [NU * 1024, 512], BF16)
    sA_d = dscr("sA_d", [NOWN, 128, 8 * 512], BF16)
    OT_d = dscr("OT_d", [NOWN, 128, 8 * 512], BF16)
    h_d = dscr("h_d", [NOWN * UT, D], F32)
    u2_d = dscr("u2_d", [NOWN * UT, D], BF16)
    xpad_d = dscr("xpad_d", [MPAD, D], BF16)
    ypad_d = dscr("ypad_d", [MPAD, D], F32)
    wcat_d = dscr("wcat_d", [64 * 128, 6144], BF16)
    dbg_d = dscr("dbg_d", [128, 8192], F32)

    sk_dump = dscr("sk_dump", [128, 512], F32)
    with ExitStack() as gst:
        c = Ctx(nc, gst, None)
        ident = c.sb("identb", [128, 128], BF16)
        identf = c.sb("identf", [128, 128], F32)
        KP = c.sb("KP", [64, S], BF16)
        SK = c.sb("SK", [128, 64, 8], F32)
        c.dma("pool", lambda e: e.dma_start(out=ident[:], in_=ident_d[:, :]), writes=[ident])
        c.dma("sp", lambda e: e.dma_start(out=identf[:], in_=ident_d[:, :]), writes=[identf])

        block = gst.enter_context(nc.Block())
        c.block = block
        if False:
            for (src, dst) in ((w1r, w1b_d), (w3r, w3b_d), (w2r, w2b_d)):
                for i in range(16):
                    c.dma("pool", lambda e, src=src, dst=dst, i=i: e.dma_start(
                        out=dst[i * 512:(i + 1) * 512, :], in_=src[i * 512:(i + 1) * 512, :]),
                        reads=[src], writes=[dst])

        if last_phase >= 1:
            with ExitStack() as st:
                phase1(c, st, locals())
                c.barrier()
                c.flush()
        S1all = c.sb("S1all", [128, 32, 64], F32)
        S2all = c.sb("S2all", [128, 32, 64], F32)
        GT = c.sb("GT", [128, 32, 2], F32)
        G_ = dict(locals())
        for (pn, fn) in ((2, phase2a), (3, phase2b), (4, phase2c), (5, phase3)):
            if last_phase >= pn:
                with ExitStack() as st:
                    fn(c, st, G_)
                    c.barrier()
                    c.flush()
        c.wait_all("sp", [out, KT_d, V_d, hT_d, dbg_d, sk_dump])
        c.flush()
    return nc


def phase1(c, st, G):
    xs, w_in, ident, KP, SK = G["xs"], G["w_in"], G["ident"], G["KP"], G["SK"]
    KT_d, V_d, hT_d = G["KT_d"], G["V_d"], G["hT_d"]
    Wxr = c.sb("Wxr", [128, 8, 1024], BF16, st)
    Wkv = c.sb("Wkv", [128, 8, 320], BF16, st)
    Wa = c.sb("Wa", [128, 8, 128], BF16, st)
    Wx = c.sb("Wx", [128, 8, 128], BF16, st)
    Wukv = c.sb("Wukv", [128, 2, 2048], BF16, st)
    w_in_v = G["w_in"].t[:, :].rearrange("(k p) n -> p k n", p=128)
    for k in range(8):
        c.dma("pool", lambda e, k=k: e.dma_start(out=Wxr[:, k, :], in_=w_in_v[:, k, 0:1024]), writes=[Wxr])
    c.dma("pool", lambda e: e.dma_start(out=Wkv[:], in_=w_in_v[:, :, 2304:2624]), writes=[Wkv])
    c.dma("pool", lambda e: e.dma_start(out=Wa[:], in_=G["lru_wa"].t[:, :, :].rearrange("n k j -> k n j")), writes=[Wa])
    c.dma("pool", lambda e: e.dma_start(out=Wx[:], in_=G["lru_wx"].t[:, :, :].rearrange("n k j -> k n j")), writes=[Wx])
    for kc in range(2):
        c.dma("pool", lambda e, kc=kc: e.dma_start(out=Wukv[:, kc, :], in_=G["w_ukv"].t[kc * 128:(kc + 1) * 128, :]), writes=[Wukv])
    def small(name, src, shape, dt=F32):
        t = c.sb(name, shape, dt, st)
        c.dma("sp", lambda e: e.dma_start(out=t[:], in_=src.t[tuple(slice(None) for _ in shape)]), writes=[t])
        return t
    g1 = small("g1s", G["g1"], [128, D])
    cw = small("cw", G["convw"], [128, 8, 4])
    cb = small("cb", G["convb"], [128, 8])
    ba = small("ba", G["lru_ba"], [128, 8])
    bx = small("bx", G["lru_bx"], [128, 8])
    lam = small("lam", G["lru_lam"], [128, 8])
    gkv = small("gkvs", G["gkv"], [128, 256])
    gkpe = small("gkpe", G["gqk_k_pe"], [128, 64])
    gkcol = small("gkcol", G["gqk_k_col"], [128, 1])
    invf = small("invfs", G["invf"], [128, 32])
    posi = small("posi", G["pos_s"], [128, 64], I32)
    posf = c.sb("posf", [128, 64], F32, st)
    cp(c, "dve", posf, posf[:], posi, posi[:])
    cosT = c.sb("cosT", [128, 64, 32], F32, st)
    sinT = c.sb("sinT", [128, 64, 32], F32, st)
    with ExitStack() as st2:
        trig_tables(c, st2, posf, 64, invf, cosT, sinT)
        c.barrier()
        c.flush()
    cl = c.sb("cl", [128, 8], F32, st)
    act(c, cl, cl[:], lam, lam[:], AF.Exp, scale=-1.0)
    ts(c, "dve", cl, cl[:], cl, cl[:], 1.0, None, ALU.add)
    act(c, cl, cl[:], cl, cl[:], AF.Ln)
    ts(c, "dve", cl, cl[:], cl, cl[:], -8.0, None, ALU.mult)

    xt_rot = Rot(c, "xt", [128, 4, D], F32, 2, st)
    ub = c.sb("ub", [128, 4, D], BF16, st)
    uT = c.sb("uT", [128, 8, UT], BF16, st)
    junk = c.sb("junk", [128, D], F32, st)
    ssq = c.sb("ssq", [128, 4], F32, st)
    rms = c.sb("rms", [128, 4], F32, st)
    rstd = c.sb("rstd", [128, 4], F32, st)
    pT_rot = Rot(c, "pT", [128, 8, 128], BF16, 2, st, psum=True)
    KTs_rot = Rot(c, "KTs", [128, 8, UT], BF16, 1, st)
    Vs_rot = Rot(c, "Vs", [128, 8, 128], BF16, 2, st)
    sv = c.sb("sv", [128, 16], F32, st)
    SS0 = c.sb("SS0", [128, 8], F32, st)
    ckvg = c.sb("ckvg", [128, 256], BF16, st)
    ckvT = c.sb("ckvT", [128, 2, 128], BF16, st)
    kpg = c.sb("kpg", [128, 1, 64], F32, st)
    kpr = c.sb("kpr", [128, 1, 64], F32, st)
    kpt = c.sb("kpt", [128, 1, 64], F32, st)
    kpb = c.sb("kpb", [128, 64], BF16, st)
    k0b = c.sb("k0b", [128, 8, 128], BF16, st)

    hT_v = hT_d.t[:, :].rearrange("(u c p) t -> u p c t", c=8, p=128)
    LT = []
    for ln in range(2):
        LT.append(dict(
            xc=c.sb("xcL", [128, UT], F32, st), xcb=c.sb("xcbL", [128, UT], BF16, st), rr=c.sb("rrL", [128, UT], F32, st),
            ii=c.sb("iiL", [128, UT], F32, st), aa=c.sb("aaL", [128, UT], F32, st), mm_=c.sb("mmL", [128, UT], F32, st),
            bi=c.sb("biL", [128, UT], F32, st), hf=c.sb("hfL", [128, UT], F32, st),
            p0=c.ps("pL0", [128, 512], F32, st), p1=c.ps("pL1", [128, 512], F32, st)))
    xrs = [c.sb("xrc", [128, UT + 3], F32, st) for _ in range(8)]
    carries = [c.sb("carryc", [128, 1], F32, st) for _ in range(8)]
    hTbs = [c.sb("hTbc", [128, UT], BF16, st) for _ in range(8)]
    for t_ in xrs + carries:
        c.op("dve", lambda e, t_=t_: e.memset(t_[:], 0.0), writes=[t_])
    pKV = [c.ps("pKV0", [128, 512], F32, st), c.ps("pKV1", [128, 512], F32, st)]
    junk2 = c.sb("junk2", [128, 320], F32, st)

    def rnn_chunk(ch, u, L):
        xc, xcb, rr, ii, aa, mm_, bi, hf = L["xc"], L["xcb"], L["rr"], L["ii"], L["aa"], L["mm_"], L["bi"], L["hf"]
        pa, pb = L["p0"], L["p1"]
        xr, carry, hTb = xrs[ch], carries[ch], hTbs[ch]
        for k in range(8):
            mm(c, pa, pa[:], Wxr, Wxr[:, k, ch * 128:(ch + 1) * 128], uT, uT[:, k, :], k == 0, k == 7)
        if u > 0:
            cp(c, "pool", xr, xr[:, 0:3], xr, xr[:, UT:UT + 3])
        yield
        cp(c, "act", xr, xr[:, 3:UT + 3], pa, pa[:])
        yield
        ts(c, "dve", xc, xc[:], xr, xr[:, 0:UT], cw[:, ch, 0:1], cb[:, ch:ch + 1], ALU.mult, ALU.add, extra_r=[cw, cb])
        for k in range(1, 4):
            stt(c, xc, xc[:], xr, xr[:, k:k + UT], cw[:, ch, k:k + 1], xc, xc[:], ALU.mult, ALU.add, extra_r=[cw])
        yield
        cp(c, "pool", xcb, xcb[:], xc, xc[:])
        yield
        mm(c, pa, pa[:], Wa, Wa[:, ch, :], xcb, xcb[:], True, True)
        mm(c, pb, pb[:], Wx, Wx[:, ch, :], xcb, xcb[:], True, True)
        yield
        act(c, rr, rr[:], pa, pa[:], AF.Sigmoid, extra_r=[ba], bias=ba[:, ch:ch + 1])
        act(c, ii, ii[:], pb, pb[:], AF.Sigmoid, extra_r=[bx], bias=bx[:, ch:ch + 1])
        yield
        act(c, aa, aa[:], rr, rr[:], AF.Exp, extra_r=[cl], scale=cl[:, ch:ch + 1])
        tt(c, "dve", bi, bi[:], xc, xc[:], ii, ii[:], ALU.mult)
        yield
        tt(c, "pool", mm_, mm_[:], aa, aa[:], aa, aa[:], ALU.mult)
        ts(c, "pool", mm_, mm_[:], mm_, mm_[:], -1.0, 1.0, ALU.mult, ALU.add)
        yield
        act(c, mm_, mm_[:], mm_, mm_[:], AF.Sqrt)
        yield
        tt(c, "dve", bi, bi[:], bi, bi[:], mm_, mm_[:], ALU.mult)
        c.op("dve", lambda e: e.tensor_tensor_scan(out=hf[:], data0=aa[:], data1=bi[:], initial=carry[:, 0:1],
                                                   op0=ALU.mult, op1=ALU.add), reads=[aa, bi, carry], writes=[hf])
        cp(c, "dve", carry, carry[:, 0:1], hf, hf[:, UT - 1:UT])
        yield
        cp(c, "act", hTb, hTb[:], hf, hf[:])
        c.dma("sp", lambda e: e.dma_start(out=hT_v[u][:, ch, :], in_=hTb[:]), reads=[hTb], writes=[hT_d])
        yield

    def kv_block(kb, u, KTs):
        blk = u * 4 + kb
        pc = pKV[0]
        for k in range(8):
            mm(c, pc, pc[:, 0:320], uT, uT[:, k, kb * 128:(kb + 1) * 128], Wkv, Wkv[:, k, :], k == 0, k == 7)
        yield
        act(c, junk2, junk2[:, 0:256], pc, pc[:, 0:256], AF.Square, extra_w=[sv], accum_out=sv[:, 0:1])
        act(c, junk2, junk2[:, 256:320], pc, pc[:, 256:320], AF.Square, extra_w=[sv], accum_out=sv[:, 1:2])
        yield
        rstd_from_ss(c, sv, sv[:, 0:1], 256, sv, sv[:, 2:3], sv, sv[:, 3:4])
        tt(c, "dve", ckvg, ckvg[:], pc, pc[:, 0:256], gkv, gkv[:], ALU.mult)
        tt(c, "dve", kpg, kpg[:, 0, :], pc, pc[:, 256:320], gkpe, gkpe[:], ALU.mult)
        yield
        pT = pT_rot.get()
        for kc in range(2):
            tr(c, pT, pT[:, kc, :], ckvg, ckvg[:, kc * 128:(kc + 1) * 128], ident, ident[:])
        yield
        cp(c, "dve", ckvT, ckvT[:], pT, pT[:, 0:2, :])
        rope(c, "pool", kpr, kpr[:], kpg, kpg[:], cosT, cosT[:, blk:blk + 1, :], sinT, sinT[:, blk:blk + 1, :], kpt, kpt[:], 1)
        yield
        ts(c, "dve", kpb, kpb[:], kpr, kpr[:, 0, :], sv[:, 2:3], None, ALU.mult, extra_r=[sv])
        pT2 = pT_rot.get()
        tr(c, pT2, pT2[0:64, 0, :], kpb, kpb[:], ident, ident[:])
        yield
        cp(c, "act", KP, KP[:, blk * 128:(blk + 1) * 128], pT2, pT2[0:64, 0, :])
        Vs = Vs_rot.get()
        for n in range(4):
            pk = pKV[(n + 1) % 2]
            for kc in range(2):
                mm(c, pk, pk[:], ckvT, ckvT[:, kc, :], Wukv, Wukv[:, kc, n * 512:(n + 1) * 512], kc == 0, kc == 1)
            yield
            for hh in range(2):
                h = n * 2 + hh
                act(c, junk2, junk2[:, 0:128], pk, pk[:, hh * 256:hh * 256 + 128], AF.Square, extra_w=[SS0], accum_out=SS0[:, h:h + 1])
                cp(c, "act", k0b, k0b[:, h, :], pk, pk[:, hh * 256:hh * 256 + 128])
                act(c, Vs, Vs[:, h, :], pk, pk[:, hh * 256 + 128:hh * 256 + 256], AF.Copy, extra_r=[sv], scale=sv[:, 3:4])
            yield
        c.dma("sp", lambda e, Vs=Vs: e.dma_start(
            out=V_d.t[:, u, :, kb * 128:(kb + 1) * 128].rearrange("h p d -> p h d"), in_=Vs[:]), reads=[Vs], writes=[V_d])
        pT3 = pT_rot.get()
        for h in range(8):
            tr(c, pT3, pT3[:, h, :], k0b, k0b[:, h, :], ident, ident[:])
        yield
        for h in range(8):
            act(c, KTs, KTs[:, h, kb * 128:(kb + 1) * 128], pT3, pT3[:, h, :], AF.Copy, extra_r=[gkcol], scale=gkcol[:, 0:1])
        yield
        tt(c, "dve", sv, sv[:, 4:5], sv, sv[:, 3:4], sv, sv[:, 3:4], ALU.mult)
        ts(c, "dve", SS0, SS0[:], SS0, SS0[:], sv[:, 4:5], sv[:, 1:2], ALU.mult, ALU.add, extra_r=[sv])
        ts(c, "dve", SS0, SS0[:], SS0, SS0[:], 1.0 / 192, EPS, ALU.mult, ALU.add)
        yield
        act(c, SS0, SS0[:], SS0, SS0[:], AF.Sqrt)
        yield
        c.op("dve", lambda e: e.reciprocal(out=SS0[:], in_=SS0[:]), reads=[SS0], writes=[SS0])
        ts(c, "dve", SK, SK[:, blk, :], SS0, SS0[:], sv[:, 3:4], QSCALE, ALU.mult, ALU.mult, extra_r=[sv])
        yield

    def chain(gens):
        for g in gens:
            yield from g

    for u in range(G["dbg"].get("nu", NU)):
        def load_x(uu):
            xt_ = xt_rot.get()
            src_ = xs.t[uu * UT:(uu + 1) * UT, :].rearrange("(kb p) d -> p kb d", p=128)
            c.dma("sp", lambda e: e.dma_start(out=xt_[:], in_=src_), writes=[xt_])
            return xt_
        xt_cur = load_x(0) if u == 0 else xt_nxt
        norm_transpose(c, st, None, g1, ident, xt_rot, ub, uT, pT_rot, ssq, rms, rstd, junk, xt=xt_cur)
        if u + 1 < G["dbg"].get("nu", NU):
            xt_nxt = load_x(u + 1)
        KTs = KTs_rot.get()
        lanes = [chain([rnn_chunk(ch, u, LT[0]) for ch in (0, 2, 4, 6)]),
                 chain([rnn_chunk(ch, u, LT[1]) for ch in (1, 3, 5, 7)]),
                 chain([kv_block(kb, u, KTs) for kb in range(4)])]
        while lanes:
            for g in list(lanes):
                try:
                    next(g)
                except StopIteration:
                    lanes.remove(g)
        c.dma("sp", lambda e, u=u, KTs=KTs: e.dma_start(
            out=KT_d.t[:, :, u * UT:(u + 1) * UT].rearrange("h p t -> p h t"), in_=KTs[:]), reads=[KTs], writes=[KT_d])
        if u == 0 and G["last_phase"] >= 5:
            for (src, col) in ((G["w1r"], 0), (G["w3r"], 2048), (G["w2r"], 4096)):
                for i in range(16):
                    c.dma("pool", lambda e, src=src, col=col, i=i: e.dma_start(
                        out=G["wcat_d"].t[i * 512:(i + 1) * 512, col:col + 2048], in_=src.t[i * 512:(i + 1) * 512, :]),
                        reads=[src], writes=[G["wcat_d"]])
        if u % 4 == 3:
            c.flush()
    if "dbg_d" in G["dbg"].get("dump", ()):
        dbt = c.sb("dbt", [128, 8192], F32, st)
        c.op("dve", lambda e: e.memset(dbt[:], 0.0), writes=[dbt])
        cp(c, "dve", dbt, dbt[0:64, :], KP, KP[:, :])
        c.dma("sp", lambda e: e.dma_start(out=G["dbg_d"].t[:, :], in_=dbt[:]), reads=[dbt], writes=[G["dbg_d"]])
        sk_d = G["sk_dump"]
        c.dma("sp", lambda e: e.dma_start(out=sk_d.t[:, :], in_=SK[:].rearrange("p a b -> p (a b)")), reads=[SK], writes=[sk_d])


def run_lanes(lanes):
    lanes = list(lanes)
    while lanes:
        for g in list(lanes):
            try:
                next(g)
            except StopIteration:
                lanes.remove(g)


def chain_gens(gens):
    for g in gens:
        yield from g


def load_w(c, st, name, src_ap_fn, nk, ncols):
    t = c.sb(name, [128, nk, ncols], BF16, st)
    for k in range(nk):
        c.dma("pool", lambda e, k=k: e.dma_start(out=t[:, k, :], in_=src_ap_fn(k)), writes=[t])
    return t


def small_t(c, st, name, src, shape, dt=F32):
    t = c.sb(name, shape, dt, st)
    c.dma("sp", lambda e: e.dma_start(out=t[:], in_=src.t[tuple(slice(None) for _ in shape)]), writes=[t])
    return t


def norm_tiles(c, st):
    return dict(xt_rot=Rot(c, "xt", [128, 4, D], F32, 1, st), ub=c.sb("ub", [128, 4, D], BF16, st),
                uT=c.sb("uT", [128, 8, UT], BF16, st), junk=c.sb("junk", [128, D], F32, st),
                ssq=c.sb("ssq", [128, 4], F32, st), rms=c.sb("rms", [128, 4], F32, st), rstd=c.sb("rstd", [128, 4], F32, st))


def do_norm(c, st, N, xsrc, g1, ident, pT_rot):
    return norm_transpose(c, st, xsrc, g1, ident, N["xt_rot"], N["ub"], N["uT"], pT_rot, N["ssq"], N["rms"], N["rstd"], N["junk"])


def phase2a(c, st, G):
    xo, ident, hT_d, sA_d = G["xo"], G["ident"], G["hT_d"], G["sA_d"]
    w_in_v = G["w_in"].t[:, :].rearrange("(k p) n -> p k n", p=128)
    wro_v = G["w_rnn_o"].t[:, :].rearrange("(k p) n -> p k n", p=128)
    Wy = load_w(c, st, "Wy", lambda k: w_in_v[:, k, 1024:2048], 8, 1024)
    Wga = load_w(c, st, "Wga", lambda k: w_in_v[:, k, 2624:3648], 8, 1024)
    Wro = load_w(c, st, "Wro", lambda k: wro_v[:, k, :], 8, 1024)
    g1 = small_t(c, st, "g1s", G["g1"], [128, D])
    hidx = small_t(c, st, "hidx", G["hidx"], [128, 64], I32)
    N = norm_tiles(c, st)
    uT = N["uT"]
    pT_rot = Rot(c, "pT", [128, 8, 128], BF16, 2, st, psum=True)
    pA_rot = Rot(c, "pA", [128, 512], F32, 4, st, psum=True)
    hs = c.sb("hs", [128, 8, UT], BF16, st)
    LA = [dict(ys=c.sb("ys", [128, UT], F32, st), y2=c.sb("y2", [128, UT], F32, st), sg=c.sb("sg", [128, UT], F32, st),
               p0=pA_rot.tiles[2 * ln], p1=pA_rot.tiles[2 * ln + 1]) for ln in range(2)]
    zT = c.sb("zT", [128, 8, UT], BF16, st)
    sAT = c.sb("sAT", [128, 8, UT], BF16, st)
    for i in range(NOWN):
        xsrc = xo.t[i * UT:(i + 1) * UT, :].rearrange("(kb p) d -> p kb d", p=128)
        do_norm(c, st, N, xsrc, g1, ident, pT_rot)
        for ch in range(8):
            c.dma("pool", lambda e, ch=ch, i=i: e.indirect_dma_start(
                out=hs[:, ch, :], out_offset=None, in_=hT_d.t[:, :],
                in_offset=bass.IndirectOffsetOnAxis(ap=hidx[:, i * 8 + ch:i * 8 + ch + 1], axis=0)),
                reads=[hidx, hT_d], writes=[hs])
        def gelu_chunk(ch, L):
            ys, y2, sg, pa = L["ys"], L["y2"], L["sg"], L["p0"]
            for k in range(8):
                mm(c, pa, pa[:], Wy, Wy[:, k, ch * 128:(ch + 1) * 128], uT, uT[:, k, :], k == 0, k == 7)
            yield
            cp(c, "act", ys, ys[:], pa, pa[:])
            yield
            tt(c, "pool", y2, y2[:], ys, ys[:], ys, ys[:], ALU.mult)
            ts(c, "pool", y2, y2[:], y2, y2[:], 0.044715, 1.0, ALU.mult, ALU.add)
            tt(c, "pool", y2, y2[:], y2, y2[:], ys, ys[:], ALU.mult)
            yield
            act(c, sg, sg[:], y2, y2[:], AF.Sigmoid, scale=1.5957691216)
            yield
            tt(c, "dve", sg, sg[:], sg, sg[:], ys, ys[:], ALU.mult)
            tt(c, "dve", zT, zT[:, ch, :], sg, sg[:], hs, hs[:, ch, :], ALU.mult)
            yield

        def a_chunk(co, L):
            sg, pa, pg = L["sg"], L["p0"], L["p1"]
            for k in range(8):
                mm(c, pa, pa[:], Wro, Wro[:, k, co * 128:(co + 1) * 128], zT, zT[:, k, :], k == 0, k == 7)
            yield
            for k in range(8):
                mm(c, pg, pg[:], Wga, Wga[:, k, co * 128:(co + 1) * 128], uT, uT[:, k, :], k == 0, k == 7)
            yield
            act(c, sg, sg[:], pg, pg[:], AF.Sigmoid)
            yield
            tt(c, "dve", sAT, sAT[:, co, :], pa, pa[:], sg, sg[:], ALU.mult)
            yield

        run_lanes([chain_gens([gelu_chunk(ch, LA[ln]) for ch in range(ln, 8, 2)]) for ln in range(2)])
        run_lanes([chain_gens([a_chunk(co, LA[ln]) for co in range(ln, 8, 2)]) for ln in range(2)])
        c.dma("sp", lambda e, i=i: e.dma_start(out=sA_d.t[i], in_=sAT[:].rearrange("p a b -> p (a b)")), reads=[sAT], writes=[sA_d])
        c.flush()


def phase2b(c, st, G):
    xo, ident, KP, SK = G["xo"], G["ident"], G["KP"], G["SK"]
    KT_d, V_d, OT_d, masks = G["KT_d"], G["V_d"], G["OT_d"], G["masks"]
    w_in_v = G["w_in"].t[:, :].rearrange("(k p) n -> p k n", p=128)
    Wcq = load_w(c, st, "Wcq", lambda k: w_in_v[:, k, 2048:2304], 8, 256)
    Wuq = load_w(c, st, "Wuq", lambda k: G["w_uq"].t[k * 128:(k + 1) * 128, :], 2, 1536)
    g1 = small_t(c, st, "g1s", G["g1"], [128, D])
    gq = small_t(c, st, "gqs", G["gq"], [128, 256])
    gqk = small_t(c, st, "gqk", G["gqk_q"], [128, 192])
    invf = small_t(c, st, "invfs", G["invf"], [128, 32])
    posi = small_t(c, st, "posi", G["pos_o"], [128, 32], I32)
    posf = c.sb("posf", [128, 32], F32, st)
    cp(c, "dve", posf, posf[:], posi, posi[:])
    cosO = c.sb("cosO", [128, 32, 32], F32, st)
    sinO = c.sb("sinO", [128, 32, 32], F32, st)
    with ExitStack() as st2:
        trig_tables(c, st2, posf, 32, invf, cosO, sinO)
        c.barrier()
        c.flush()
    N = norm_tiles(c, st)
    uT = N["uT"]
    junk = N["junk"]
    pT_rot = Rot(c, "pT", [128, 8, 128], BF16, 1, st, psum=True)
    pS_rot = Rot(c, "pS", [128, 512], F32, 3, st, psum=True)
    pO = [c.ps("pO%d" % q, [128, 512], F32, st) for q in range(4)]
    onesb = c.sb("onesb2", [128, 128], BF16, st)
    c.op("dve", lambda e: e.memset(onesb[:], 1.0), writes=[onesb])
    rinv = c.sb("rinv", [128, 512], F32, st)
    sv = c.sb("sv", [128, 16], F32, st)
    SSQ = c.sb("SSQ", [128, 8], F32, st)
    FQ = c.sb("FQ", [128, 8], F32, st)
    cqg = c.sb("cqg", [128, 256], BF16, st)
    cqT = c.sb("cqT", [128, 2, 128], BF16, st)
    q0s = c.sb("q0s", [128, 8, 192], F32, st)
    qr = c.sb("qr", [128, 8, 64], F32, st)
    qtmp = c.sb("qtmp", [128, 8, 64], F32, st)
    qbn = c.sb("qbn", [128, 8, 128], BF16, st)
    qbp = c.sb("qbp", [128, 8, 64], BF16, st)
    QT = c.sb("QT", [128, 8, UT], BF16, st)
    QP = c.sb("QP", [64, 8, UT], BF16, st)
    msk = [c.sb("msk%d" % w, [128, 4, 512], BF16, st) for w in range(2)]
    KT_rot = Rot(c, "KTt", [128, 512], BF16, 3, st)
    V_rot = Rot(c, "Vt", [128, 4, 130], BF16, 3, st)
    for vt in V_rot.tiles:
        c.op("dve", lambda e, vt=vt: e.memset(vt[:], 1.0), writes=[vt])
    PT_rot = Rot(c, "PT", [128, 512], BF16, 3, st)
    rs = c.sb("rs", [128, 4], F32, st)
    ob = c.sb("ob", [128, 128], BF16, st)
    OT = c.sb("OT", [128, 8, UT], BF16, st)
    q0f = q0s[:].rearrange("p a b -> p (a b)")
    for i in range(NOWN):
        xsrc = xo.t[i * UT:(i + 1) * UT, :].rearrange("(kb p) d -> p kb d", p=128)
        do_norm(c, st, N, xsrc, g1, ident, pT_rot)
        for w in range(2):
            c.dma("sp", lambda e, w=w, i=i: e.dma_start(out=msk[w][:].rearrange("p a b -> p (a b)"), in_=masks.t[i, w]), writes=[msk[w]])
        for kb in range(4):
            blk = i * 4 + kb
            pc = pO[0]
            for k in range(8):
                mm(c, pc, pc[:, 0:256], uT, uT[:, k, kb * 128:(kb + 1) * 128], Wcq, Wcq[:, k, :], k == 0, k == 7)
            act(c, junk, junk[:, 0:256], pc, pc[:, 0:256], AF.Square, extra_w=[sv], accum_out=sv[:, 0:1])
            rstd_from_ss(c, sv, sv[:, 0:1], 256, sv, sv[:, 2:3], sv, sv[:, 3:4])
            tt(c, "dve", cqg, cqg[:], pc, pc[:, 0:256], gq, gq[:], ALU.mult)
            pT = pT_rot.get()
            for kc in range(2):
                tr(c, pT, pT[:, kc, :], cqg, cqg[:, kc * 128:(kc + 1) * 128], ident, ident[:])
            cp(c, "dve", cqT, cqT[:], pT, pT[:, 0:2, :])
            for n in range(3):
                pq = pO[1 + n]
                for kc in range(2):
                    mm(c, pq, pq[:], cqT, cqT[:, kc, :], Wuq, Wuq[:, kc, n * 512:(n + 1) * 512], kc == 0, kc == 1)
                cp(c, "act", q0s, q0f[:, n * 512:(n + 1) * 512], pq, pq[:])
            for h in range(8):
                act(c, junk, junk[:, 0:192], q0s, q0s[:, h, :], AF.Square, extra_w=[SSQ], accum_out=SSQ[:, h:h + 1])
            tt(c, "dve", sv, sv[:, 4:5], sv, sv[:, 3:4], sv, sv[:, 3:4], ALU.mult)
            ts(c, "dve", FQ, FQ[:], SSQ, SSQ[:], sv[:, 4:5], 1.0 / 192, ALU.mult, ALU.mult, extra_r=[sv])
            ts(c, "dve", FQ, FQ[:], FQ, FQ[:], EPS, None, ALU.add)
            act(c, FQ, FQ[:], FQ, FQ[:], AF.Sqrt)
            c.op("dve", lambda e: e.reciprocal(out=FQ[:], in_=FQ[:]), reads=[FQ], writes=[FQ])
            ts(c, "dve", FQ, FQ[:], FQ, FQ[:], sv[:, 3:4], None, ALU.mult, extra_r=[sv])
            tt(c, "dve", q0s, q0s[:], q0s, q0s[:], FQ, FQ[:].unsqueeze(2).to_broadcast([128, 8, 192]), ALU.mult)
            tt(c, "pool", q0s, q0s[:], q0s, q0s[:], gqk, gqk[:].unsqueeze(1).to_broadcast([128, 8, 192]), ALU.mult)
            rope(c, "pool", qr, qr[:], q0s, q0s[:, :, 128:192], cosO, cosO[:, blk:blk + 1, :].to_broadcast([128, 8, 32]),
                 sinO, sinO[:, blk:blk + 1, :].to_broadcast([128, 8, 32]), qtmp, qtmp[:], 8)
            cp(c, "dve", qbn, qbn[:], q0s, q0s[:, :, 0:128])
            cp(c, "dve", qbp, qbp[:], qr, qr[:])
            pT = pT_rot.get()
            for h in range(8):
                tr(c, pT, pT[:, h, :], qbn, qbn[:, h, :], ident, ident[:])
            cp(c, "dve", QT, QT[:, :, kb * 128:(kb + 1) * 128], pT, pT[:])
            pT = pT_rot.get()
            for h in range(8):
                tr(c, pT, pT[0:64, h, :], qbp, qbp[:, h, :], ident, ident[:])
            cp(c, "dve", QP, QP[:, :, kb * 128:(kb + 1) * 128], pT, pT[0:64, :, :])
        E = EXT[i]
        for h in range(8):
            tiles = [(ku, kb) for ku in range(E) for kb in range(4)]
            kv = {}

            def load_kv(ku, h=h):
                KTt = KT_rot.get()
                Vt = V_rot.get()
                c.dma("sp", lambda e, KTt=KTt: e.dma_start(out=KTt[:], in_=KT_d.t[h, :, ku * 512:(ku + 1) * 512]),
                      reads=[KT_d], writes=[KTt])
                c.dma("sp", lambda e, Vt=Vt: e.dma_start(
                    out=Vt[:, :, 0:128], in_=V_d.t[h, ku].rearrange("p (kb d) -> p kb d", d=128)), reads=[V_d], writes=[Vt])
                kv[ku] = (KTt, Vt)

            def qk(t, h=h):
                ku, kb = tiles[t]
                if kb == 0 and ku + 1 < E:
                    load_kv(ku + 1)
                KTt = kv[ku][0]
                kblk = ku * 4 + kb
                pS = pS_rot.get()
                mm(c, pS, pS[:], KTt, KTt[:, kb * 128:(kb + 1) * 128], QT, QT[:, h, :], True, False)
                mm(c, pS, pS[:], KP, KP[:, kblk * 128:(kblk + 1) * 128], QP, QP[:, h, :], False, True)
                return pS

            load_kv(0)
            pSq = [qk(0)]
            if len(tiles) > 1:
                pSq.append(qk(1))
            for t in range(len(tiles)):
                ku, kb = tiles[t]
                kblk = ku * 4 + kb
                pS = pSq.pop(0)
                if t + 2 < len(tiles):
                    pSq.append(qk(t + 2))
                Vt = kv[ku][1]
                PT = PT_rot.get()
                act(c, PT, PT[:], pS, pS[:], AF.Exp, extra_r=[SK], scale=SK[:, kblk, h:h + 1])
                if ku >= E - 2:
                    w = ku - (E - 2)
                    tt(c, "pool", PT, PT[:], PT, PT[:], msk[w], msk[w][:, kb, :], ALU.mult)
                pOT, pRS = pO[2 * (h % 2)], pO[2 * (h % 2) + 1]
                mm(c, pOT, pOT[:], Vt, Vt[:, kb, 0:128], PT, PT[:], t == 0, t == len(tiles) - 1)
                mm(c, pRS, pRS[:], onesb, onesb[:], PT, PT[:], t == 0, t == len(tiles) - 1)
            pOT, pRS = pO[2 * (h % 2)], pO[2 * (h % 2) + 1]
            c.op("dve", lambda e, pRS=pRS: e.reciprocal(out=rinv[:], in_=pRS[:]), reads=[pRS], writes=[rinv])
            tt(c, "dve", OT, OT[:, h, :], pOT, pOT[:], rinv, rinv[:], ALU.mult)
            c.flush()
        c.dma("sp", lambda e, i=i: e.dma_start(out=OT_d.t[i], in_=OT[:].rearrange("p a b -> p (a b)")), reads=[OT], writes=[OT_d])


def phase2c(c, st, G):
    xo, ident, OT_d, sA_d, h_d, u2_d = G["xo"], G["ident"], G["OT_d"], G["sA_d"], G["h_d"], G["u2_d"]
    S1all, S2all, GT = G["S1all"], G["S2all"], G["GT"]
    w_in_v = G["w_in"].t[:, :].rearrange("(k p) n -> p k n", p=128)
    wmo_v = G["w_mla_o"].t[:, :].rearrange("(k p) n -> p k n", p=128)
    wo_v = G["w_out"].t[:, :].rearrange("(k p) n -> p k n", p=128)
    wr_v = G["wr"].t[:, :].rearrange("(k p) n -> p k n", p=128)
    Wgb = load_w(c, st, "Wgb", lambda k: w_in_v[:, k, 3648:4672], 8, 1024)
    Wmo = load_w(c, st, "Wmo", lambda k: wmo_v[:, k, :], 8, 1024)
    Wo = load_w(c, st, "Wo", lambda k: wo_v[:, k, :], 8, 1024)
    Wr = load_w(c, st, "Wr", lambda k: wr_v[:, k, :], 8, 72)
    g1 = small_t(c, st, "g1s", G["g1"], [128, D])
    g2 = small_t(c, st, "g2s", G["g2"], [128, D])
    brt = small_t(c, st, "brt", G["br"], [128, 72])
    N = norm_tiles(c, st)
    uT = N["uT"]
    junk = N["junk"]
    pT_rot = Rot(c, "pT", [128, 8, 128], BF16, 2, st, psum=True)
    pA_rot = Rot(c, "pA", [128, 512], F32, 4, st, psum=True)
    OT = c.sb("OT", [128, 8, UT], BF16, st)
    sAT = c.sb("sAT", [128, 8, UT], BF16, st)
    LC = [dict(sg=c.sb("sg", [128, UT], F32, st), p0=pA_rot.tiles[2 * ln], p1=pA_rot.tiles[2 * ln + 1]) for ln in range(2)]
    mT = c.sb("mT", [128, 8, UT], BF16, st)
    hrow = c.sb("hrow", [128, D], F32, st)
    u2b = c.sb("u2b", [128, D], BF16, st)
    u2T = c.sb("u2T", [128, 8, UT], BF16, st)
    sv = c.sb("sv", [128, 16], F32, st)
    lg = c.sb("lg", [128, 72], F32, st)
    m8 = c.sb("m8", [128, 8], F32, st)
    oh = c.sb("oh", [128, 8], F32, st)
    em = c.sb("em", [128, 8, 8], F32, st)
    s1 = c.sb("s1", [128, 64], F32, st)
    s2 = c.sb("s2", [128, 64], F32, st)
    emf = em[:].rearrange("p a b -> p (a b)")
    for i in range(NOWN):
        xsrc = xo.t[i * UT:(i + 1) * UT, :].rearrange("(kb p) d -> p kb d", p=128)
        xt = do_norm(c, st, N, xsrc, g1, ident, pT_rot)
        c.dma("sp", lambda e, i=i: e.dma_start(out=OT[:].rearrange("p a b -> p (a b)"), in_=OT_d.t[i]), reads=[OT_d], writes=[OT])
        c.dma("sp", lambda e, i=i: e.dma_start(out=sAT[:].rearrange("p a b -> p (a b)"), in_=sA_d.t[i]), reads=[sA_d], writes=[sAT])
        def merge_chunk(co, L):
            sg, pb, pg = L["sg"], L["p0"], L["p1"]
            for k in range(8):
                mm(c, pb, pb[:], Wmo, Wmo[:, k, co * 128:(co + 1) * 128], OT, OT[:, k, :], k == 0, k == 7)
            yield
            for k in range(8):
                mm(c, pg, pg[:], Wgb, Wgb[:, k, co * 128:(co + 1) * 128], uT, uT[:, k, :], k == 0, k == 7)
            yield
            act(c, sg, sg[:], pg, pg[:], AF.Sigmoid)
            yield
            tt(c, "dve", sg, sg[:], pb, pb[:], sg, sg[:], ALU.mult)
            tt(c, "dve", mT, mT[:, co, :], sg, sg[:], sAT, sAT[:, co, :], ALU.add)
            yield

        run_lanes([chain_gens([merge_chunk(co, LC[ln]) for co in range(ln, 8, 2)]) for ln in range(2)])
        for kb in range(4):
            blk = i * 4 + kb
            for n in range(2):
                ph = pA_rot.get()
                for k in range(8):
                    mm(c, ph, ph[:], mT, mT[:, k, kb * 128:(kb + 1) * 128], Wo, Wo[:, k, n * 512:(n + 1) * 512], k == 0, k == 7)
                tt(c, "dve", hrow, hrow[:, n * 512:(n + 1) * 512], ph, ph[:], xt, xt[:, kb, n * 512:(n + 1) * 512], ALU.add)
            c.dma("sp", lambda e, blk=blk: e.dma_start(out=h_d.t[blk * 128:(blk + 1) * 128, :], in_=hrow[:]), reads=[hrow], writes=[h_d])
            act(c, junk, junk[:], hrow, hrow[:], AF.Square, extra_w=[sv], accum_out=sv[:, 0:1])
            rstd_from_ss(c, sv, sv[:, 0:1], D, sv, sv[:, 2:3], sv, sv[:, 3:4])
            stt(c, u2b, u2b[:], hrow, hrow[:], sv[:, 3:4], g2, g2[:], ALU.mult, ALU.mult, extra_r=[sv])
            c.dma("sp", lambda e, blk=blk: e.dma_start(out=u2_d.t[blk * 128:(blk + 1) * 128, :], in_=u2b[:]), reads=[u2b], writes=[u2_d])
            pT = pT_rot.get()
            for k in range(8):
                tr(c, pT, pT[:, k, :], u2b, u2b[:, k * 128:(k + 1) * 128], ident, ident[:])
            cp(c, "dve", u2T, u2T[:, :, kb * 128:(kb + 1) * 128], pT, pT[:])
            pl = pA_rot.get()
            for k in range(8):
                mm(c, pl, pl[:, 0:72], u2T, u2T[:, k, kb * 128:(kb + 1) * 128], Wr, Wr[:, k, :], k == 0, k == 7)
            tt(c, "dve", lg, lg[:], pl, pl[:, 0:72], brt, brt[:], ALU.add)
            c.op("dve", lambda e: e.max(out=m8[:], in_=lg[:, 0:8]), reads=[lg], writes=[m8])
            ts(c, "dve", oh, oh[:], lg, lg[:, 0:8], m8[:, 0:1], None, ALU.is_ge, extra_r=[m8])
            ts(c, "dve", sv, sv[:, 5:6], m8, m8[:, 0:1], -1.0, None, ALU.mult)
            act(c, junk, junk[:, 0:8], lg, lg[:, 0:8], AF.Exp, extra_r=[sv], extra_w=[sv], bias=sv[:, 5:6], accum_out=sv[:, 6:7])
            c.op("dve", lambda e: e.reciprocal(out=sv[:, 7:8], in_=sv[:, 6:7]), reads=[sv], writes=[sv])
            ts(c, "dve", oh, oh[:], oh, oh[:], -1.0, 1e9, ALU.add, ALU.mult)
            tt(c, "dve", em, em[:], lg, lg[:, 8:72].rearrange("p (a b) -> p a b", b=8), oh, oh[:].unsqueeze(2).to_broadcast([128, 8, 8]), ALU.add)
            c.op("dve", lambda e: e.max(out=m8[:], in_=emf), reads=[em], writes=[m8])
            ts(c, "dve", s1, s1[:], em, emf, m8[:, 0:1], None, ALU.is_ge, extra_r=[m8])
            ts(c, "dve", s2, s2[:], em, emf, m8[:, 1:2], None, ALU.is_ge, extra_r=[m8])
            tt(c, "dve", sv, sv[:, 8:9], m8, m8[:, 0:1], m8, m8[:, 1:2], ALU.subtract)
            act(c, sv, sv[:, 9:10], sv, sv[:, 8:9], AF.Sigmoid)
            tt(c, "dve", sv, sv[:, 10:11], sv, sv[:, 9:10], sv, sv[:, 7:8], ALU.mult)
            tt(c, "dve", sv, sv[:, 11:12], sv, sv[:, 7:8], sv, sv[:, 10:11], ALU.subtract)
            tt(c, "dve", sv, sv[:, 12:13], sv, sv[:, 10:11], sv, sv[:, 11:12], ALU.subtract)
            cp(c, "dve", S1all, S1all[:, blk, :], s1, s1[:])
            tt(c, "dve", S2all, S2all[:, blk, :], s2, s2[:], s1, s1[:], ALU.subtract)
            cp(c, "dve", GT, GT[:, blk, 0:2], sv, sv[:, 10:12])
        c.flush()


def phase3(c, st, G):
    ident, h_d, u2_d, out = G["ident"], G["h_d"], G["u2_d"], G["out"]
    S1all, S2all, GT = G["S1all"], G["S2all"], G["GT"]
    xpad_d, ypad_d, w1r, w3r, w2r = G["xpad_d"], G["ypad_d"], G["w1r"], G["w3r"], G["w2r"]
    ustr = c.sb("ustr", [128, 128], BF16, st)
    c.dma("pool", lambda e: e.dma_start(out=ustr[:], in_=G["ustrict"].t[:, :]), writes=[ustr])
    onesb = c.sb("onesb", [128, 128], BF16, st)
    c.op("dve", lambda e: e.memset(onesb[:], 1.0), writes=[onesb])
    thr = small_t(c, st, "thr", G["thr"], [128, 128])
    pidx = small_t(c, st, "pidx", G["pidx"], [128, 1])
    pA_rot = Rot(c, "pA3", [128, 512], F32, 2, st, psum=True)
    A = c.sb("A3", [128, 64], BF16, st)
    Acum = c.sb("Acum", [128, 64], BF16, st)
    c.op("dve", lambda e: e.memset(Acum[:], 0.0), writes=[Acum])
    RK = c.sb("RK", [128, 32, 64], F32, st)
    for b in range(32):
        tt(c, "dve", A, A[:], S1all, S1all[:, b, :], S2all, S2all[:, b, :], ALU.add)
        pr = pA_rot.get()
        mm(c, pr, pr[:, 0:64], ustr, ustr[:], A, A[:], True, False)
        mm(c, pr, pr[:, 0:64], onesb, onesb[:], Acum, Acum[:], False, True)
        cp(c, "act", RK, RK[:, b, :], pr, pr[:, 0:64])
        tt(c, "dve", Acum, Acum[:], Acum, Acum[:], A, A[:], ALU.add)
    pr = pA_rot.get()
    mm(c, pr, pr[:, 0:64], onesb, onesb[:], Acum, Acum[:], True, True)
    tot = c.sb("tot", [128, 64], F32, st)
    f1 = c.sb("f1", [128, 64], F32, st)
    f2 = c.sb("f2", [128, 64], F32, st)
    ki = c.sb("ki3", [128, 64], I32, st)
    cp(c, "act", tot, tot[:], pr, pr[:, 0:64])
    ts(c, "dve", f1, f1[:], tot, tot[:], 127.0, 1.0 / 128, ALU.add, ALU.mult)
    cp(c, "dve", ki, ki[:], f1, f1[:])
    cp(c, "dve", f2, f2[:], ki, ki[:])
    tt(c, "dve", f1, f1[:], f2, f2[:], f1, f1[:], ALU.is_gt)
    tt(c, "dve", f2, f2[:], f2, f2[:], f1, f1[:], ALU.subtract)
    padded = c.sb("padded", [128, 64], F32, st)
    ts(c, "dve", padded, padded[:], f2, f2[:], 128.0, None, ALU.mult)
    pend = c.sb("pend", [128, 64], F32, st)
    pstart = c.sb("pstart", [128, 64], F32, st)
    ones64 = c.sb("ones64", [128, 64], F32, st)
    c.op("dve", lambda e: e.memset(ones64[:], 1.0), writes=[ones64])
    c.op("dve", lambda e: e.tensor_tensor_scan(out=pend[:], data0=ones64[:], data1=padded[:], initial=0.0, op0=ALU.mult, op1=ALU.add),
         reads=[ones64, padded], writes=[pend])
    tt(c, "dve", pstart, pstart[:], pend, pend[:], padded, padded[:], ALU.subtract)
    DST = c.sb("DST", [128, 64], F32, st)
    DSTi = c.sb("DSTi", [128, 64], I32, st)
    junk = c.sb("junk3", [128, 64], F32, st)
    for b in range(32):
        tt(c, "dve", f1, f1[:], RK, RK[:, b, :], pstart, pstart[:], ALU.add)
        stt(c, junk, junk[:], f1, f1[:], 1.0, S1all, S1all[:, b, :], ALU.mult, ALU.mult, extra_w=[DST], accum_out=DST[:, 2 * b:2 * b + 1])
        stt(c, junk, junk[:], f1, f1[:], 1.0, S2all, S2all[:, b, :], ALU.mult, ALU.mult, extra_w=[DST], accum_out=DST[:, 2 * b + 1:2 * b + 2])
    cp(c, "dve", DSTi, DSTi[:], DST, DST[:])
    u2r_rot = Rot(c, "u2r", [128, D], BF16, 2, st)
    for b in range(32):
        u2r = u2r_rot.get()
        c.dma("sp", lambda e, b=b, u2r=u2r: e.dma_start(out=u2r[:], in_=u2_d.t[b * 128:(b + 1) * 128, :]), reads=[u2_d], writes=[u2r])
        for sl in range(2):
            c.dma("pool", lambda e, b=b, sl=sl, u2r=u2r: e.indirect_dma_start(
                out=xpad_d.t[:, :], out_offset=bass.IndirectOffsetOnAxis(ap=DSTi[:, 2 * b + sl:2 * b + sl + 1], axis=0),
                in_=u2r[:], in_offset=None), reads=[u2r, DSTi], writes=[xpad_d])
    cmp = c.sb("cmp3", [128, 128, 64], F32, st)
    tt(c, "dve", cmp, cmp[:], pend, pend[:].unsqueeze(1).to_broadcast([128, 128, 64]),
       thr, thr[:].unsqueeze(2).to_broadcast([128, 128, 64]), ALU.is_le)
    BE = c.sb("BE", [128, 128], F32, st)
    c.op("dve", lambda e: e.tensor_reduce(out=BE[:], in_=cmp[:], axis=mybir.AxisListType.X, op=ALU.add), reads=[cmp], writes=[BE])
    ts(c, "dve", BE, BE[:], BE, BE[:], 63.0, 128.0, ALU.min, ALU.mult)
    ts(c, "dve", BE, BE[:], BE, BE[:], pidx[:, 0:1], None, ALU.add, extra_r=[pidx])
    WIDX = c.sb("WIDX", [128, 128], I32, st)
    cp(c, "dve", WIDX, WIDX[:], BE, BE[:])
    c.flush()
    Wc_rot = Rot(c, "Wcat", [128, 6144], BF16, 3, st)
    wcat_d = G["wcat_d"]
    xr_rot = Rot(c, "xrow", [128, D], BF16, 3, st)
    xT = c.sb("xT3", [128, 8, 128], BF16, st)
    pH1 = Rot(c, "pH1", [128, 512], F32, 1, st, psum=True)
    pH3 = Rot(c, "pH3", [128, 512], F32, 1, st, psum=True)
    pY_rot = Rot(c, "pY", [128, 512], F32, 2, st, psum=True)
    pT_rot = Rot(c, "pT3", [128, 8, 128], BF16, 2, st, psum=True)
    sg = c.sb("sg3", [128, 256], F32, st)
    hg = c.sb("hg3", [128, 256], BF16, st)
    hgT = c.sb("hgT3", [128, 2, 128], BF16, st)
    yrow_rot = Rot(c, "yrow", [128, D], F32, 2, st)
    stg = {}

    def fetch(i):
        Wc, xr = Wc_rot.get(), xr_rot.get()
        c.dma("pool", lambda e, Wc=Wc, i=i: e.indirect_dma_start(
            out=Wc[:], out_offset=None, in_=wcat_d.t[:, :],
            in_offset=bass.IndirectOffsetOnAxis(ap=WIDX[:, i:i + 1], axis=0)), reads=[WIDX, wcat_d], writes=[Wc])
        c.dma("sp", lambda e, xr=xr, i=i: e.dma_start(out=xr[:], in_=xpad_d.t[i * 128:(i + 1) * 128, :]), reads=[xpad_d], writes=[xr])
        stg[i] = (Wc, xr)

    fetch(0)
    fetch(1)
    for i in range(NBLK):
        if i + 2 < NBLK:
            fetch(i + 2)
        Wc, xr = stg.pop(i)
        pT = pT_rot.get()
        for k in range(8):
            tr(c, pT, pT[:, k, :], xr, xr[:, k * 128:(k + 1) * 128], ident, ident[:])
        cp(c, "dve", xT, xT[:], pT, pT[:])
        p1 = pH1.get()
        p3 = pH3.get()
        for k in range(8):
            mm(c, p1, p1[:, 0:256], xT, xT[:, k, :], Wc, Wc[:, k * 256:(k + 1) * 256], k == 0, k == 7)
        for k in range(8):
            mm(c, p3, p3[:, 0:256], xT, xT[:, k, :], Wc, Wc[:, 2048 + k * 256:2048 + (k + 1) * 256], k == 0, k == 7)
        act(c, sg, sg[:], p1, p1[:, 0:256], AF.Sigmoid)
        tt(c, "dve", sg, sg[:], p1, p1[:, 0:256], sg, sg[:], ALU.mult)
        tt(c, "dve", hg, hg[:], p3, p3[:, 0:256], sg, sg[:], ALU.mult)
        pT = pT_rot.get()
        for kc in range(2):
            tr(c, pT, pT[:, kc, :], hg, hg[:, kc * 128:(kc + 1) * 128], ident, ident[:])
        cp(c, "dve", hgT, hgT[:], pT, pT[:, 0:2, :])
        yrow = yrow_rot.get()
        for n in range(2):
            py = pY_rot.get()
            for kc in range(2):
                mm(c, py, py[:], hgT, hgT[:, kc, :], Wc, Wc[:, 4096 + kc * 1024 + n * 512:4096 + kc * 1024 + (n + 1) * 512], kc == 0, kc == 1)
            cp(c, "act" if n == 0 else "dve", yrow, yrow[:, n * 512:(n + 1) * 512], py, py[:])
        c.dma("sp", lambda e, i=i, yrow=yrow: e.dma_start(out=ypad_d.t[i * 128:(i + 1) * 128, :], in_=yrow[:]), reads=[yrow], writes=[ypad_d])
        if i % 16 == 15:
            c.flush()
    y1_rot = Rot(c, "y1", [128, D], F32, 2, st)
    y2_rot = Rot(c, "y2", [128, D], F32, 2, st)
    hr_rot = Rot(c, "hr3", [128, D], F32, 2, st)
    for b in range(32):
        y1, y2, hr = y1_rot.get(), y2_rot.get(), hr_rot.get()
        for (dst, sl) in ((y1, 0), (y2, 1)):
            c.dma("pool", lambda e, dst=dst, sl=sl, b=b: e.indirect_dma_start(
                out=dst[:], out_offset=None, in_=ypad_d.t[:, :],
                in_offset=bass.IndirectOffsetOnAxis(ap=DSTi[:, 2 * b + sl:2 * b + sl + 1], axis=0)), reads=[DSTi, ypad_d], writes=[dst])
        c.dma("sp", lambda e, b=b, hr=hr: e.dma_start(out=hr[:], in_=h_d.t[b * 128:(b + 1) * 128, :]), reads=[h_d], writes=[hr])
        stt(c, hr, hr[:], y1, y1[:], GT[:, b, 0:1], hr, hr[:], ALU.mult, ALU.add, extra_r=[GT])
        stt(c, hr, hr[:], y2, y2[:], GT[:, b, 1:2], hr, hr[:], ALU.mult, ALU.add, extra_r=[GT])
        c.dma("sp", lambda e, b=b, hr=hr: e.dma_start(out=out.t[b * 128:(b + 1) * 128, :], in_=hr[:]), reads=[hr], writes=[out])
    c.flush()


def phase3_dense(c, st, G):
    ident, h_d, u2T_d, Gall, out = G["ident"], G["h_d"], G["u2T_d"], G["Gall"], G["out"]
    w1r, w3r, w2r = G["w1r"], G["w3r"], G["w2r"]
    NB = 16
    yacc = c.sb("yacc", [128, NB, D], F32, st)
    u2T = c.sb("u2Ta", [128, 8, NB * 128], BF16, st)
    W13_rot = Rot(c, "W13", [128, 8, 512], BF16, 2, st)
    W2_rot = Rot(c, "W2", [128, 2, 1024], BF16, 2, st)
    pH_rot = Rot(c, "pH", [128, 512], F32, 2, st, psum=True)
    pY_rot = Rot(c, "pY", [128, 512], F32, 4, st, psum=True)
    pT_rot = Rot(c, "pT", [128, 8, 128], BF16, 2, st, psum=True)
    sg = c.sb("sg3", [128, 256], F32, st)
    hg = c.sb("hg", [128, 256], BF16, st)
    hgT = c.sb("hgT", [128, 2, 128], BF16, st)
    hrow = c.sb("hrow3", [128, D], F32, st)
    for half in range(2):
        for j in range(4):
            c.dma("sp", lambda e, j=j, half=half: e.dma_start(
                out=u2T[:, :, j * 512:(j + 1) * 512], in_=u2T_d.t[half * 4 + j].rearrange("p (a b) -> p a b", b=512)),
                reads=[u2T_d], writes=[u2T])
        for ex in range(64):
            W13 = W13_rot.get()
            W2 = W2_rot.get()
            c.dma("pool", lambda e, ex=ex, W13=W13: e.dma_start(
                out=W13[:, :, 0:256], in_=w1r.t[ex * 128:(ex + 1) * 128, :].rearrange("p (a b) -> p a b", b=256)), writes=[W13])
            c.dma("pool", lambda e, ex=ex, W13=W13: e.dma_start(
                out=W13[:, :, 256:512], in_=w3r.t[ex * 128:(ex + 1) * 128, :].rearrange("p (a b) -> p a b", b=256)), writes=[W13])
            c.dma("pool", lambda e, ex=ex, W2=W2: e.dma_start(
                out=W2[:].rearrange("p a b -> p (a b)"), in_=w2r.t[ex * 128:(ex + 1) * 128, :]), writes=[W2])
            for b in range(NB):
                blk = half * NB + b
                ph = pH_rot.get()
                for k in range(8):
                    mm(c, ph, ph[:], u2T, u2T[:, k, b * 128:(b + 1) * 128], W13, W13[:, k, :], k == 0, k == 7)
                act(c, sg, sg[:], ph, ph[:, 0:256], AF.Sigmoid)
                tt(c, "dve", sg, sg[:], ph, ph[:, 0:256], sg, sg[:], ALU.mult)
                tt(c, "dve", sg, sg[:], ph, ph[:, 256:512], sg, sg[:], ALU.mult)
                ts(c, "dve", hg, hg[:], sg, sg[:], Gall[:, blk, ex:ex + 1], None, ALU.mult, extra_r=[Gall])
                pT = pT_rot.get()
                for kc in range(2):
                    tr(c, pT, pT[:, kc, :], hg, hg[:, kc * 128:(kc + 1) * 128], ident, ident[:])
                cp(c, "dve", hgT, hgT[:], pT, pT[:, 0:2, :])
                for n in range(2):
                    py = pY_rot.get()
                    for kc in range(2):
                        mm(c, py, py[:], hgT, hgT[:, kc, :], W2, W2[:, kc, n * 512:(n + 1) * 512], kc == 0, kc == 1)
                    if ex == 0:
                        cp(c, "act", yacc, yacc[:, b, n * 512:(n + 1) * 512], py, py[:])
                    else:
                        tt(c, "dve", yacc, yacc[:, b, n * 512:(n + 1) * 512], py, py[:], yacc, yacc[:, b, n * 512:(n + 1) * 512], ALU.add)
            if ex % 4 == 3:
                c.flush()
        for b in range(NB):
            blk = half * NB + b
            c.dma("sp", lambda e, blk=blk: e.dma_start(out=hrow[:], in_=h_d.t[blk * 128:(blk + 1) * 128, :]), reads=[h_d], writes=[hrow])
            tt(c, "dve", yacc, yacc[:, b, :], yacc, yacc[:, b, :], hrow, hrow[:], ALU.add)
            c.dma("sp", lambda e, blk=blk, b=b: e.dma_start(out=out.t[blk * 128:(blk + 1) * 128, :], in_=yacc[:, b, :]), reads=[yacc], writes=[out])
        c.flush()


def _prep_core(inp, cidx):
    b, j = cidx // 2, cidx % 2
    f = np.float32
    x = inp["x"]
    own = OWN[j]
    rep = lambda v, n=128: np.ascontiguousarray(np.broadcast_to(np.asarray(v, f).reshape(1, -1), (n, np.asarray(v).size)))
    colmaj = lambda v: np.ascontiguousarray(np.asarray(v, f).reshape(8, 128).T)
    m = {}
    m["xs"] = np.ascontiguousarray(x[b])
    m["xo"] = np.ascontiguousarray(np.concatenate([x[b, u * UT:(u + 1) * UT] for u in own], 0))
    pos = np.asarray(inp["positions"][b], np.int32)
    m["pos_s"] = np.ascontiguousarray(pos.reshape(64, 128).T)
    pown = np.concatenate([pos[u * UT:(u + 1) * UT] for u in own])
    m["pos_o"] = np.ascontiguousarray(pown.reshape(32, 128).T)
    invf = (10000.0 ** (-np.arange(0, 64, 2, dtype=np.float32) / 64)).astype(f)
    m["invf"] = rep(invf)
    m["ident"] = np.eye(128, dtype=f)
    m["g1"] = rep(inp["norm1_g"][0])
    m["w_in"] = np.ascontiguousarray(inp["w_in"][0])
    m["convw"] = np.ascontiguousarray(np.asarray(inp["conv_w"][0], f).reshape(4, 8, 128).transpose(2, 1, 0))
    m["convb"] = colmaj(inp["conv_b"][0])
    m["lru_wa"] = np.ascontiguousarray(inp["lru_wa"][0])
    m["lru_wx"] = np.ascontiguousarray(inp["lru_wx"][0])
    m["lru_ba"] = colmaj(inp["lru_ba"][0])
    m["lru_bx"] = colmaj(inp["lru_bx"][0])
    m["lru_lam"] = colmaj(inp["lru_lambda"][0])
    m["w_rnn_o"] = np.ascontiguousarray(inp["w_rnn_o"][0])
    m["gq"] = rep(inp["q_norm_g"][0])
    m["w_uq"] = np.ascontiguousarray(inp["w_uq"][0])
    m["gkv"] = rep(inp["kv_norm_g"][0])
    m["w_ukv"] = np.ascontiguousarray(inp["w_ukv"][0])
    m["gqk_q"] = rep(inp["qk_norm_q_g"][0])
    gk = np.asarray(inp["qk_norm_k_g"][0], f)
    m["gqk_k_pe"] = rep(gk[128:192])
    m["gqk_k_col"] = np.ascontiguousarray(gk[0:128].reshape(128, 1))
    m["w_mla_o"] = np.ascontiguousarray(inp["w_mla_o"][0])
    m["w_out"] = np.ascontiguousarray(inp["w_out"][0])
    m["g2"] = rep(inp["norm2_g"][0])
    m["wr"] = np.ascontiguousarray(np.concatenate([inp["router_wg"][0], inp["router_we"][0]], 1))
    m["br"] = rep(np.concatenate([inp["router_bg"][0], inp["router_be"][0]]))
    m["w1r"] = np.ascontiguousarray(np.asarray(inp["exp_w1"][0], f).reshape(64, 8, 128, 256).transpose(0, 2, 1, 3).reshape(64 * 128, 2048))
    m["w3r"] = np.ascontiguousarray(np.asarray(inp["exp_w3"][0], f).reshape(64, 8, 128, 256).transpose(0, 2, 1, 3).reshape(64 * 128, 2048))
    m["w2r"] = np.ascontiguousarray(np.asarray(inp["exp_w2"][0], f).reshape(64, 2, 128, 1024).transpose(0, 2, 1, 3).reshape(64 * 128, 2048))
    kk = (np.arange(4)[None, :, None] * 128 + np.arange(128)[:, None, None])
    qq = np.arange(512)[None, None, :]
    diag = (kk <= qq).astype(f)
    full = np.ones_like(diag)
    zero = np.zeros_like(diag)
    mk = np.zeros((NOWN, 2, 128, 4, 512), f)
    for i in range(NOWN):
        for w in range(2):
            ku = EXT[i] - 2 + w
            mk[i, w] = full if ku < own[i] else (diag if ku == own[i] else zero)
    m["masks"] = mk.reshape(NOWN, 2, 128, 2048).astype(ml_dtypes.bfloat16)
    hid = np.zeros((128, 64), np.int32)
    for i in range(NOWN):
        for ch in range(8):
            hid[:, i * 8 + ch] = own[i] * 1024 + ch * 128 + np.arange(128)
    m["hidx"] = hid
    m["pidx"] = np.arange(128, dtype=f).reshape(128, 1)
    m["ustrict"] = np.triu(np.ones((128, 128), f), 1)
    m["thr"] = np.ascontiguousarray(np.broadcast_to((np.arange(128, dtype=f) * 128).reshape(1, 128), (128, 128)))
    return m


_NC_CACHE = {}


def kernel(**inputs):
    inp = {k: np.asarray(v) for k, v in inputs.items()}
    if "nc" not in _NC_CACHE:
        _NC_CACHE["nc"] = build()
    nc = _NC_CACHE["nc"]
    in_maps = [_prep_core(inp, cidx) for cidx in range(8)]
    res = run_bass_kernel_spmd(nc, in_maps, core_ids=list(range(8)))
    outp = np.zeros((4, S, D), np.float32)
    for cidx in range(8):
        b, j = cidx // 2, cidx % 2
        o = res.results[cidx]["out"]
        for i, u in enumerate(OWN[j]):
            outp[b, u * UT:(u + 1) * UT] = o[i * UT:(i + 1) * UT]
    return outp
```

```python
import math
import numpy as np
from contextlib import ExitStack
import ml_dtypes
import concourse.bass as bass
import concourse.mybir as mybir
from concourse.bass_utils import run_bass_kernel_spmd

F32 = mybir.dt.float32
BF16 = mybir.dt.bfloat16
I32 = mybir.dt.int32
AF = mybir.ActivationFunctionType
ALU = mybir.AluOpType

D = 1024
S = 8192
NU = 16
UT = 512
NOWN = 8
H = 8
EPS = 1e-6
OWN = {0: [0, 3, 4, 7, 8, 11, 12, 15], 1: [1, 2, 5, 6, 9, 10, 13, 14]}
EXT = [4 * (i // 2) + (2 if i % 2 == 0 else 4) for i in range(NOWN)]
MPAD = 16384
NBLK = MPAD // 128
TWO_PI = 6.283185
QSCALE = 192.0 ** -0.5


class T:
    __slots__ = ("t", "name", "writes", "reads")

    def __init__(self, t, name=""):
        self.t = t
        self.name = name
        self.writes = {}
        self.reads = {}

    def __getitem__(self, idx):
        return self.t[idx]


class Ctx:
    ENG = ("pe", "act", "dve", "pool", "sp")

    def __init__(self, nc, stack, block, n_dma_sems=10):
        self.nc = nc
        self.stack = stack
        self.block = block
        self.cnt = {}
        self.semobj = {}
        for n in self.ENG:
            self.semobj["e_" + n] = stack.enter_context(nc.semaphore("s_" + n))
            self.cnt["e_" + n] = 0
        self.dq = {}
        for q in ("sp", "act", "pool"):
            lst = []
            for i in range(n_dma_sems):
                key = "d_%s_%d" % (q, i)
                self.semobj[key] = stack.enter_context(nc.semaphore(key))
                self.cnt[key] = 0
                lst.append(key)
            self.dq[q] = [lst, 0]
        self.seen = {n: {} for n in self.ENG}
        self.prog = {n: [] for n in self.ENG}
        self.ninst = 0

    def flush(self):
        b = self.block
        starters = {"pe": b.tensor, "act": b.scalar, "dve": b.vector, "pool": b.gpsimd, "sp": b.sync}
        for n in self.ENG:
            lst = self.prog[n]
            if not lst:
                continue

            def body(eng, lst=lst):
                for f in lst:
                    f(eng)
            starters[n](body)
            self.prog[n] = []

    def barrier(self):
        toks = dict(self.cnt)
        for n in self.ENG:
            self._need(n, {k: v for k, v in toks.items() if v > 0}, force_own=False)

    def sb(self, name, shape, dt, stack=None):
        self.uid = getattr(self, "uid", 0) + 1
        name = "%s_%d" % (name, self.uid)
        return T((stack or self.stack).enter_context(self.nc.sbuf_tensor(name, list(shape), dt)), name)

    def ps(self, name, shape, dt=F32, stack=None):
        self.uid = getattr(self, "uid", 0) + 1
        name = "%s_%d" % (name, self.uid)
        return T((stack or self.stack).enter_context(self.nc.psum_tensor(name, list(shape), dt)), name)

    def _need(self, eng, toks, force_own=True):
        seen = self.seen[eng]
        own = "e_" + eng
        for k, v in toks.items():
            if k == own and (eng == "pe" or not force_own):
                continue
            if seen.get(k, 0) >= v:
                continue
            self.prog[eng].append(lambda e, s=self.semobj[k], v=v: e.wait_ge(s, v))
            seen[k] = v
            self.ninst += 1

    def _deps(self, eng, reads, writes):
        toks = {}
        for t in reads:
            for k, v in t.writes.items():
                if toks.get(k, 0) < v:
                    toks[k] = v
        for t in writes:
            for k, v in t.writes.items():
                if toks.get(k, 0) < v:
                    toks[k] = v
            for k, v in t.reads.items():
                if toks.get(k, 0) < v:
                    toks[k] = v
        self._need(eng, toks)

    def op(self, eng, fn, reads=(), writes=()):
        self._deps(eng, reads, writes)
        key = "e_" + eng
        self.cnt[key] += 1
        v = self.cnt[key]
        self.prog[eng].append(lambda e, fn=fn, s=self.semobj[key]: fn(e).then_inc(s, 1))
        for t in reads:
            t.reads[key] = v
        for t in writes:
            t.writes[key] = v
        self.ninst += 1

    def dma(self, q, fn, reads=(), writes=()):
        self._deps(q, reads, writes)
        lst, i = self.dq[q]
        key = lst[i % len(lst)]
        self.dq[q][1] = i + 1
        if self.cnt[key] > 0 and self.seen[q].get(key, 0) < self.cnt[key]:
            self.prog[q].append(lambda e, s=self.semobj[key], v=self.cnt[key]: e.wait_ge(s, v))
            self.seen[q][key] = self.cnt[key]
        self.cnt[key] += 16
        v = self.cnt[key]
        self.prog[q].append(lambda e, fn=fn, s=self.semobj[key]: fn(e).then_inc(s, 16))
        for t in reads:
            t.reads[key] = v
        for t in writes:
            t.writes[key] = v
        self.ninst += 1

    def wait_all(self, eng, tiles):
        toks = {}
        for t in tiles:
            for d in (t.writes, t.reads):
                for k, v in d.items():
                    if toks.get(k, 0) < v:
                        toks[k] = v
        self._need(eng, toks)


class Rot:
    def __init__(self, c, name, shape, dt, n, st, psum=False):
        self.tiles = [(c.ps if psum else c.sb)("%s%d" % (name, i), shape, dt, st) for i in range(n)]
        self.i = 0

    def get(self):
        t = self.tiles[self.i % len(self.tiles)]
        self.i += 1
        return t


def act(c, out_t, out_ap, in_t, in_ap, func, extra_r=(), extra_w=(), **kw):
    c.op("act", lambda e: e.activation(out=out_ap, in_=in_ap, func=func, **kw),
         reads=[in_t] + list(extra_r), writes=[out_t] + list(extra_w))


def ts(c, eng, out_t, out_ap, in_t, in_ap, s1, s2, op0, op1=None, extra_r=()):
    if op1 is None:
        c.op(eng, lambda e: e.tensor_scalar(out=out_ap, in0=in_ap, scalar1=s1, scalar2=None, op0=op0),
             reads=[in_t] + list(extra_r), writes=[out_t])
    else:
        c.op(eng, lambda e: e.tensor_scalar(out=out_ap, in0=in_ap, scalar1=s1, scalar2=s2, op0=op0, op1=op1),
             reads=[in_t] + list(extra_r), writes=[out_t])


def tt(c, eng, out_t, out_ap, a_t, a_ap, b_t, b_ap, op):
    c.op(eng, lambda e: e.tensor_tensor(out=out_ap, in0=a_ap, in1=b_ap, op=op), reads=[a_t, b_t], writes=[out_t])


def stt(c, out_t, out_ap, a_t, a_ap, scalar, b_t, b_ap, op0, op1, extra_r=(), extra_w=(), **kw):
    c.op("dve", lambda e: e.scalar_tensor_tensor(out=out_ap, in0=a_ap, scalar=scalar, in1=b_ap, op0=op0, op1=op1, **kw),
         reads=[a_t, b_t] + list(extra_r), writes=[out_t] + list(extra_w))


def cp(c, eng, out_t, out_ap, in_t, in_ap):
    if eng == "act":
        c.op("act", lambda e: e.copy(out=out_ap, in_=in_ap), reads=[in_t], writes=[out_t])
    else:
        c.op(eng, lambda e: e.tensor_copy(out=out_ap, in_=in_ap), reads=[in_t], writes=[out_t])


def mm(c, out_t, out_ap, l_t, l_ap, r_t, r_ap, start, stop):
    c.op("pe", lambda e: e.matmul(out_ap, lhsT=l_ap, rhs=r_ap, start=start, stop=stop, skip_group_check=True),
         reads=[l_t, r_t], writes=[out_t])


def tr(c, out_t, out_ap, in_t, in_ap, id_t, id_ap):
    c.op("pe", lambda e: e.transpose(out=out_ap, in_=in_ap, identity=id_ap), reads=[in_t, id_t], writes=[out_t])


def rstd_from_ss(c, ss_t, ss_ap, n, rms_t, rms_ap, rstd_t, rstd_ap):
    ts(c, "dve", rms_t, rms_ap, ss_t, ss_ap, 1.0 / n, EPS, ALU.mult, ALU.add)
    act(c, rms_t, rms_ap, rms_t, rms_ap, AF.Sqrt)
    c.op("dve", lambda e: e.reciprocal(out=rstd_ap, in_=rms_ap), reads=[rms_t], writes=[rstd_t])


def trig_tables(c, st, posf_t, nblk, invf_t, cos_t, sin_t):
    ang = c.sb("tg_ang", [128, nblk, 32], F32, st)
    ki = c.sb("tg_ki", [128, nblk, 32], I32, st)
    kf = c.sb("tg_kf", [128, nblk, 32], F32, st)
    fl = c.sb("tg_fl", [128, nblk, 32], F32, st)
    tt(c, "dve", ang, ang[:], posf_t, posf_t[:, 0:nblk].unsqueeze(2).to_broadcast([128, nblk, 32]),
       invf_t, invf_t[:, :].unsqueeze(1).to_broadcast([128, nblk, 32]), ALU.mult)
    for (dst, shift) in ((sin_t, 0.0), (cos_t, 0.25)):
        ts(c, "dve", kf, kf[:], ang, ang[:], 1.0 / (2 * math.pi), shift, ALU.mult, ALU.add)
        cp(c, "dve", ki, ki[:], kf, kf[:])
        cp(c, "dve", fl, fl[:], ki, ki[:])
        tt(c, "dve", kf, kf[:], kf, kf[:], fl, fl[:], ALU.subtract)
        ts(c, "dve", fl, fl[:], kf, kf[:], 0.5, None, ALU.is_gt)
        tt(c, "dve", kf, kf[:], kf, kf[:], fl, fl[:], ALU.subtract)
        ts(c, "dve", fl, fl[:], kf, kf[:], -0.5, None, ALU.is_lt)
        tt(c, "dve", kf, kf[:], kf, kf[:], fl, fl[:], ALU.add)
        act(c, dst, dst[:], kf, kf[:], AF.Sin, scale=TWO_PI)


def rope(c, eng, out_t, out_ap3, x_t, x_ap3, cos_t, cos_ap3, sin_t, sin_ap3, tmp_t, tmp_ap3, nh):
    x1, x2 = x_ap3[:, :, 0:32], x_ap3[:, :, 32:64]
    o1, o2 = out_ap3[:, :, 0:32], out_ap3[:, :, 32:64]
    t1 = tmp_ap3[:, :, 0:32]
    t2 = tmp_ap3[:, :, 32:64]
    tt(c, eng, tmp_t, t1, x_t, x2, sin_t, sin_ap3, ALU.mult)
    tt(c, eng, tmp_t, t2, x_t, x1, sin_t, sin_ap3, ALU.mult)
    tt(c, eng, out_t, o1, x_t, x1, cos_t, cos_ap3, ALU.mult)
    tt(c, eng, out_t, o2, x_t, x2, cos_t, cos_ap3, ALU.mult)
    tt(c, eng, out_t, o1, out_t, o1, tmp_t, t1, ALU.subtract)
    tt(c, eng, out_t, o2, out_t, o2, tmp_t, t2, ALU.add)


def norm_transpose(c, st, x_dram_ap, g1_t, ident_t, xt_rot, ub, uT, pT_rot, ssq, rms, rstd, junk, xt=None):
    if xt is None:
        xt = xt_rot.get()
        c.dma("sp", lambda e: e.dma_start(out=xt[:], in_=x_dram_ap), writes=[xt])
    for kb in range(4):
        act(c, junk, junk[:], xt, xt[:, kb, :], AF.Square, extra_w=[ssq], accum_out=ssq[:, kb:kb + 1])
    import os
    NT = int(os.environ.get("NT", "9"))
    rstd_from_ss(c, ssq, ssq[:, 0:4], D, rms, rms[:, 0:4], rstd, rstd[:, 0:4])
    for kb in range(4):
        if NT >= 1:
            stt(c, ub, ub[:, kb, :], xt, xt[:, kb, :], rstd[:, kb:kb + 1], g1_t, g1_t[:], ALU.mult, ALU.mult, extra_r=[rstd])
        pT = pT_rot.get()
        if NT >= 2:
            for k in range(8):
                tr(c, pT, pT[:, k, :], ub, ub[:, kb, k * 128:(k + 1) * 128], ident_t, ident_t[:])
        if NT >= 3:
            cp(c, "dve", uT, uT[:, :, kb * 128:(kb + 1) * 128], pT, pT[:])
    return xt


def build(dbg=None):
    nc = bass.Bass("TRN2", target_bir_lowering=False)
    dbg = dbg or {}
    last_phase = dbg.get("last_phase", 9)

    def din(name, shape, dt=F32):
        return T(nc.dram_tensor(name, list(shape), dt, kind="ExternalInput"), name)

    def dscr(name, shape, dt):
        kind = "ExternalOutput" if name in dbg.get("dump", ()) else "Internal"
        return T(nc.dram_tensor(name, list(shape), dt, kind=kind), name)

    xs = din("xs", [S, D])
    xo = din("xo", [NOWN * UT, D])
    pos_s = din("pos_s", [128, 64], I32)
    pos_o = din("pos_o", [128, 32], I32)
    invf = din("invf", [128, 32])
    ident_d = din("ident", [128, 128])
    g1 = din("g1", [128, D])
    w_in = din("w_in", [D, 4672])
    convw = din("convw", [128, 8, 4])
    convb = din("convb", [128, 8])
    lru_wa = din("lru_wa", [8, 128, 128])
    lru_wx = din("lru_wx", [8, 128, 128])
    lru_ba = din("lru_ba", [128, 8])
    lru_bx = din("lru_bx", [128, 8])
    lru_lam = din("lru_lam", [128, 8])
    w_rnn_o = din("w_rnn_o", [D, D])
    gq = din("gq", [128, 256])
    w_uq = din("w_uq", [256, 1536])
    gkv = din("gkv", [128, 256])
    w_ukv = din("w_ukv", [256, 2048])
    gqk_q = din("gqk_q", [128, 192])
    gqk_k_pe = din("gqk_k_pe", [128, 64])
    gqk_k_col = din("gqk_k_col", [128, 1])
    w_mla_o = din("w_mla_o", [D, D])
    w_out = din("w_out", [D, D])
    g2 = din("g2", [128, D])
    wr = din("wr", [D, 72])
    br = din("br", [128, 72])
    w1r = din("w1r", [64 * 128, 2048])
    w3r = din("w3r", [64 * 128, 2048])
    w2r = din("w2r", [64 * 128, 2048])
    masks = din("masks", [NOWN, 2, 128, 4 * 512], BF16)
    hidx = din("hidx", [128, 64], I32)
    pidx = din("pidx", [128, 1])
    ustrict = din("ustrict", [128, 128])
    thr = din("thr", [128, 128])
    out = T(nc.dram_tensor("out", [NOWN * UT, D], F32, kind="ExternalOutput"), "out")

    KT_d = dscr("KT_d", [H, 128, S], BF16)
    V_d = dscr("V_d", [H, NU, 128, 512], BF16)
    hT_d = dscr("hT_d", [NU * 1024, 512], BF16)
    sA_d = dscr("sA_d", [NOWN, 128, 8 * 512], BF16)
    OT_d = dscr("OT_d", [NOWN, 128, 8 * 512], BF16)
    h_d = dscr("h_d", [NOWN * UT, D], F32)
    u2_d = dscr("u2_d", [NOWN * UT, D], BF16)
    xpad_d = dscr("xpad_d", [MPAD, D], BF16)
    ypad_d = dscr("ypad_d", [MPAD, D], F32)
    wcat_d = dscr("wcat_d", [64 * 128, 6144], BF16)
    dbg_d = dscr("dbg_d", [128, 8192], F32)

    sk_dump = dscr("sk_dump", [128, 512], F32)
    with ExitStack() as gst:
        c = Ctx(nc, gst, None)
        ident = c.sb("identb", [128, 128], BF16)
        identf = c.sb("identf", [128, 128], F32)
        KP = c.sb("KP", [64, S], BF16)
        SK = c.sb("SK", [128, 64, 8], F32)
        c.dma("pool", lambda e: e.dma_start(out=ident[:], in_=ident_d[:, :]), writes=[ident])
        c.dma("sp", lambda e: e.dma_start(out=identf[:], in_=ident_d[:, :]), writes=[identf])

        block = gst.enter_context(nc.Block())
        c.block = block
        if False:
            for (src, dst) in ((w1r, w1b_d), (w3r, w3b_d), (w2r, w2b_d)):
                for i in range(16):
                    c.dma("pool", lambda e, src=src, dst=dst, i=i: e.dma_start(
                        out=dst[i * 512:(i + 1) * 512, :], in_=src[i * 512:(i + 1) * 512, :]),
                        reads=[src], writes=[dst])

        if last_phase >= 1:
            with ExitStack() as st:
                phase1(c, st, locals())
                c.barrier()
                c.flush()
        S1all = c.sb("S1all", [128, 32, 64], F32)
        S2all = c.sb("S2all", [128, 32, 64], F32)
        GT = c.sb("GT", [128, 32, 2], F32)
        G_ = dict(locals())
        for (pn, fn) in ((2, phase2a), (3, phase2b), (4, phase2c), (5, phase3)):
            if last_phase >= pn:
                with ExitStack() as st:
                    fn(c, st, G_)
                    c.barrier()
                    c.flush()
        c.wait_all("sp", [out, KT_d, V_d, hT_d, dbg_d, sk_dump])
        c.flush()
    return nc


def phase1(c, st, G):
    xs, w_in, ident, KP, SK = G["xs"], G["w_in"], G["ident"], G["KP"], G["SK"]
    KT_d, V_d, hT_d = G["KT_d"], G["V_d"], G["hT_d"]
    Wxr = c.sb("Wxr", [128, 8, 1024], BF16, st)
    Wkv = c.sb("Wkv", [128, 8, 320], BF16, st)
    Wa = c.sb("Wa", [128, 8, 128], BF16, st)
    Wx = c.sb("Wx", [128, 8, 128], BF16, st)
    Wukv = c.sb("Wukv", [128, 2, 2048], BF16, st)
    w_in_v = G["w_in"].t[:, :].rearrange("(k p) n -> p k n", p=128)
    for k in range(8):
        c.dma("pool", lambda e, k=k: e.dma_start(out=Wxr[:, k, :], in_=w_in_v[:, k, 0:1024]), writes=[Wxr])
    c.dma("pool", lambda e: e.dma_start(out=Wkv[:], in_=w_in_v[:, :, 2304:2624]), writes=[Wkv])
    c.dma("pool", lambda e: e.dma_start(out=Wa[:], in_=G["lru_wa"].t[:, :, :].rearrange("n k j -> k n j")), writes=[Wa])
    c.dma("pool", lambda e: e.dma_start(out=Wx[:], in_=G["lru_wx"].t[:, :, :].rearrange("n k j -> k n j")), writes=[Wx])
    for kc in range(2):
        c.dma("pool", lambda e, kc=kc: e.dma_start(out=Wukv[:, kc, :], in_=G["w_ukv"].t[kc * 128:(kc + 1) * 128, :]), writes=[Wukv])
    def small(name, src, shape, dt=F32):
        t = c.sb(name, shape, dt, st)
        c.dma("sp", lambda e: e.dma_start(out=t[:], in_=src.t[tuple(slice(None) for _ in shape)]), writes=[t])
        return t
    g1 = small("g1s", G["g1"], [128, D])
    cw = small("cw", G["convw"], [128, 8, 4])
    cb = small("cb", G["convb"], [128, 8])
    ba = small("ba", G["lru_ba"], [128, 8])
    bx = small("bx", G["lru_bx"], [128, 8])
    lam = small("lam", G["lru_lam"], [128, 8])
    gkv = small("gkvs", G["gkv"], [128, 256])
    gkpe = small("gkpe", G["gqk_k_pe"], [128, 64])
    gkcol = small("gkcol", G["gqk_k_col"], [128, 1])
    invf = small("invfs", G["invf"], [128, 32])
    posi = small("posi", G["pos_s"], [128, 64], I32)
    posf = c.sb("posf", [128, 64], F32, st)
    cp(c, "dve", posf, posf[:], posi, posi[:])
    cosT = c.sb("cosT", [128, 64, 32], F32, st)
    sinT = c.sb("sinT", [128, 64, 32], F32, st)
    with ExitStack() as st2:
        trig_tables(c, st2, posf, 64, invf, cosT, sinT)
        c.barrier()
        c.flush()
    cl = c.sb("cl", [128, 8], F32, st)
    act(c, cl, cl[:], lam, lam[:], AF.Exp, scale=-1.0)
    ts(c, "dve", cl, cl[:], cl, cl[:], 1.0, None, ALU.add)
    act(c, cl, cl[:], cl, cl[:], AF.Ln)
    ts(c, "dve", cl, cl[:], cl, cl[:], -8.0, None, ALU.mult)

    xt_rot = Rot(c, "xt", [128, 4, D], F32, 2, st)
    ub = c.sb("ub", [128, 4, D], BF16, st)
    uT = c.sb("uT", [128, 8, UT], BF16, st)
    junk = c.sb("junk", [128, D], F32, st)
    ssq = c.sb("ssq", [128, 4], F32, st)
    rms = c.sb("rms", [128, 4], F32, st)
    rstd = c.sb("rstd", [128, 4], F32, st)
    pT_rot = Rot(c, "pT", [128, 8, 128], BF16, 2, st, psum=True)
    KTs_rot = Rot(c, "KTs", [128, 8, UT], BF16, 1, st)
    Vs_rot = Rot(c, "Vs", [128, 8, 128], BF16, 2, st)
    sv = c.sb("sv", [128, 16], F32, st)
    SS0 = c.sb("SS0", [128, 8], F32, st)
    ckvg = c.sb("ckvg", [128, 256], BF16, st)
    ckvT = c.sb("ckvT", [128, 2, 128], BF16, st)
    kpg = c.sb("kpg", [128, 1, 64], F32, st)
    kpr = c.sb("kpr", [128, 1, 64], F32, st)
    kpt = c.sb("kpt", [128, 1, 64], F32, st)
    kpb = c.sb("kpb", [128, 64], BF16, st)
    k0b = c.sb("k0b", [128, 8, 128], BF16, st)

    hT_v = hT_d.t[:, :].rearrange("(u c p) t -> u p c t", c=8, p=128)
    LT = []
    for ln in range(2):
        LT.append(dict(
            xc=c.sb("xcL", [128, UT], F32, st), xcb=c.sb("xcbL", [128, UT], BF16, st), rr=c.sb("rrL", [128, UT], F32, st),
            ii=c.sb("iiL", [128, UT], F32, st), aa=c.sb("aaL", [128, UT], F32, st), mm_=c.sb("mmL", [128, UT], F32, st),
            bi=c.sb("biL", [128, UT], F32, st), hf=c.sb("hfL", [128, UT], F32, st),
            p0=c.ps("pL0", [128, 512], F32, st), p1=c.ps("pL1", [128, 512], F32, st)))
    xrs = [c.sb("xrc", [128, UT + 3], F32, st) for _ in range(8)]
    carries = [c.sb("carryc", [128, 1], F32, st) for _ in range(8)]
    hTbs = [c.sb("hTbc", [128, UT], BF16, st) for _ in range(8)]
    for t_ in xrs + carries:
        c.op("dve", lambda e, t_=t_: e.memset(t_[:], 0.0), writes=[t_])
    pKV = [c.ps("pKV0", [128, 512], F32, st), c.ps("pKV1", [128, 512], F32, st)]
    junk2 = c.sb("junk2", [128, 320], F32, st)

    def rnn_chunk(ch, u, L):
        xc, xcb, rr, ii, aa, mm_, bi, hf = L["xc"], L["xcb"], L["rr"], L["ii"], L["aa"], L["mm_"], L["bi"], L["hf"]
        pa, pb = L["p0"], L["p1"]
        xr, carry, hTb = xrs[ch], carries[ch], hTbs[ch]
        for k in range(8):
            mm(c, pa, pa[:], Wxr, Wxr[:, k, ch * 128:(ch + 1) * 128], uT, uT[:, k, :], k == 0, k == 7)
        if u > 0:
            cp(c, "pool", xr, xr[:, 0:3], xr, xr[:, UT:UT + 3])
        yield
        cp(c, "act", xr, xr[:, 3:UT + 3], pa, pa[:])
        yield
        ts(c, "dve", xc, xc[:], xr, xr[:, 0:UT], cw[:, ch, 0:1], cb[:, ch:ch + 1], ALU.mult, ALU.add, extra_r=[cw, cb])
        for k in range(1, 4):
            stt(c, xc, xc[:], xr, xr[:, k:k + UT], cw[:, ch, k:k + 1], xc, xc[:], ALU.mult, ALU.add, extra_r=[cw])
        yield
        cp(c, "pool", xcb, xcb[:], xc, xc[:])
        yield
        mm(c, pa, pa[:], Wa, Wa[:, ch, :], xcb, xcb[:], True, True)
        mm(c, pb, pb[:], Wx, Wx[:, ch, :], xcb, xcb[:], True, True)
        yield
        act(c, rr, rr[:], pa, pa[:], AF.Sigmoid, extra_r=[ba], bias=ba[:, ch:ch + 1])
        act(c, ii, ii[:], pb, pb[:], AF.Sigmoid, extra_r=[bx], bias=bx[:, ch:ch + 1])
        yield
        act(c, aa, aa[:], rr, rr[:], AF.Exp, extra_r=[cl], scale=cl[:, ch:ch + 1])
        tt(c, "dve", bi, bi[:], xc, xc[:], ii, ii[:], ALU.mult)
        yield
        tt(c, "pool", mm_, mm_[:], aa, aa[:], aa, aa[:], ALU.mult)
        ts(c, "pool", mm_, mm_[:], mm_, mm_[:], -1.0, 1.0, ALU.mult, ALU.add)
        yield
        act(c, mm_, mm_[:], mm_, mm_[:], AF.Sqrt)
        yield
        tt(c, "dve", bi, bi[:], bi, bi[:], mm_, mm_[:], ALU.mult)
        c.op("dve", lambda e: e.tensor_tensor_scan(out=hf[:], data0=aa[:], data1=bi[:], initial=carry[:, 0:1],
                                                   op0=ALU.mult, op1=ALU.add), reads=[aa, bi, carry], writes=[hf])
        cp(c, "dve", carry, carry[:, 0:1], hf, hf[:, UT - 1:UT])
        yield
        cp(c, "act", hTb, hTb[:], hf, hf[:])
        c.dma("sp", lambda e: e.dma_start(out=hT_v[u][:, ch, :], in_=hTb[:]), reads=[hTb], writes=[hT_d])
        yield

    def kv_block(kb, u, KTs):
        blk = u * 4 + kb
        pc = pKV[0]
        for k in range(8):
            mm(c, pc, pc[:, 0:320], uT, uT[:, k, kb * 128:(kb + 1) * 128], Wkv, Wkv[:, k, :], k == 0, k == 7)
        yield
        act(c, junk2, junk2[:, 0:256], pc, pc[:, 0:256], AF.Square, extra_w=[sv], accum_out=sv[:, 0:1])
        act(c, junk2, junk2[:, 256:320], pc, pc[:, 256:320], AF.Square, extra_w=[sv], accum_out=sv[:, 1:2])
        yield
        rstd_from_ss(c, sv, sv[:, 0:1], 256, sv, sv[:, 2:3], sv, sv[:, 3:4])
        tt(c, "dve", ckvg, ckvg[:], pc, pc[:, 0:256], gkv, gkv[:], ALU.mult)
        tt(c, "dve", kpg, kpg[:, 0, :], pc, pc[:, 256:320], gkpe, gkpe[:], ALU.mult)
        yield
        pT = pT_rot.get()
        for kc in range(2):
            tr(c, pT, pT[:, kc, :], ckvg, ckvg[:, kc * 128:(kc + 1) * 128], ident, ident[:])
        yield
        cp(c, "dve", ckvT, ckvT[:], pT, pT[:, 0:2, :])
        rope(c, "pool", kpr, kpr[:], kpg, kpg[:], cosT, cosT[:, blk:blk + 1, :], sinT, sinT[:, blk:blk + 1, :], kpt, kpt[:], 1)
        yield
        ts(c, "dve", kpb, kpb[:], kpr, kpr[:, 0, :], sv[:, 2:3], None, ALU.mult, extra_r=[sv])
        pT2 = pT_rot.get()
        tr(c, pT2, pT2[0:64, 0, :], kpb, kpb[:], ident, ident[:])
        yield
        cp(c, "act", KP, KP[:, blk * 128:(blk + 1) * 128], pT2, pT2[0:64, 0, :])
        Vs = Vs_rot.get()
        for n in range(4):
            pk = pKV[(n + 1) % 2]
            for kc in range(2):
                mm(c, pk, pk[:], ckvT, ckvT[:, kc, :], Wukv, Wukv[:, kc, n * 512:(n + 1) * 512], kc == 0, kc == 1)
            yield
            for hh in range(2):
                h = n * 2 + hh
                act(c, junk2, junk2[:, 0:128], pk, pk[:, hh * 256:hh * 256 + 128], AF.Square, extra_w=[SS0], accum_out=SS0[:, h:h + 1])
                cp(c, "act", k0b, k0b[:, h, :], pk, pk[:, hh * 256:hh * 256 + 128])
                act(c, Vs, Vs[:, h, :], pk, pk[:, hh * 256 + 128:hh * 256 + 256], AF.Copy, extra_r=[sv], scale=sv[:, 3:4])
            yield
        c.dma("sp", lambda e, Vs=Vs: e.dma_start(
            out=V_d.t[:, u, :, kb * 128:(kb + 1) * 128].rearrange("h p d -> p h d"), in_=Vs[:]), reads=[Vs], writes=[V_d])
        pT3 = pT_rot.get()
        for h in range(8):
            tr(c, pT3, pT3[:, h, :], k0b, k0b[:, h, :], ident, ident[:])
        yield
        for h in range(8):
            act(c, KTs, KTs[:, h, kb * 128:(kb + 1) * 128], pT3, pT3[:, h, :], AF.Copy, extra_r=[gkcol], scale=gkcol[:, 0:1])
        yield
        tt(c, "dve", sv, sv[:, 4:5], sv, sv[:, 3:4], sv, sv[:, 3:4], ALU.mult)
        ts(c, "dve", SS0, SS0[:], SS0, SS0[:], sv[:, 4:5], sv[:, 1:2], ALU.mult, ALU.add, extra_r=[sv])
        ts(c, "dve", SS0, SS0[:], SS0, SS0[:], 1.0 / 192, EPS, ALU.mult, ALU.add)
        yield
        act(c, SS0, SS0[:], SS0, SS0[:], AF.Sqrt)
        yield
        c.op("dve", lambda e: e.reciprocal(out=SS0[:], in_=SS0[:]), reads=[SS0], writes=[SS0])
        ts(c, "dve", SK, SK[:, blk, :], SS0, SS0[:], sv[:, 3:4], QSCALE, ALU.mult, ALU.mult, extra_r=[sv])
        yield

    def chain(gens):
        for g in gens:
            yield from g

    for u in range(G["dbg"].get("nu", NU)):
        def load_x(uu):
            xt_ = xt_rot.get()
            src_ = xs.t[uu * UT:(uu + 1) * UT, :].rearrange("(kb p) d -> p kb d", p=128)
            c.dma("sp", lambda e: e.dma_start(out=xt_[:], in_=src_), writes=[xt_])
            return xt_
        xt_cur = load_x(0) if u == 0 else xt_nxt
        norm_transpose(c, st, None, g1, ident, xt_rot, ub, uT, pT_rot, ssq, rms, rstd, junk, xt=xt_cur)
        if u + 1 < G["dbg"].get("nu", NU):
            xt_nxt = load_x(u + 1)
        KTs = KTs_rot.get()
        lanes = [chain([rnn_chunk(ch, u, LT[0]) for ch in (0, 2, 4, 6)]),
                 chain([rnn_chunk(ch, u, LT[1]) for ch in (1, 3, 5, 7)]),
                 chain([kv_block(kb, u, KTs) for kb in range(4)])]
        while lanes:
            for g in list(lanes):
                try:
                    next(g)
                except StopIteration:
                    lanes.remove(g)
        c.dma("sp", lambda e, u=u, KTs=KTs: e.dma_start(
            out=KT_d.t[:, :, u * UT:(u + 1) * UT].rearrange("h p t -> p h t"), in_=KTs[:]), reads=[KTs], writes=[KT_d])
        if u == 0 and G["last_phase"] >= 5:
            for (src, col) in ((G["w1r"], 0), (G["w3r"], 2048), (G["w2r"], 4096)):
                for i in range(16):
                    c.dma("pool", lambda e, src=src, col=col, i=i: e.dma_start(
                        out=G["wcat_d"].t[i * 512:(i + 1) * 512, col:col + 2048], in_=src.t[i * 512:(i + 1) * 512, :]),
                        reads=[src], writes=[G["wcat_d"]])
        if u % 4 == 3:
            c.flush()
    if "dbg_d" in G["dbg"].get("dump", ()):
        dbt = c.sb("dbt", [128, 8192], F32, st)
        c.op("dve", lambda e: e.memset(dbt[:], 0.0), writes=[dbt])
        cp(c, "dve", dbt, dbt[0:64, :], KP, KP[:, :])
        c.dma("sp", lambda e: e.dma_start(out=G["dbg_d"].t[:, :], in_=dbt[:]), reads=[dbt], writes=[G["dbg_d"]])
        sk_d = G["sk_dump"]
        c.dma("sp", lambda e: e.dma_start(out=sk_d.t[:, :], in_=SK[:].rearrange("p a b -> p (a b)")), reads=[SK], writes=[sk_d])


def run_lanes(lanes):
    lanes = list(lanes)
    while lanes:
        for g in list(lanes):
            try:
                next(g)
            except StopIteration:
                lanes.remove(g)


def chain_gens(gens):
    for g in gens:
        yield from g


def load_w(c, st, name, src_ap_fn, nk, ncols):
    t = c.sb(name, [128, nk, ncols], BF16, st)
    for k in range(nk):
        c.dma("pool", lambda e, k=k: e.dma_start(out=t[:, k, :], in_=src_ap_fn(k)), writes=[t])
    return t


def small_t(c, st, name, src, shape, dt=F32):
    t = c.sb(name, shape, dt, st)
    c.dma("sp", lambda e: e.dma_start(out=t[:], in_=src.t[tuple(slice(None) for _ in shape)]), writes=[t])
    return t


def norm_tiles(c, st):
    return dict(xt_rot=Rot(c, "xt", [128, 4, D], F32, 2, st), ub=c.sb("ub", [128, 4, D], BF16, st),
                uT=c.sb("uT", [128, 8, UT], BF16, st), junk=c.sb("junk", [128, D], F32, st),
                ssq=c.sb("ssq", [128, 4], F32, st), rms=c.sb("rms", [128, 4], F32, st), rstd=c.sb("rstd", [128, 4], F32, st))


def do_norm(c, st, N, xsrc, g1, ident, pT_rot, xt=None):
    return norm_transpose(c, st, xsrc, g1, ident, N["xt_rot"], N["ub"], N["uT"], pT_rot, N["ssq"], N["rms"], N["rstd"], N["junk"], xt=xt)


def own_prefetch(c, N, xo, i, state):
    def load(ii):
        xt_ = N["xt_rot"].get()
        src_ = xo.t[ii * UT:(ii + 1) * UT, :].rearrange("(kb p) d -> p kb d", p=128)
        c.dma("sp", lambda e: e.dma_start(out=xt_[:], in_=src_), writes=[xt_])
        return xt_
    cur = load(0) if i == 0 else state["nxt"]
    state["load"] = load
    return cur


def phase2a(c, st, G):
    xo, ident, hT_d, sA_d = G["xo"], G["ident"], G["hT_d"], G["sA_d"]
    w_in_v = G["w_in"].t[:, :].rearrange("(k p) n -> p k n", p=128)
    wro_v = G["w_rnn_o"].t[:, :].rearrange("(k p) n -> p k n", p=128)
    Wy = load_w(c, st, "Wy", lambda k: w_in_v[:, k, 1024:2048], 8, 1024)
    Wga = load_w(c, st, "Wga", lambda k: w_in_v[:, k, 2624:3648], 8, 1024)
    Wro = load_w(c, st, "Wro", lambda k: wro_v[:, k, :], 8, 1024)
    g1 = small_t(c, st, "g1s", G["g1"], [128, D])
    hidx = small_t(c, st, "hidx", G["hidx"], [128, 64], I32)
    N = norm_tiles(c, st)
    uT = N["uT"]
    pT_rot = Rot(c, "pT", [128, 8, 128], BF16, 2, st, psum=True)
    pA_rot = Rot(c, "pA", [128, 512], F32, 4, st, psum=True)
    hs = c.sb("hs", [128, 8, UT], BF16, st)
    LA = [dict(ys=c.sb("ys", [128, UT], F32, st), y2=c.sb("y2", [128, UT], F32, st), sg=c.sb("sg", [128, UT], F32, st),
               p0=pA_rot.tiles[2 * ln], p1=pA_rot.tiles[2 * ln + 1]) for ln in range(2)]
    zT = c.sb("zT", [128, 8, UT], BF16, st)
    sAT = c.sb("sAT", [128, 8, UT], BF16, st)
    for i in range(NOWN):
        if i == 0:
            pf = {}
        xt = do_norm(c, st, N, None, g1, ident, pT_rot, xt=own_prefetch(c, N, xo, i, pf))
        if i + 1 < NOWN:
            pf["nxt"] = pf["load"](i + 1)
        for ch in range(8):
            c.dma("pool", lambda e, ch=ch, i=i: e.indirect_dma_start(
                out=hs[:, ch, :], out_offset=None, in_=hT_d.t[:, :],
                in_offset=bass.IndirectOffsetOnAxis(ap=hidx[:, i * 8 + ch:i * 8 + ch + 1], axis=0)),
                reads=[hidx, hT_d], writes=[hs])
        def gelu_chunk(ch, L):
            ys, y2, sg, pa = L["ys"], L["y2"], L["sg"], L["p0"]
            for k in range(8):
                mm(c, pa, pa[:], Wy, Wy[:, k, ch * 128:(ch + 1) * 128], uT, uT[:, k, :], k == 0, k == 7)
            yield
            cp(c, "act", ys, ys[:], pa, pa[:])
            yield
            tt(c, "pool", y2, y2[:], ys, ys[:], ys, ys[:], ALU.mult)
            ts(c, "pool", y2, y2[:], y2, y2[:], 0.044715, 1.0, ALU.mult, ALU.add)
            tt(c, "pool", y2, y2[:], y2, y2[:], ys, ys[:], ALU.mult)
            yield
            act(c, sg, sg[:], y2, y2[:], AF.Sigmoid, scale=1.5957691216)
            yield
            tt(c, "dve", sg, sg[:], sg, sg[:], ys, ys[:], ALU.mult)
            tt(c, "dve", zT, zT[:, ch, :], sg, sg[:], hs, hs[:, ch, :], ALU.mult)
            yield

        def a_chunk(co, L):
            sg, pa, pg = L["sg"], L["p0"], L["p1"]
            for k in range(8):
                mm(c, pa, pa[:], Wro, Wro[:, k, co * 128:(co + 1) * 128], zT, zT[:, k, :], k == 0, k == 7)
            yield
            for k in range(8):
                mm(c, pg, pg[:], Wga, Wga[:, k, co * 128:(co + 1) * 128], uT, uT[:, k, :], k == 0, k == 7)
            yield
            act(c, sg, sg[:], pg, pg[:], AF.Sigmoid)
            yield
            tt(c, "dve", sAT, sAT[:, co, :], pa, pa[:], sg, sg[:], ALU.mult)
            yield

        run_lanes([chain_gens([gelu_chunk(ch, LA[ln]) for ch in range(ln, 8, 2)]) for ln in range(2)])
        run_lanes([chain_gens([a_chunk(co, LA[ln]) for co in range(ln, 8, 2)]) for ln in range(2)])
        c.dma("sp", lambda e, i=i: e.dma_start(out=sA_d.t[i], in_=sAT[:].rearrange("p a b -> p (a b)")), reads=[sAT], writes=[sA_d])
        c.flush()


def phase2b(c, st, G):
    xo, ident, KP, SK = G["xo"], G["ident"], G["KP"], G["SK"]
    KT_d, V_d, OT_d, masks = G["KT_d"], G["V_d"], G["OT_d"], G["masks"]
    w_in_v = G["w_in"].t[:, :].rearrange("(k p) n -> p k n", p=128)
    Wcq = load_w(c, st, "Wcq", lambda k: w_in_v[:, k, 2048:2304], 8, 256)
    Wuq = load_w(c, st, "Wuq", lambda k: G["w_uq"].t[k * 128:(k + 1) * 128, :], 2, 1536)
    g1 = small_t(c, st, "g1s", G["g1"], [128, D])
    gq = small_t(c, st, "gqs", G["gq"], [128, 256])
    gqk = small_t(c, st, "gqk", G["gqk_q"], [128, 192])
    invf = small_t(c, st, "invfs", G["invf"], [128, 32])
    posi = small_t(c, st, "posi", G["pos_o"], [128, 32], I32)
    posf = c.sb("posf", [128, 32], F32, st)
    cp(c, "dve", posf, posf[:], posi, posi[:])
    cosO = c.sb("cosO", [128, 32, 32], F32, st)
    sinO = c.sb("sinO", [128, 32, 32], F32, st)
    with ExitStack() as st2:
        trig_tables(c, st2, posf, 32, invf, cosO, sinO)
        c.barrier()
        c.flush()
    N = norm_tiles(c, st)
    uT = N["uT"]
    junk = N["junk"]
    pT_rot = Rot(c, "pT", [128, 8, 128], BF16, 1, st, psum=True)
    pS_rot = Rot(c, "pS", [128, 512], F32, 3, st, psum=True)
    pO = [c.ps("pO%d" % q, [128, 512], F32, st) for q in range(4)]
    onesb = c.sb("onesb2", [128, 128], BF16, st)
    c.op("dve", lambda e: e.memset(onesb[:], 1.0), writes=[onesb])
    rinv = c.sb("rinv", [128, 512], F32, st)
    sv = c.sb("sv", [128, 16], F32, st)
    SSQ = c.sb("SSQ", [128, 8], F32, st)
    FQ = c.sb("FQ", [128, 8], F32, st)
    cqg = c.sb("cqg", [128, 256], BF16, st)
    cqT = c.sb("cqT", [128, 2, 128], BF16, st)
    q0s = c.sb("q0s", [128, 8, 192], F32, st)
    qr = c.sb("qr", [128, 8, 64], F32, st)
    qtmp = c.sb("qtmp", [128, 8, 64], F32, st)
    qbn = c.sb("qbn", [128, 8, 128], BF16, st)
    qbp = c.sb("qbp", [128, 8, 64], BF16, st)
    QT = c.sb("QT", [128, 8, UT], BF16, st)
    QP = c.sb("QP", [64, 8, UT], BF16, st)
    msk = [c.sb("msk%d" % w, [128, 4, 512], BF16, st) for w in range(2)]
    KT_rot = Rot(c, "KTt", [128, 512], BF16, 3, st)
    V_rot = Rot(c, "Vt", [128, 4, 130], BF16, 3, st)
    for vt in V_rot.tiles:
        c.op("dve", lambda e, vt=vt: e.memset(vt[:], 1.0), writes=[vt])
    PT_rot = Rot(c, "PT", [128, 512], BF16, 3, st)
    rs = c.sb("rs", [128, 4], F32, st)
    ob = c.sb("ob", [128, 128], BF16, st)
    OT = c.sb("OT", [128, 8, UT], BF16, st)
    q0f = q0s[:].rearrange("p a b -> p (a b)")
    for i in range(NOWN):
        if i == 0:
            pf = {}
        xt = do_norm(c, st, N, None, g1, ident, pT_rot, xt=own_prefetch(c, N, xo, i, pf))
        if i + 1 < NOWN:
            pf["nxt"] = pf["load"](i + 1)
        for w in range(2):
            c.dma("sp", lambda e, w=w, i=i: e.dma_start(out=msk[w][:].rearrange("p a b -> p (a b)"), in_=masks.t[i, w]), writes=[msk[w]])
        for kb in range(4):
            blk = i * 4 + kb
            pc = pO[0]
            for k in range(8):
                mm(c, pc, pc[:, 0:256], uT, uT[:, k, kb * 128:(kb + 1) * 128], Wcq, Wcq[:, k, :], k == 0, k == 7)
            act(c, junk, junk[:, 0:256], pc, pc[:, 0:256], AF.Square, extra_w=[sv], accum_out=sv[:, 0:1])
            rstd_from_ss(c, sv, sv[:, 0:1], 256, sv, sv[:, 2:3], sv, sv[:, 3:4])
            tt(c, "dve", cqg, cqg[:], pc, pc[:, 0:256], gq, gq[:], ALU.mult)
            pT = pT_rot.get()
            for kc in range(2):
                tr(c, pT, pT[:, kc, :], cqg, cqg[:, kc * 128:(kc + 1) * 128], ident, ident[:])
            cp(c, "dve", cqT, cqT[:], pT, pT[:, 0:2, :])
            for n in range(3):
                pq = pO[1 + n]
                for kc in range(2):
                    mm(c, pq, pq[:], cqT, cqT[:, kc, :], Wuq, Wuq[:, kc, n * 512:(n + 1) * 512], kc == 0, kc == 1)
                cp(c, "act", q0s, q0f[:, n * 512:(n + 1) * 512], pq, pq[:])
            for h in range(8):
                act(c, junk, junk[:, 0:192], q0s, q0s[:, h, :], AF.Square, extra_w=[SSQ], accum_out=SSQ[:, h:h + 1])
            tt(c, "dve", sv, sv[:, 4:5], sv, sv[:, 3:4], sv, sv[:, 3:4], ALU.mult)
            ts(c, "dve", FQ, FQ[:], SSQ, SSQ[:], sv[:, 4:5], 1.0 / 192, ALU.mult, ALU.mult, extra_r=[sv])
            ts(c, "dve", FQ, FQ[:], FQ, FQ[:], EPS, None, ALU.add)
            act(c, FQ, FQ[:], FQ, FQ[:], AF.Sqrt)
            c.op("dve", lambda e: e.reciprocal(out=FQ[:], in_=FQ[:]), reads=[FQ], writes=[FQ])
            ts(c, "dve", FQ, FQ[:], FQ, FQ[:], sv[:, 3:4], None, ALU.mult, extra_r=[sv])
            tt(c, "dve", q0s, q0s[:], q0s, q0s[:], FQ, FQ[:].unsqueeze(2).to_broadcast([128, 8, 192]), ALU.mult)
            tt(c, "pool", q0s, q0s[:], q0s, q0s[:], gqk, gqk[:].unsqueeze(1).to_broadcast([128, 8, 192]), ALU.mult)
            rope(c, "pool", qr, qr[:], q0s, q0s[:, :, 128:192], cosO, cosO[:, blk:blk + 1, :].to_broadcast([128, 8, 32]),
                 sinO, sinO[:, blk:blk + 1, :].to_broadcast([128, 8, 32]), qtmp, qtmp[:], 8)
            cp(c, "dve", qbn, qbn[:], q0s, q0s[:, :, 0:128])
            cp(c, "dve", qbp, qbp[:], qr, qr[:])
            pT = pT_rot.get()
            for h in range(8):
                tr(c, pT, pT[:, h, :], qbn, qbn[:, h, :], ident, ident[:])
            cp(c, "dve", QT, QT[:, :, kb * 128:(kb + 1) * 128], pT, pT[:])
            pT = pT_rot.get()
            for h in range(8):
                tr(c, pT, pT[0:64, h, :], qbp, qbp[:, h, :], ident, ident[:])
            cp(c, "dve", QP, QP[:, :, kb * 128:(kb + 1) * 128], pT, pT[0:64, :, :])
        E = EXT[i]
        for h in range(8):
            tiles = [(ku, kb) for ku in range(E) for kb in range(4)]
            kv = {}

            def load_kv(ku, h=h):
                KTt = KT_rot.get()
                Vt = V_rot.get()
                c.dma("sp", lambda e, KTt=KTt: e.dma_start(out=KTt[:], in_=KT_d.t[h, :, ku * 512:(ku + 1) * 512]),
                      reads=[KT_d], writes=[KTt])
                c.dma("sp", lambda e, Vt=Vt: e.dma_start(
                    out=Vt[:, :, 0:128], in_=V_d.t[h, ku].rearrange("p (kb d) -> p kb d", d=128)), reads=[V_d], writes=[Vt])
                kv[ku] = (KTt, Vt)

            def qk(t, h=h):
                ku, kb = tiles[t]
                if kb == 0 and ku + 1 < E:
                    load_kv(ku + 1)
                KTt = kv[ku][0]
                kblk = ku * 4 + kb
                pS = pS_rot.get()
                mm(c, pS, pS[:], KTt, KTt[:, kb * 128:(kb + 1) * 128], QT, QT[:, h, :], True, False)
                mm(c, pS, pS[:], KP, KP[:, kblk * 128:(kblk + 1) * 128], QP, QP[:, h, :], False, True)
                return pS

            load_kv(0)
            pSq = [qk(0)]
            if len(tiles) > 1:
                pSq.append(qk(1))
            for t in range(len(tiles)):
                ku, kb = tiles[t]
                kblk = ku * 4 + kb
                pS = pSq.pop(0)
                if t + 2 < len(tiles):
                    pSq.append(qk(t + 2))
                Vt = kv[ku][1]
                PT = PT_rot.get()
                act(c, PT, PT[:], pS, pS[:], AF.Exp, extra_r=[SK], scale=SK[:, kblk, h:h + 1])
                if ku >= E - 2:
                    w = ku - (E - 2)
                    tt(c, "pool", PT, PT[:], PT, PT[:], msk[w], msk[w][:, kb, :], ALU.mult)
                pOT, pRS = pO[2 * (h % 2)], pO[2 * (h % 2) + 1]
                mm(c, pOT, pOT[:], Vt, Vt[:, kb, 0:128], PT, PT[:], t == 0, t == len(tiles) - 1)
                mm(c, pRS, pRS[:], onesb, onesb[:], PT, PT[:], t == 0, t == len(tiles) - 1)
            pOT, pRS = pO[2 * (h % 2)], pO[2 * (h % 2) + 1]
            c.op("dve", lambda e, pRS=pRS: e.reciprocal(out=rinv[:], in_=pRS[:]), reads=[pRS], writes=[rinv])
            tt(c, "dve", OT, OT[:, h, :], pOT, pOT[:], rinv, rinv[:], ALU.mult)
            c.flush()
        c.dma("sp", lambda e, i=i: e.dma_start(out=OT_d.t[i], in_=OT[:].rearrange("p a b -> p (a b)")), reads=[OT], writes=[OT_d])


def phase2c(c, st, G):
    xo, ident, OT_d, sA_d, h_d, u2_d = G["xo"], G["ident"], G["OT_d"], G["sA_d"], G["h_d"], G["u2_d"]
    S1all, S2all, GT = G["S1all"], G["S2all"], G["GT"]
    w_in_v = G["w_in"].t[:, :].rearrange("(k p) n -> p k n", p=128)
    wmo_v = G["w_mla_o"].t[:, :].rearrange("(k p) n -> p k n", p=128)
    wo_v = G["w_out"].t[:, :].rearrange("(k p) n -> p k n", p=128)
    wr_v = G["wr"].t[:, :].rearrange("(k p) n -> p k n", p=128)
    Wgb = load_w(c, st, "Wgb", lambda k: w_in_v[:, k, 3648:4672], 8, 1024)
    Wmo = load_w(c, st, "Wmo", lambda k: wmo_v[:, k, :], 8, 1024)
    Wo = load_w(c, st, "Wo", lambda k: wo_v[:, k, :], 8, 1024)
    Wr = load_w(c, st, "Wr", lambda k: wr_v[:, k, :], 8, 72)
    g1 = small_t(c, st, "g1s", G["g1"], [128, D])
    g2 = small_t(c, st, "g2s", G["g2"], [128, D])
    brt = small_t(c, st, "brt", G["br"], [128, 72])
    N = norm_tiles(c, st)
    uT = N["uT"]
    junk = N["junk"]
    pT_rot = Rot(c, "pT", [128, 8, 128], BF16, 2, st, psum=True)
    pA_rot = Rot(c, "pA", [128, 512], F32, 4, st, psum=True)
    OT = c.sb("OT", [128, 8, UT], BF16, st)
    sAT = c.sb("sAT", [128, 8, UT], BF16, st)
    LC = [dict(sg=c.sb("sg", [128, UT], F32, st), p0=pA_rot.tiles[2 * ln], p1=pA_rot.tiles[2 * ln + 1]) for ln in range(2)]
    mT = c.sb("mT", [128, 8, UT], BF16, st)
    hrow = c.sb("hrow", [128, D], F32, st)
    u2b = c.sb("u2b", [128, D], BF16, st)
    u2T = c.sb("u2T", [128, 8, UT], BF16, st)
    sv = c.sb("sv", [128, 16], F32, st)
    lg = c.sb("lg", [128, 72], F32, st)
    m8 = c.sb("m8", [128, 8], F32, st)
    oh = c.sb("oh", [128, 8], F32, st)
    em = c.sb("em", [128, 8, 8], F32, st)
    s1 = c.sb("s1", [128, 64], F32, st)
    s2 = c.sb("s2", [128, 64], F32, st)
    emf = em[:].rearrange("p a b -> p (a b)")
    for i in range(NOWN):
        if i == 0:
            pf = {}
        xt = do_norm(c, st, N, None, g1, ident, pT_rot, xt=own_prefetch(c, N, xo, i, pf))
        if i + 1 < NOWN:
            pf["nxt"] = pf["load"](i + 1)
        c.dma("sp", lambda e, i=i: e.dma_start(out=OT[:].rearrange("p a b -> p (a b)"), in_=OT_d.t[i]), reads=[OT_d], writes=[OT])
        c.dma("sp", lambda e, i=i: e.dma_start(out=sAT[:].rearrange("p a b -> p (a b)"), in_=sA_d.t[i]), reads=[sA_d], writes=[sAT])
        def merge_chunk(co, L):
            sg, pb, pg = L["sg"], L["p0"], L["p1"]
            for k in range(8):
                mm(c, pb, pb[:], Wmo, Wmo[:, k, co * 128:(co + 1) * 128], OT, OT[:, k, :], k == 0, k == 7)
            yield
            for k in range(8):
                mm(c, pg, pg[:], Wgb, Wgb[:, k, co * 128:(co + 1) * 128], uT, uT[:, k, :], k == 0, k == 7)
            yield
            act(c, sg, sg[:], pg, pg[:], AF.Sigmoid)
            yield
            tt(c, "dve", sg, sg[:], pb, pb[:], sg, sg[:], ALU.mult)
            tt(c, "dve", mT, mT[:, co, :], sg, sg[:], sAT, sAT[:, co, :], ALU.add)
            yield

        run_lanes([chain_gens([merge_chunk(co, LC[ln]) for co in range(ln, 8, 2)]) for ln in range(2)])
        for kb in range(4):
            blk = i * 4 + kb
            for n in range(2):
                ph = pA_rot.get()
                for k in range(8):
                    mm(c, ph, ph[:], mT, mT[:, k, kb * 128:(kb + 1) * 128], Wo, Wo[:, k, n * 512:(n + 1) * 512], k == 0, k == 7)
                tt(c, "dve", hrow, hrow[:, n * 512:(n + 1) * 512], ph, ph[:], xt, xt[:, kb, n * 512:(n + 1) * 512], ALU.add)
            c.dma("sp", lambda e, blk=blk: e.dma_start(out=h_d.t[blk * 128:(blk + 1) * 128, :], in_=hrow[:]), reads=[hrow], writes=[h_d])
            act(c, junk, junk[:], hrow, hrow[:], AF.Square, extra_w=[sv], accum_out=sv[:, 0:1])
            rstd_from_ss(c, sv, sv[:, 0:1], D, sv, sv[:, 2:3], sv, sv[:, 3:4])
            stt(c, u2b, u2b[:], hrow, hrow[:], sv[:, 3:4], g2, g2[:], ALU.mult, ALU.mult, extra_r=[sv])
            c.dma("sp", lambda e, blk=blk: e.dma_start(out=u2_d.t[blk * 128:(blk + 1) * 128, :], in_=u2b[:]), reads=[u2b], writes=[u2_d])
            pT = pT_rot.get()
            for k in range(8):
                tr(c, pT, pT[:, k, :], u2b, u2b[:, k * 128:(k + 1) * 128], ident, ident[:])
            cp(c, "dve", u2T, u2T[:, :, kb * 128:(kb + 1) * 128], pT, pT[:])
            pl = pA_rot.get()
            for k in range(8):
                mm(c, pl, pl[:, 0:72], u2T, u2T[:, k, kb * 128:(kb + 1) * 128], Wr, Wr[:, k, :], k == 0, k == 7)
            tt(c, "dve", lg, lg[:], pl, pl[:, 0:72], brt, brt[:], ALU.add)
            c.op("dve", lambda e: e.max(out=m8[:], in_=lg[:, 0:8]), reads=[lg], writes=[m8])
            ts(c, "dve", oh, oh[:], lg, lg[:, 0:8], m8[:, 0:1], None, ALU.is_ge, extra_r=[m8])
            ts(c, "dve", sv, sv[:, 5:6], m8, m8[:, 0:1], -1.0, None, ALU.mult)
            act(c, junk, junk[:, 0:8], lg, lg[:, 0:8], AF.Exp, extra_r=[sv], extra_w=[sv], bias=sv[:, 5:6], accum_out=sv[:, 6:7])
            c.op("dve", lambda e: e.reciprocal(out=sv[:, 7:8], in_=sv[:, 6:7]), reads=[sv], writes=[sv])
            ts(c, "dve", oh, oh[:], oh, oh[:], -1.0, 1e9, ALU.add, ALU.mult)
            tt(c, "dve", em, em[:], lg, lg[:, 8:72].rearrange("p (a b) -> p a b", b=8), oh, oh[:].unsqueeze(2).to_broadcast([128, 8, 8]), ALU.add)
            c.op("dve", lambda e: e.max(out=m8[:], in_=emf), reads=[em], writes=[m8])
            ts(c, "dve", s1, s1[:], em, emf, m8[:, 0:1], None, ALU.is_ge, extra_r=[m8])
            ts(c, "dve", s2, s2[:], em, emf, m8[:, 1:2], None, ALU.is_ge, extra_r=[m8])
            tt(c, "dve", sv, sv[:, 8:9], m8, m8[:, 0:1], m8, m8[:, 1:2], ALU.subtract)
            act(c, sv, sv[:, 9:10], sv, sv[:, 8:9], AF.Sigmoid)
            tt(c, "dve", sv, sv[:, 10:11], sv, sv[:, 9:10], sv, sv[:, 7:8], ALU.mult)
            tt(c, "dve", sv, sv[:, 11:12], sv, sv[:, 7:8], sv, sv[:, 10:11], ALU.subtract)
            tt(c, "dve", sv, sv[:, 12:13], sv, sv[:, 10:11], sv, sv[:, 11:12], ALU.subtract)
            cp(c, "dve", S1all, S1all[:, blk, :], s1, s1[:])
            tt(c, "dve", S2all, S2all[:, blk, :], s2, s2[:], s1, s1[:], ALU.subtract)
            cp(c, "dve", GT, GT[:, blk, 0:2], sv, sv[:, 10:12])
        c.flush()


def phase3(c, st, G):
    ident, h_d, u2_d, out = G["ident"], G["h_d"], G["u2_d"], G["out"]
    S1all, S2all, GT = G["S1all"], G["S2all"], G["GT"]
    xpad_d, ypad_d, w1r, w3r, w2r = G["xpad_d"], G["ypad_d"], G["w1r"], G["w3r"], G["w2r"]
    ustr = c.sb("ustr", [128, 128], BF16, st)
    c.dma("pool", lambda e: e.dma_start(out=ustr[:], in_=G["ustrict"].t[:, :]), writes=[ustr])
    onesb = c.sb("onesb", [128, 128], BF16, st)
    c.op("dve", lambda e: e.memset(onesb[:], 1.0), writes=[onesb])
    thr = small_t(c, st, "thr", G["thr"], [128, 128])
    pidx = small_t(c, st, "pidx", G["pidx"], [128, 1])
    pA_rot = Rot(c, "pA3", [128, 512], F32, 2, st, psum=True)
    A = c.sb("A3", [128, 64], BF16, st)
    Acum = c.sb("Acum", [128, 64], BF16, st)
    c.op("dve", lambda e: e.memset(Acum[:], 0.0), writes=[Acum])
    RK = c.sb("RK", [128, 32, 64], F32, st)
    for b in range(32):
        tt(c, "dve", A, A[:], S1all, S1all[:, b, :], S2all, S2all[:, b, :], ALU.add)
        pr = pA_rot.get()
        mm(c, pr, pr[:, 0:64], ustr, ustr[:], A, A[:], True, False)
        mm(c, pr, pr[:, 0:64], onesb, onesb[:], Acum, Acum[:], False, True)
        cp(c, "act", RK, RK[:, b, :], pr, pr[:, 0:64])
        tt(c, "dve", Acum, Acum[:], Acum, Acum[:], A, A[:], ALU.add)
    pr = pA_rot.get()
    mm(c, pr, pr[:, 0:64], onesb, onesb[:], Acum, Acum[:], True, True)
    tot = c.sb("tot", [128, 64], F32, st)
    f1 = c.sb("f1", [128, 64], F32, st)
    f2 = c.sb("f2", [128, 64], F32, st)
    ki = c.sb("ki3", [128, 64], I32, st)
    cp(c, "act", tot, tot[:], pr, pr[:, 0:64])
    ts(c, "dve", f1, f1[:], tot, tot[:], 127.0, 1.0 / 128, ALU.add, ALU.mult)
    cp(c, "dve", ki, ki[:], f1, f1[:])
    cp(c, "dve", f2, f2[:], ki, ki[:])
    tt(c, "dve", f1, f1[:], f2, f2[:], f1, f1[:], ALU.is_gt)
    tt(c, "dve", f2, f2[:], f2, f2[:], f1, f1[:], ALU.subtract)
    padded = c.sb("padded", [128, 64], F32, st)
    ts(c, "dve", padded, padded[:], f2, f2[:], 128.0, None, ALU.mult)
    pend = c.sb("pend", [128, 64], F32, st)
    pstart = c.sb("pstart", [128, 64], F32, st)
    ones64 = c.sb("ones64", [128, 64], F32, st)
    c.op("dve", lambda e: e.memset(ones64[:], 1.0), writes=[ones64])
    c.op("dve", lambda e: e.tensor_tensor_scan(out=pend[:], data0=ones64[:], data1=padded[:], initial=0.0, op0=ALU.mult, op1=ALU.add),
         reads=[ones64, padded], writes=[pend])
    tt(c, "dve", pstart, pstart[:], pend, pend[:], padded, padded[:], ALU.subtract)
    DST = c.sb("DST", [128, 64], F32, st)
    DSTi = c.sb("DSTi", [128, 64], I32, st)
    junk = c.sb("junk3", [128, 64], F32, st)
    for b in range(32):
        tt(c, "dve", f1, f1[:], RK, RK[:, b, :], pstart, pstart[:], ALU.add)
        stt(c, junk, junk[:], f1, f1[:], 1.0, S1all, S1all[:, b, :], ALU.mult, ALU.mult, extra_w=[DST], accum_out=DST[:, 2 * b:2 * b + 1])
        stt(c, junk, junk[:], f1, f1[:], 1.0, S2all, S2all[:, b, :], ALU.mult, ALU.mult, extra_w=[DST], accum_out=DST[:, 2 * b + 1:2 * b + 2])
    cp(c, "dve", DSTi, DSTi[:], DST, DST[:])
    u2r_rot = Rot(c, "u2r", [128, D], BF16, 2, st)
    for b in range(32):
        u2r = u2r_rot.get()
        c.dma("sp", lambda e, b=b, u2r=u2r: e.dma_start(out=u2r[:], in_=u2_d.t[b * 128:(b + 1) * 128, :]), reads=[u2_d], writes=[u2r])
        for sl in range(2):
            c.dma("pool", lambda e, b=b, sl=sl, u2r=u2r: e.indirect_dma_start(
                out=xpad_d.t[:, :], out_offset=bass.IndirectOffsetOnAxis(ap=DSTi[:, 2 * b + sl:2 * b + sl + 1], axis=0),
                in_=u2r[:], in_offset=None), reads=[u2r, DSTi], writes=[xpad_d])
    cmp = c.sb("cmp3", [128, 128, 64], F32, st)
    tt(c, "dve", cmp, cmp[:], pend, pend[:].unsqueeze(1).to_broadcast([128, 128, 64]),
       thr, thr[:].unsqueeze(2).to_broadcast([128, 128, 64]), ALU.is_le)
    BE = c.sb("BE", [128, 128], F32, st)
    c.op("dve", lambda e: e.tensor_reduce(out=BE[:], in_=cmp[:], axis=mybir.AxisListType.X, op=ALU.add), reads=[cmp], writes=[BE])
    ts(c, "dve", BE, BE[:], BE, BE[:], 63.0, 128.0, ALU.min, ALU.mult)
    ts(c, "dve", BE, BE[:], BE, BE[:], pidx[:, 0:1], None, ALU.add, extra_r=[pidx])
    WIDX = c.sb("WIDX", [128, 128], I32, st)
    cp(c, "dve", WIDX, WIDX[:], BE, BE[:])
    c.flush()
    Wc_rot = Rot(c, "Wcat", [128, 6144], BF16, 3, st)
    wcat_d = G["wcat_d"]
    xr_rot = Rot(c, "xrow", [128, D], BF16, 3, st)
    xT = c.sb("xT3", [128, 8, 128], BF16, st)
    pH1 = Rot(c, "pH1", [128, 512], F32, 1, st, psum=True)
    pH3 = Rot(c, "pH3", [128, 512], F32, 1, st, psum=True)
    pY_rot = Rot(c, "pY", [128, 512], F32, 2, st, psum=True)
    pT_rot = Rot(c, "pT3", [128, 8, 128], BF16, 2, st, psum=True)
    sg = c.sb("sg3", [128, 256], F32, st)
    hg = c.sb("hg3", [128, 256], BF16, st)
    hgT = c.sb("hgT3", [128, 2, 128], BF16, st)
    yrow_rot = Rot(c, "yrow", [128, D], F32, 2, st)
    stg = {}

    def fetch(i):
        Wc, xr = Wc_rot.get(), xr_rot.get()
        c.dma("pool", lambda e, Wc=Wc, i=i: e.indirect_dma_start(
            out=Wc[:], out_offset=None, in_=wcat_d.t[:, :],
            in_offset=bass.IndirectOffsetOnAxis(ap=WIDX[:, i:i + 1], axis=0)), reads=[WIDX, wcat_d], writes=[Wc])
        c.dma("sp", lambda e, xr=xr, i=i: e.dma_start(out=xr[:], in_=xpad_d.t[i * 128:(i + 1) * 128, :]), reads=[xpad_d], writes=[xr])
        stg[i] = (Wc, xr)

    fetch(0)
    fetch(1)
    for i in range(NBLK):
        if i + 2 < NBLK:
            fetch(i + 2)
        Wc, xr = stg.pop(i)
        pT = pT_rot.get()
        for k in range(8):
            tr(c, pT, pT[:, k, :], xr, xr[:, k * 128:(k + 1) * 128], ident, ident[:])
        cp(c, "dve", xT, xT[:], pT, pT[:])
        p1 = pH1.get()
        p3 = pH3.get()
        for k in range(8):
            mm(c, p1, p1[:, 0:256], xT, xT[:, k, :], Wc, Wc[:, k * 256:(k + 1) * 256], k == 0, k == 7)
        for k in range(8):
            mm(c, p3, p3[:, 0:256], xT, xT[:, k, :], Wc, Wc[:, 2048 + k * 256:2048 + (k + 1) * 256], k == 0, k == 7)
        act(c, sg, sg[:], p1, p1[:, 0:256], AF.Sigmoid)
        tt(c, "dve", sg, sg[:], p1, p1[:, 0:256], sg, sg[:], ALU.mult)
        tt(c, "dve", hg, hg[:], p3, p3[:, 0:256], sg, sg[:], ALU.mult)
        pT = pT_rot.get()
        for kc in range(2):
            tr(c, pT, pT[:, kc, :], hg, hg[:, kc * 128:(kc + 1) * 128], ident, ident[:])
        cp(c, "dve", hgT, hgT[:], pT, pT[:, 0:2, :])
        yrow = yrow_rot.get()
        for n in range(2):
            py = pY_rot.get()
            for kc in range(2):
                mm(c, py, py[:], hgT, hgT[:, kc, :], Wc, Wc[:, 4096 + kc * 1024 + n * 512:4096 + kc * 1024 + (n + 1) * 512], kc == 0, kc == 1)
            cp(c, "act" if n == 0 else "dve", yrow, yrow[:, n * 512:(n + 1) * 512], py, py[:])
        c.dma("sp", lambda e, i=i, yrow=yrow: e.dma_start(out=ypad_d.t[i * 128:(i + 1) * 128, :], in_=yrow[:]), reads=[yrow], writes=[ypad_d])
        if i % 16 == 15:
            c.flush()
    y1_rot = Rot(c, "y1", [128, D], F32, 2, st)
    y2_rot = Rot(c, "y2", [128, D], F32, 2, st)
    hr_rot = Rot(c, "hr3", [128, D], F32, 2, st)
    for b in range(32):
        y1, y2, hr = y1_rot.get(), y2_rot.get(), hr_rot.get()
        for (dst, sl) in ((y1, 0), (y2, 1)):
            c.dma("pool", lambda e, dst=dst, sl=sl, b=b: e.indirect_dma_start(
                out=dst[:], out_offset=None, in_=ypad_d.t[:, :],
                in_offset=bass.IndirectOffsetOnAxis(ap=DSTi[:, 2 * b + sl:2 * b + sl + 1], axis=0)), reads=[DSTi, ypad_d], writes=[dst])
        c.dma("sp", lambda e, b=b, hr=hr: e.dma_start(out=hr[:], in_=h_d.t[b * 128:(b + 1) * 128, :]), reads=[h_d], writes=[hr])
        stt(c, hr, hr[:], y1, y1[:], GT[:, b, 0:1], hr, hr[:], ALU.mult, ALU.add, extra_r=[GT])
        stt(c, hr, hr[:], y2, y2[:], GT[:, b, 1:2], hr, hr[:], ALU.mult, ALU.add, extra_r=[GT])
        c.dma("sp", lambda e, b=b, hr=hr: e.dma_start(out=out.t[b * 128:(b + 1) * 128, :], in_=hr[:]), reads=[hr], writes=[out])
    c.flush()


def phase3_dense(c, st, G):
    ident, h_d, u2T_d, Gall, out = G["ident"], G["h_d"], G["u2T_d"], G["Gall"], G["out"]
    w1r, w3r, w2r = G["w1r"], G["w3r"], G["w2r"]
    NB = 16
    yacc = c.sb("yacc", [128, NB, D], F32, st)
    u2T = c.sb("u2Ta", [128, 8, NB * 128], BF16, st)
    W13_rot = Rot(c, "W13", [128, 8, 512], BF16, 2, st)
    W2_rot = Rot(c, "W2", [128, 2, 1024], BF16, 2, st)
    pH_rot = Rot(c, "pH", [128, 512], F32, 2, st, psum=True)
    pY_rot = Rot(c, "pY", [128, 512], F32, 4, st, psum=True)
    pT_rot = Rot(c, "pT", [128, 8, 128], BF16, 2, st, psum=True)
    sg = c.sb("sg3", [128, 256], F32, st)
    hg = c.sb("hg", [128, 256], BF16, st)
    hgT = c.sb("hgT", [128, 2, 128], BF16, st)
    hrow = c.sb("hrow3", [128, D], F32, st)
    for half in range(2):
        for j in range(4):
            c.dma("sp", lambda e, j=j, half=half: e.dma_start(
                out=u2T[:, :, j * 512:(j + 1) * 512], in_=u2T_d.t[half * 4 + j].rearrange("p (a b) -> p a b", b=512)),
                reads=[u2T_d], writes=[u2T])
        for ex in range(64):
            W13 = W13_rot.get()
            W2 = W2_rot.get()
            c.dma("pool", lambda e, ex=ex, W13=W13: e.dma_start(
                out=W13[:, :, 0:256], in_=w1r.t[ex * 128:(ex + 1) * 128, :].rearrange("p (a b) -> p a b", b=256)), writes=[W13])
            c.dma("pool", lambda e, ex=ex, W13=W13: e.dma_start(
                out=W13[:, :, 256:512], in_=w3r.t[ex * 128:(ex + 1) * 128, :].rearrange("p (a b) -> p a b", b=256)), writes=[W13])
            c.dma("pool", lambda e, ex=ex, W2=W2: e.dma_start(
                out=W2[:].rearrange("p a b -> p (a b)"), in_=w2r.t[ex * 128:(ex + 1) * 128, :]), writes=[W2])
            for b in range(NB):
                blk = half * NB + b
                ph = pH_rot.get()
                for k in range(8):
                    mm(c, ph, ph[:], u2T, u2T[:, k, b * 128:(b + 1) * 128], W13, W13[:, k, :], k == 0, k == 7)
                act(c, sg, sg[:], ph, ph[:, 0:256], AF.Sigmoid)
                tt(c, "dve", sg, sg[:], ph, ph[:, 0:256], sg, sg[:], ALU.mult)
                tt(c, "dve", sg, sg[:], ph, ph[:, 256:512], sg, sg[:], ALU.mult)
                ts(c, "dve", hg, hg[:], sg, sg[:], Gall[:, blk, ex:ex + 1], None, ALU.mult, extra_r=[Gall])
                pT = pT_rot.get()
                for kc in range(2):
                    tr(c, pT, pT[:, kc, :], hg, hg[:, kc * 128:(kc + 1) * 128], ident, ident[:])
                cp(c, "dve", hgT, hgT[:], pT, pT[:, 0:2, :])
                for n in range(2):
                    py = pY_rot.get()
                    for kc in range(2):
                        mm(c, py, py[:], hgT, hgT[:, kc, :], W2, W2[:, kc, n * 512:(n + 1) * 512], kc == 0, kc == 1)
                    if ex == 0:
                        cp(c, "act", yacc, yacc[:, b, n * 512:(n + 1) * 512], py, py[:])
                    else:
                        tt(c, "dve", yacc, yacc[:, b, n * 512:(n + 1) * 512], py, py[:], yacc, yacc[:, b, n * 512:(n + 1) * 512], ALU.add)
            if ex % 4 == 3:
                c.flush()
        for b in range(NB):
            blk = half * NB + b
            c.dma("sp", lambda e, blk=blk: e.dma_start(out=hrow[:], in_=h_d.t[blk * 128:(blk + 1) * 128, :]), reads=[h_d], writes=[hrow])
            tt(c, "dve", yacc, yacc[:, b, :], yacc, yacc[:, b, :], hrow, hrow[:], ALU.add)
            c.dma("sp", lambda e, blk=blk, b=b: e.dma_start(out=out.t[blk * 128:(blk + 1) * 128, :], in_=yacc[:, b, :]), reads=[yacc], writes=[out])
        c.flush()


def _prep_core(inp, cidx):
    b, j = cidx // 2, cidx % 2
    f = np.float32
    x = inp["x"]
    own = OWN[j]
    rep = lambda v, n=128: np.ascontiguousarray(np.broadcast_to(np.asarray(v, f).reshape(1, -1), (n, np.asarray(v).size)))
    colmaj = lambda v: np.ascontiguousarray(np.asarray(v, f).reshape(8, 128).T)
    m = {}
    m["xs"] = np.ascontiguousarray(x[b])
    m["xo"] = np.ascontiguousarray(np.concatenate([x[b, u * UT:(u + 1) * UT] for u in own], 0))
    pos = np.asarray(inp["positions"][b], np.int32)
    m["pos_s"] = np.ascontiguousarray(pos.reshape(64, 128).T)
    pown = np.concatenate([pos[u * UT:(u + 1) * UT] for u in own])
    m["pos_o"] = np.ascontiguousarray(pown.reshape(32, 128).T)
    invf = (10000.0 ** (-np.arange(0, 64, 2, dtype=np.float32) / 64)).astype(f)
    m["invf"] = rep(invf)
    m["ident"] = np.eye(128, dtype=f)
    m["g1"] = rep(inp["norm1_g"][0])
    m["w_in"] = np.ascontiguousarray(inp["w_in"][0])
    m["convw"] = np.ascontiguousarray(np.asarray(inp["conv_w"][0], f).reshape(4, 8, 128).transpose(2, 1, 0))
    m["convb"] = colmaj(inp["conv_b"][0])
    m["lru_wa"] = np.ascontiguousarray(inp["lru_wa"][0])
    m["lru_wx"] = np.ascontiguousarray(inp["lru_wx"][0])
    m["lru_ba"] = colmaj(inp["lru_ba"][0])
    m["lru_bx"] = colmaj(inp["lru_bx"][0])
    m["lru_lam"] = colmaj(inp["lru_lambda"][0])
    m["w_rnn_o"] = np.ascontiguousarray(inp["w_rnn_o"][0])
    m["gq"] = rep(inp["q_norm_g"][0])
    m["w_uq"] = np.ascontiguousarray(inp["w_uq"][0])
    m["gkv"] = rep(inp["kv_norm_g"][0])
    m["w_ukv"] = np.ascontiguousarray(inp["w_ukv"][0])
    m["gqk_q"] = rep(inp["qk_norm_q_g"][0])
    gk = np.asarray(inp["qk_norm_k_g"][0], f)
    m["gqk_k_pe"] = rep(gk[128:192])
    m["gqk_k_col"] = np.ascontiguousarray(gk[0:128].reshape(128, 1))
    m["w_mla_o"] = np.ascontiguousarray(inp["w_mla_o"][0])
    m["w_out"] = np.ascontiguousarray(inp["w_out"][0])
    m["g2"] = rep(inp["norm2_g"][0])
    m["wr"] = np.ascontiguousarray(np.concatenate([inp["router_wg"][0], inp["router_we"][0]], 1))
    m["br"] = rep(np.concatenate([inp["router_bg"][0], inp["router_be"][0]]))
    m["w1r"] = np.ascontiguousarray(np.asarray(inp["exp_w1"][0], f).reshape(64, 8, 128, 256).transpose(0, 2, 1, 3).reshape(64 * 128, 2048))
    m["w3r"] = np.ascontiguousarray(np.asarray(inp["exp_w3"][0], f).reshape(64, 8, 128, 256).transpose(0, 2, 1, 3).reshape(64 * 128, 2048))
    m["w2r"] = np.ascontiguousarray(np.asarray(inp["exp_w2"][0], f).reshape(64, 2, 128, 1024).transpose(0, 2, 1, 3).reshape(64 * 128, 2048))
    kk = (np.arange(4)[None, :, None] * 128 + np.arange(128)[:, None, None])
    qq = np.arange(512)[None, None, :]
    diag = (kk <= qq).astype(f)
    full = np.ones_like(diag)
    zero = np.zeros_like(diag)
    mk = np.zeros((NOWN, 2, 128, 4, 512), f)
    for i in range(NOWN):
        for w in range(2):
            ku = EXT[i] - 2 + w
            mk[i, w] = full if ku < own[i] else (diag if ku == own[i] else zero)
    m["masks"] = mk.reshape(NOWN, 2, 128, 2048).astype(ml_dtypes.bfloat16)
    hid = np.zeros((128, 64), np.int32)
    for i in range(NOWN):
        for ch in range(8):
            hid[:, i * 8 + ch] = own[i] * 1024 + ch * 128 + np.arange(128)
    m["hidx"] = hid
    m["pidx"] = np.arange(128, dtype=f).reshape(128, 1)
    m["ustrict"] = np.triu(np.ones((128, 128), f), 1)
    m["thr"] = np.ascontiguousarray(np.broadcast_to((np.arange(128, dtype=f) * 128).reshape(1, 128), (128, 128)))
    return m


_NC_CACHE = {}


def kernel(**inputs):
    inp = {k: np.asarray(v) for k, v in inputs.items()}
    if "nc" not in _NC_CACHE:
        _NC_CACHE["nc"] = build()
    nc = _NC_CACHE["nc"]
    in_maps = [_prep_core(inp, cidx) for cidx in range(8)]
    res = run_bass_kernel_spmd(nc, in_maps, core_ids=list(range(8)))
    outp = np.zeros((4, S, D), np.float32)
    for cidx in range(8):
        b, j = cidx // 2, cidx % 2
        o = res.results[cidx]["out"]
        for i, u in enumerate(OWN[j]):
            outp[b, u * UT:(u + 1) * UT] = o[i * UT:(i + 1) * UT]
    return outp
```
